# Optimizing a Trainium2 kernel written in Bass

```python
import math
import jax, jax.numpy as jnp
from jax import lax
import numpy as np

D_MODEL = 4096
BATCH = 4
SEQ = 4096
DEPTH = 1

D_MIX = 2 * D_MODEL
D_SSD = D_MIX // 2
D_CONV = D_MIX - D_SSD
SSD_HEAD_DIM = 64
SSD_HEADS = D_SSD // SSD_HEAD_DIM
SSD_GROUPS = 8
SSD_HEADS_PER_GROUP = SSD_HEADS // SSD_GROUPS
SSD_STATE = 128
SSD_CONV_WIDTH = 4
SSD_CHUNK = 128
D_XBC = D_SSD + 2 * SSD_GROUPS * SSD_STATE
SC_CONV_WIDTH = 3
D_IN_PROJ = D_SSD + D_XBC + SSD_HEADS + 3 * D_CONV
PEER_HEADS = 8
PEER_N_KEYS = 128
PEER_N_EXPERTS = PEER_N_KEYS * PEER_N_KEYS
PEER_TOPK = 16
PEER_QUERY_DIM = 256
PEER_TOKEN_BLOCK = 128
RMS_EPS = 1e-6

kernel_name = "hybrid_ssd_shortconv_peer_block"


def rmsnorm(x, w, groups=1):
    shape = x.shape
    xf = x.astype(jnp.float32).reshape(*shape[:-1], groups, shape[-1] // groups)
    xf = xf * lax.rsqrt(jnp.mean(xf * xf, axis=-1, keepdims=True) + RMS_EPS)
    return (xf.reshape(shape) * w.astype(jnp.float32)).astype(x.dtype)


def causal_dwconv(x, w):
    k_w = w.shape[0]
    s = x.shape[1]
    xp = jnp.pad(x, ((0, 0), (k_w - 1, 0), (0, 0)))
    acc = xp[:, k_w - 1:k_w - 1 + s] * w[k_w - 1]
    for k in range(k_w - 1):
        acc = acc + xp[:, k:k + s] * w[k]
    return acc


def ssd_chunked_scan(xh, dt, a_neg, bm, cm):
    b, s = xh.shape[:2]
    nc = s // SSD_CHUNK

    def to_chunks(t):
        return jnp.moveaxis(t.reshape(b, nc, SSD_CHUNK, *t.shape[2:]), 1, 0)

    causal = jnp.tril(jnp.ones((SSD_CHUNK, SSD_CHUNK), dtype=bool))

    def step(state, inp):
        xc, dtc, bc, cc = inp
        acum = jnp.cumsum(dtc * a_neg, axis=1)
        seg = acum[:, :, None] - acum[:, None, :]
        decay = jnp.exp(jnp.where(causal[None, :, :, None, None], seg, -jnp.inf))
        cb = jnp.einsum('btgn,bsgn->btsg', cc, bc)
        xdt = xc * dtc[..., None]
        y_diag = jnp.einsum('btsg,btsgr,bsgrp->btgrp', cb, decay, xdt)
        y_off = jnp.einsum('btgn,bgrpn->btgrp', cc, state) * jnp.exp(acum)[..., None]
        to_end = jnp.exp(acum[:, -1:] - acum)
        new_state = (state * jnp.exp(acum[:, -1])[..., None, None]
                     + jnp.einsum('bsgn,bsgrp->bgrpn', bc, xdt * to_end[..., None]))
        return new_state, y_diag + y_off

    state0 = jnp.zeros((b, SSD_GROUPS, SSD_HEADS_PER_GROUP, SSD_HEAD_DIM, SSD_STATE), jnp.float32)
    _, ys = lax.scan(step, state0, (to_chunks(xh), to_chunks(dt), to_chunks(bm), to_chunks(cm)))
    return jnp.moveaxis(ys, 0, 1).reshape(xh.shape)


def mixer(xn, w_in, ssd_conv_w, ssd_conv_b, ssd_dt_bias, ssd_a_log, ssd_d, ssd_norm_w,
          sc_conv_w, sc_norm_w, w_out):
    b, s, _ = xn.shape
    proj = xn @ w_in
    o1 = D_SSD
    o2 = o1 + D_XBC
    o3 = o2 + SSD_HEADS
    o4 = o3 + D_CONV
    o5 = o4 + D_CONV
    z, xbc, dt_raw, sc_b, sc_c, sc_h = jnp.split(proj, [o1, o2, o3, o4, o5], axis=-1)

    xbc = jax.nn.silu(causal_dwconv(xbc, ssd_conv_w) + ssd_conv_b)
    xs, bm, cm = jnp.split(xbc, [D_SSD, D_SSD + SSD_GROUPS * SSD_STATE], axis=-1)
    dt = jax.nn.softplus(dt_raw.astype(jnp.float32) + ssd_dt_bias.astype(jnp.float32))
    a_neg = -jnp.exp(ssd_a_log.astype(jnp.float32))
    shp_h = (b, s, SSD_GROUPS, SSD_HEADS_PER_GROUP)
    xh = xs.astype(jnp.float32).reshape(*shp_h, SSD_HEAD_DIM)
    y = ssd_chunked_scan(
        xh, dt.reshape(shp_h), a_neg.reshape(SSD_GROUPS, SSD_HEADS_PER_GROUP),
        bm.astype(jnp.float32).reshape(b, s, SSD_GROUPS, SSD_STATE),
        cm.astype(jnp.float32).reshape(b, s, SSD_GROUPS, SSD_STATE))
    y = y + ssd_d.astype(jnp.float32).reshape(SSD_GROUPS, SSD_HEADS_PER_GROUP, 1) * xh
    y = y.reshape(b, s, D_SSD) * jax.nn.silu(z.astype(jnp.float32))
    y_ssd = rmsnorm(y, ssd_norm_w, groups=SSD_GROUPS).astype(xn.dtype)

    y_sc = sc_b * causal_dwconv(sc_c * sc_h, sc_conv_w)
    y_sc = rmsnorm(y_sc, sc_norm_w)

    return jnp.concatenate([y_ssd, y_sc], axis=-1) @ w_out


def peer_ffn(xn, w_query, sub_keys, u_tab, v_tab):
    b, s, d = xn.shape
    t = b * s
    xt = xn.reshape(t, d)
    q = (xt @ w_query).reshape(t, PEER_HEADS, 2, PEER_QUERY_DIM // 2)
    sc = jnp.einsum('thcd,hckd->thck', q.astype(jnp.float32), sub_keys.astype(jnp.float32))
    top_s, top_i = lax.top_k(sc, PEER_TOPK)
    kk = PEER_TOPK * PEER_TOPK
    cand_s = (top_s[:, :, 0, :, None] + top_s[:, :, 1, None, :]).reshape(t, PEER_HEADS, kk)
    cand_i = (top_i[:, :, 0, :, None] * PEER_N_KEYS + top_i[:, :, 1, None, :]).reshape(t, PEER_HEADS, kk)
    best_s, best_pos = lax.top_k(cand_s, PEER_TOPK)
    expert_idx = jnp.take_along_axis(cand_i, best_pos, axis=-1)
    gates = jax.nn.softmax(best_s, axis=-1).astype(xn.dtype)

    nblk = t // PEER_TOKEN_BLOCK
    hk = PEER_HEADS * PEER_TOPK

    def block(args):
        xb, ib, gb = args
        act = jax.nn.gelu(jnp.einsum('td,tkd->tk', xb, u_tab[ib]), approximate=False) * gb
        return jnp.einsum('tk,tkd->td', act, v_tab[ib])

    out = lax.map(block, (xt.reshape(nblk, PEER_TOKEN_BLOCK, d),
                          expert_idx.reshape(nblk, PEER_TOKEN_BLOCK, hk),
                          gates.reshape(nblk, PEER_TOKEN_BLOCK, hk)))
    return out.reshape(b, s, d)


def setup_inputs(seed: int = 0) -> dict:
    key = jax.random.key(seed)
    ks = jax.random.split(key, 20)
    f32 = jnp.float32
    L = DEPTH

    def nrm(k, shape, scale):
        return jax.random.normal(k, shape, f32) * scale

    def gain(k, shape):
        return 1.0 + 0.05 * jax.random.normal(k, shape, f32)

    dt0 = jnp.exp(jax.random.uniform(ks[4], (L, SSD_HEADS), f32)
                  * (math.log(0.1) - math.log(0.001)) + math.log(0.001))
    dt_bias = dt0 + jnp.log(-jnp.expm1(-dt0))
    a_log = jnp.log(jax.random.uniform(ks[5], (L, SSD_HEADS), f32, 1.0, 16.0))
    return {
        "x": nrm(ks[0], (BATCH, SEQ, D_MODEL), 1.0),
        "w_in": nrm(ks[1], (L, D_MODEL, D_IN_PROJ), D_MODEL ** -0.5),
        "ssd_conv_w": nrm(ks[2], (L, SSD_CONV_WIDTH, D_XBC), SSD_CONV_WIDTH ** -0.5),
        "ssd_conv_b": nrm(ks[3], (L, D_XBC), 0.01),
        "ssd_dt_bias": dt_bias,
        "ssd_a_log": a_log,
        "ssd_d": gain(ks[6], (L, SSD_HEADS)),
        "ssd_norm_w": gain(ks[7], (L, D_SSD)),
        "sc_conv_w": nrm(ks[8], (L, SC_CONV_WIDTH, D_CONV), SC_CONV_WIDTH ** -0.5),
        "sc_norm_w": gain(ks[9], (L, D_CONV)),
        "w_out": nrm(ks[10], (L, D_MIX, D_MODEL), D_MIX ** -0.5),
        "norm_mix_w": gain(ks[11], (L, D_MODEL)),
        "norm_ffn_w": gain(ks[12], (L, D_MODEL)),
        "peer_w_query": nrm(ks[13], (L, D_MODEL, PEER_HEADS * PEER_QUERY_DIM), D_MODEL ** -0.5),
        "peer_sub_keys": nrm(ks[14], (L, PEER_HEADS, 2, PEER_N_KEYS, PEER_QUERY_DIM // 2),
                             (PEER_QUERY_DIM // 2) ** -0.5),
        "peer_u": nrm(ks[15], (L, PEER_N_EXPERTS, D_MODEL), D_MODEL ** -0.5),
        "peer_v": nrm(ks[16], (L, PEER_N_EXPERTS, D_MODEL), PEER_HEADS ** -0.5),
        "norm_final_w": gain(ks[17], (D_MODEL,)),
    }


def reference(x, w_in, ssd_conv_w, ssd_conv_b, ssd_dt_bias, ssd_a_log, ssd_d, ssd_norm_w,
              sc_conv_w, sc_norm_w, w_out, norm_mix_w, norm_ffn_w, peer_w_query,
              peer_sub_keys, peer_u, peer_v, norm_final_w):
    h = x
    for l in range(DEPTH):
        xn = rmsnorm(h, norm_mix_w[l])
        h = h + mixer(xn, w_in[l], ssd_conv_w[l], ssd_conv_b[l], ssd_dt_bias[l], ssd_a_log[l],
                      ssd_d[l], ssd_norm_w[l], sc_conv_w[l], sc_norm_w[l], w_out[l])
        xn = rmsnorm(h, norm_ffn_w[l])
        h = h + peer_ffn(xn, peer_w_query[l], peer_sub_keys[l], peer_u[l], peer_v[l])
    return rmsnorm(h, norm_final_w)
```

```python
import numpy as np
from contextlib import ExitStack
import concourse.bass as bass
import concourse.mybir as mybir
from concourse.bass_utils import run_bass_kernel_spmd

F32 = mybir.dt.float32
BF16 = mybir.dt.bfloat16
U32 = mybir.dt.uint32
AF = mybir.ActivationFunctionType
ALU = mybir.AluOpType
AX = mybir.AxisListType

D = 4096
KC = D // 128
NH = 64
NG = 8
NFM = 144
HALO = 64
EPS = 1e-6
NEG = -30000.0


class Buf:
    __slots__ = ("name", "w", "r", "dsem", "dcnt", "multi")

    def __init__(self, name, multi=False):
        self.name = name
        self.w = []
        self.r = []
        self.dsem = None
        self.dcnt = 0
        self.multi = multi


class K:
    def __init__(self, nc, stack):
        self.nc = nc
        self.stack = stack
        self.E = {"pe": nc.tensor, "act": nc.scalar, "dve": nc.vector,
                  "pool": nc.gpsimd, "sp": nc.sync}
        self.sem = {}
        self.cnt = {}
        self.seen = {}
        for e in self.E:
            self.sem[e] = stack.enter_context(nc.semaphore("s_" + e))
            self.cnt[e] = 0
            self.seen[e] = {}
        self.nsem = len(self.E)
        self.out_tokens = []
        self.nbuf = 0
        self.bufs = []

    def buf(self, name=None, multi=False):
        self.nbuf += 1
        b = Buf(name or f"b{self.nbuf}", multi)
        self.bufs.append(b)
        return b

    def barrier(self):
        toks = [(self.sem[e], self.cnt[e]) for e in self.E if self.cnt[e] > 0]
        for b in self.bufs:
            toks += b.w
            toks += b.r
        toks = self._compress(toks)
        for e in self.E:
            self._wait(e, toks)
        self.bufs = [b for b in self.bufs if b.multi or b.name.startswith("const") or b.name.startswith("bank")]

    def _wait(self, e, toks, hazard=()):
        own = self.sem.get(e)
        best = {}
        for (s, v) in toks:
            if e == "pe" and s is own:
                continue
            k = id(s)
            if k not in best or best[k][1] < v:
                best[k] = (s, v)
        for (s, v) in hazard:
            if s is own:
                continue
            k = id(s)
            if k not in best or best[k][1] < v:
                best[k] = (s, v)
        seen = self.seen[e]
        for k, (s, v) in best.items():
            if seen.get(k, 0) >= v:
                continue
            self.E[e].wait_ge(s, v)
            seen[k] = v

    @staticmethod
    def _compress(toks):
        best = {}
        for (s, v) in toks:
            k = id(s)
            if k not in best or best[k][1] < v:
                best[k] = (s, v)
        return list(best.values())

    def _deps(self, reads, writes):
        raw, haz = [], []
        for b in reads:
            raw += b.w
        for b in writes:
            haz += b.w
            haz += b.r
        return raw, haz

    def op(self, e, fn, reads=(), writes=(), inc=True):
        raw, haz = self._deps(reads, writes)
        self._wait(e, raw, haz)
        ins = fn(self.E[e])
        tok = (self.sem[e], self.cnt[e] + 1)
        if inc:
            ins.then_inc(self.sem[e], 1)
            self.cnt[e] += 1
        for b in reads:
            b.r.append(tok)
            if len(b.r) > 16:
                b.r = self._compress(b.r)
        for b in writes:
            b.w = [tok]
            b.r = []
        return ins

    def _dsem(self, b):
        if b.dsem is None:
            b.dsem = self.stack.enter_context(self.nc.semaphore(f"d{self.nsem}"))
            b.dcnt = 0
            self.nsem += 1
        return b.dsem

    def dma(self, q, out, in_, reads=(), write=None, is_output=False, **kw):
        raw, haz = self._deps(reads, [] if write.multi else [write])
        if write.multi:
            haz = haz + write.r
        self._wait(q, raw, haz)
        s = self._dsem(write)
        write.dcnt += 16
        ins = self.E[q].dma_start(out=out, in_=in_, **kw)
        ins.then_inc(s, 16)
        tok = (s, write.dcnt)
        for b in reads:
            b.r.append(tok)
            if len(b.r) > 16:
                b.r = self._compress(b.r)
        write.w = [tok]
        if not write.multi:
            write.r = []
        if is_output:
            self.out_tokens.append(tok)
        return ins

    def finish(self):
        self._wait("sp", self.out_tokens)


class Ring:
    def __init__(self, items):
        self.items = items
        self.i = 0

    def get(self):
        it = self.items[self.i % len(self.items)]
        self.i += 1
        return it


def split_tiles(lo, hi, tmax, smax=512):
    n = hi - lo
    nt = (n + tmax - 1) // tmax
    base = (n + nt - 1) // nt
    base = ((base + 31) // 32) * 32
    tiles = []
    p = lo
    while p < hi:
        ln = min(base, hi - p)
        subs = []
        q = 0
        while q < ln:
            sl = min(smax, ln - q)
            subs.append((q, sl))
            q += sl
        tiles.append((p, ln, subs))
        p += ln
    return tiles


def build(TPRE, TOWN, debug=False, phases=None, scratch_in=()):
    TALL = TPRE + TOWN
    nc = bass.Bass("TRN2", target_bir_lowering=False)
    ALLP = ["norm1", "inproj", "zdt", "ssd", "scpack", "outproj", "norm2", "qg", "gemm1", "gemm2", "final"]
    phases = set(ALLP) if phases is None else set(phases)

    _din = {}

    def din(name, shape, dt=F32):
        if name not in _din:
            _din[name] = nc.dram_tensor(name, list(shape), dt, kind="ExternalInput").ap()
        return _din[name]

    def dscr(name, shape, dt=F32):
        if name in scratch_in:
            kind = "ExternalInput"
        else:
            kind = "ExternalOutput" if debug else "Internal"
        return nc.dram_tensor(name, list(shape), dt, kind=kind).ap()

    class _Lazy:
        def __init__(self, name, shape, dt=F32):
            self.a = (name, shape, dt)

        def __call__(self):
            return din(*self.a)

    I = dict(
        x_all=_Lazy("x_all", [TALL, D]), flag=_Lazy("flag", [128, 1]),
        w_fm=_Lazy("w_fm", [NFM, 128, KC * 128]), w_z=_Lazy("w_z", [8, 128, KC * 512]),
        w_dt=_Lazy("w_dt", [128, KC * 64]), cw_xbc=_Lazy("cw_xbc", [128, 48, 4]),
        cb_xbc=_Lazy("cb_xbc", [128, 48]), cw_sc=_Lazy("cw_sc", [128, 32, 3]),
        g_mix=_Lazy("g_mix", [D]), g_ffn=_Lazy("g_ffn", [D]), g_fin=_Lazy("g_fin", [D]),
        g_ssd=_Lazy("g_ssd", [D]), g_sc=_Lazy("g_sc", [128, 32]),
        dt_bias=_Lazy("dt_bias", [NH]), a_log=_Lazy("a_log", [NH]), d_skip=_Lazy("d_skip", [NH]),
        w_out=_Lazy("w_out", [8, 2, 128, 32 * 512]), w_q=_Lazy("w_q", [16, 128, KC * 128]),
        keys_t=_Lazy("keys_t", [16, 128, 128]), u_t=_Lazy("u_t", [128, 128, KC * 128]),
        v_nat=_Lazy("v_nat", [128 * 128, D]), c_ident=_Lazy("c_ident", [128, 128]),
        c_triu=_Lazy("c_triu", [128, 128]), c_negm=_Lazy("c_negm", [128, 512]),
        c_iota=_Lazy("c_iota", [128, 128]),
    )

    out = nc.dram_tensor("out", [TOWN, D], F32, kind="ExternalOutput").ap()

    xnT_d = dscr("xnT_d", [128, KC, TALL], BF16)
    xbcT_d = dscr("xbcT_d", [48 * 128, TALL])
    ysc_d = dscr("ysc_d", [32 * 128, TOWN + HALO])
    rstd_sc_d = dscr("rstd_sc_d", [128, TOWN + HALO])
    zs_d = dscr("zs_d", [TOWN, D])
    dtr_d = dscr("dtr_d", [TALL, NH])
    yT_d = dscr("yT_d", [2 * D, TOWN], BF16)
    h1_d = dscr("h1_d", [TOWN, D])
    xn2T_d = dscr("xn2T_d", [128, KC, TOWN], BF16)
    G_d = dscr("G_d", [128, 128, TOWN], BF16)
    HG_d = dscr("HG_d", [128 * 128, TOWN], BF16)
    h2_d = dscr("h2_d", [TOWN, D])
    vb_d = dscr("vb_d", [128 * 128, D], BF16)

    with ExitStack() as top:
        k = K(nc, top)
        B_xnT = k.buf("xnT_d", multi=True)
        B_xbcT = k.buf("xbcT_d", multi=True)
        B_ysc = k.buf("ysc_d", multi=True)
        B_rstdsc = k.buf("rstd_sc_d", multi=True)
        B_zs = k.buf("zs_d", multi=True)
        B_dtr = k.buf("dtr_d", multi=True)
        B_out = k.buf("out", multi=True)
        B_yT = k.buf("yT_d", multi=True)
        B_h1 = k.buf("h1_d", multi=True)
        B_xn2T = k.buf("xn2T_d", multi=True)
        B_G = k.buf("G_d", multi=True)
        B_HG = k.buf("HG_d", multi=True)
        B_h2 = k.buf("h2_d", multi=True)
        B_vb = k.buf("vb_d", multi=True)

        def bcast_row(vec_ap, n):
            return bass.AP(vec_ap.tensor, vec_ap.offset, [[0, 128], [1, n]])

        def sb(st, name, shape, dt):
            return st.enter_context(nc.sbuf_tensor(name, list(shape), dt))

        ident_f = sb(top, "ident_f", [128, 128], F32)
        ident_b = sb(top, "ident_b", [128, 128], BF16)
        ones_f = sb(top, "ones_f", [128, 128], F32)
        B_const = k.buf("const")
        k.dma("sp", ident_f[:], I["c_ident"](), write=B_const)
        B_c2 = k.buf("const2")
        k.dma("pool", ident_b[:], I["c_ident"](), write=B_c2)
        B_c3 = k.buf("const3")
        k.op("dve", lambda e: e.memset(ones_f[:], 1.0), writes=[B_c3])
        CONST = [B_const, B_c2, B_c3]

        vb_pieces = [r_ for r_ in range(0, 128 * 128, 256)] if "gemm2" in phases else []

        def vb_piece():
            if vb_pieces:
                r_ = vb_pieces.pop(0)
                k.dma("pool", vb_d[r_:r_ + 256, :], I["v_nat"]()[r_:r_ + 256, :], write=B_vb)

        if "inproj" not in phases:
            while vb_pieces:
                vb_piece()

        banks = []
        for i in range(8):
            t = top.enter_context(nc.psum_tensor(f"bank{i}", [128, 512], F32))
            banks.append((t, k.buf(f"bank{i}")))
        psum = Ring(banks)

        def phase_norm(src_d, nrows, gain_d, dstT_d, B_dst, tag, src_dep=None):
            k.barrier()
            with ExitStack() as st:
                gain = sb(st, tag + "gain", [128, D], F32)
                Bg = k.buf()
                k.dma("sp", gain[:], bcast_row(gain_d, D), write=Bg)
                xr = Ring([(sb(st, f"{tag}x{i}", [128, D], F32), k.buf()) for i in range(2)])
                sqr = Ring([(sb(st, f"{tag}sq{i}", [128, D], F32), k.buf()) for i in range(1)])
                xnr = Ring([(sb(st, f"{tag}xn{i}", [128, D], BF16), k.buf()) for i in range(2)])
                str_ = Ring([(sb(st, f"{tag}st{i}", [128, KC, 512], BF16), k.buf()) for i in range(2)])
                smr = Ring([(sb(st, f"{tag}sm{i}", [128, 2], F32), k.buf()) for i in range(2)])
                for t0 in range(0, nrows, 512):
                    stg, Bst = str_.get()
                    for bi in range(4):
                        r0 = t0 + bi * 128
                        xt, Bx = xr.get()
                        k.dma("sp", xt[:], src_d[r0:r0 + 128, :], reads=([src_dep] if src_dep is not None else []), write=Bx)
                        sq, Bsq = sqr.get()
                        sm, Bsm = smr.get()
                        k.op("act", lambda e: e.activation(out=sq[:], in_=xt[:], func=AF.Square,
                                                           accum_out=sm[:, 0:1]),
                             reads=[Bx], writes=[Bsq, Bsm])
                        k.op("act", lambda e: e.activation(out=sm[:, 1:2], in_=sm[:, 0:1], func=AF.Sqrt,
                                                           bias=EPS, scale=1.0 / D),
                             reads=[Bsm], writes=[Bsm])
                        k.op("dve", lambda e: e.reciprocal(out=sm[:, 1:2], in_=sm[:, 1:2]),
                             reads=[Bsm], writes=[Bsm])
                        xn, Bxn = xnr.get()
                        k.op("dve", lambda e: e.scalar_tensor_tensor(
                            out=xn[:], in0=xt[:], scalar=sm[:, 1:2], in1=gain[:],
                            op0=ALU.mult, op1=ALU.mult), reads=[Bx, Bsm, Bg], writes=[Bxn])
                        for grp in range(4):
                            pt, Bp = psum.get()
                            ptb = pt[:].bitcast(BF16).rearrange("p (a b) -> p a b", a=8)
                            for j in range(8):
                                c = grp * 8 + j
                                k.op("pe", lambda e: e.transpose(out=ptb[:, j, :],
                                                                 in_=xn[:, c * 128:(c + 1) * 128],
                                                                 identity=ident_b[:]),
                                     reads=[Bxn, B_c2], writes=[Bp], inc=(j == 7))
                            k.op("act", lambda e: e.copy(
                                out=stg[:, grp * 8:(grp + 1) * 8, bi * 128:(bi + 1) * 128], in_=ptb),
                                 reads=[Bp], writes=[Bst])
                    k.dma("sp", dstT_d[:, :, t0:t0 + 512], stg[:], reads=[Bst], write=B_dst)

        if "norm1" in phases:
            phase_norm(I["x_all"](), TALL, I["g_mix"](), xnT_d, B_xnT, "n1")

        def phase_inproj_fm():
            k.barrier()
            with ExitStack() as st:
                TMAX = 1056
                cwx = sb(st, "cwx", [128, 48, 4], F32)
                cbx = sb(st, "cbx", [128, 48], F32)
                cws = sb(st, "cws", [128, 32, 3], F32)
                Bcw = k.buf()
                k.dma("sp", cwx[:], I["cw_xbc"](), write=Bcw)
                Bcb = k.buf()
                k.dma("sp", cbx[:], I["cb_xbc"](), write=Bcb)
                Bcs = k.buf()
                k.dma("sp", cws[:], I["cw_sc"](), write=Bcs)
                hs = sb(st, "hs", [128, 112, 4], F32)
                Bhs = k.buf()
                k.op("dve", lambda e: e.memset(hs[:], 0.0), writes=[Bhs])
                xT = sb(st, "ipxT", [128, KC, TMAX], BF16)
                BxT = k.buf()
                wr = Ring([(sb(st, f"ipw{i}", [128, KC, 128], BF16), k.buf()) for i in range(3)])
                pr = Ring([(sb(st, f"ipP{i}", [128, 4 + TMAX], F32), k.buf()) for i in range(2)])
                ar = Ring([(sb(st, f"ipA{i}", [128, TMAX], F32), k.buf()) for i in range(2)])
                orr = Ring([(sb(st, f"ipO{i}", [128, TMAX], F32), k.buf()) for i in range(2)])
                csr = Ring([(sb(st, f"ipC{i}", [128, TMAX], F32), k.buf()) for i in range(1)])
                ssq = sb(st, "ipssq", [128, TMAX], F32)
                Bssq = k.buf()
                sqt = sb(st, "ipsqt", [128, TMAX], F32)
                Bsqt = k.buf()

                ncall = [0]

                def gemm_chunk(j, ln, subs):
                    wt, Bw = wr.get()
                    k.dma("pool", wt[:].rearrange("p c n -> p (c n)"), I["w_fm"]()[j], write=Bw)
                    ncall[0] += 1
                    if ncall[0] % 4 == 0:
                        vb_piece()
                    res = []
                    for (so, sl) in subs:
                        pt, Bp = psum.get()
                        for c in range(KC):
                            k.op("pe", lambda e: e.matmul(pt[:, 0:sl], lhsT=wt[:, c, :],
                                                          rhs=xT[:, c, so:so + sl],
                                                          start=(c == 0), stop=(c == KC - 1)),
                                 reads=[Bw, BxT], writes=[Bp], inc=(c == KC - 1))
                        res.append((pt, Bp, so, sl))
                    return res

                def conv(P, BP, A, BA, ln, taps, wtile, Bwt, ci, eng):
                    base = 4 - (taps - 1)
                    k.op(eng, lambda e: e.tensor_scalar(out=A[:, 0:ln], in0=P[:, base:base + ln],
                                                        scalar1=wtile[:, ci, 0:1], scalar2=None,
                                                        op0=ALU.mult),
                         reads=[BP, Bwt], writes=[BA])
                    for t in range(1, taps):
                        k.op(eng, lambda e: e.scalar_tensor_tensor(
                            out=A[:, 0:ln], in0=P[:, base + t:base + t + ln], scalar=wtile[:, ci, t:t + 1],
                            in1=A[:, 0:ln], op0=ALU.mult, op1=ALU.add),
                             reads=[BP, Bwt, BA], writes=[BA])

                def run_tile(t0, ln, subs, chunks, own):
                    k.dma("sp", xT[:, :, 0:ln], xnT_d[:, :, t0:t0 + ln], reads=[B_xnT], write=BxT)
                    for j in chunks:
                        res = gemm_chunk(j, ln, subs)
                        P, BP = pr.get()
                        k.op("dve", lambda e: e.tensor_copy(out=P[:, 0:4], in_=hs[:, j, :]),
                             reads=[Bhs], writes=[BP])
                        for (pt, Bp, so, sl) in res:
                            k.op("act", lambda e: e.copy(out=P[:, 4 + so:4 + so + sl], in_=pt[:, 0:sl]),
                                 reads=[Bp], writes=[BP])
                        k.op("dve", lambda e: e.tensor_copy(out=hs[:, j, :], in_=P[:, ln:ln + 4]),
                             reads=[BP], writes=[Bhs])
                        A, BA = ar.get()
                        conv(P, BP, A, BA, ln, 4, cwx, Bcw, j, "dve")
                        O, BO = orr.get()
                        k.op("act", lambda e: e.activation(out=O[:, 0:ln], in_=A[:, 0:ln], func=AF.Silu,
                                                           bias=cbx[:, j:j + 1], scale=1.0),
                             reads=[BA, Bcb], writes=[BO])
                        k.dma("sp", xbcT_d[j * 128:(j + 1) * 128, t0:t0 + ln], O[:, 0:ln],
                              reads=[BO], write=B_xbcT)
                    if not own:
                        return
                    o0 = t0 - (TPRE - HALO)
                    for i in range(32):
                        res_c = gemm_chunk(48 + 32 + i, ln, subs)
                        Cs, BCs = csr.get()
                        for (pt, Bp, so, sl) in res_c:
                            k.op("act", lambda e: e.copy(out=Cs[:, so:so + sl], in_=pt[:, 0:sl]),
                                 reads=[Bp], writes=[BCs])
                        res_h = gemm_chunk(48 + 64 + i, ln, subs)
                        P, BP = pr.get()
                        k.op("dve", lambda e: e.tensor_copy(out=P[:, 0:4], in_=hs[:, 48 + i, :]),
                             reads=[Bhs], writes=[BP])
                        for (pt, Bp, so, sl) in res_h:
                            k.op("dve", lambda e: e.tensor_tensor(out=P[:, 4 + so:4 + so + sl],
                                                                  in0=pt[:, 0:sl], in1=Cs[:, so:so + sl],
                                                                  op=ALU.mult),
                                 reads=[Bp, BCs], writes=[BP])
                        k.op("dve", lambda e: e.tensor_copy(out=hs[:, 48 + i, :], in_=P[:, ln:ln + 4]),
                             reads=[BP], writes=[Bhs])
                        A, BA = ar.get()
                        conv(P, BP, A, BA, ln, 3, cws, Bcs, i, "dve")
                        res_b = gemm_chunk(48 + i, ln, subs)
                        O, BO = orr.get()
                        for (pt, Bp, so, sl) in res_b:
                            k.op("dve", lambda e: e.tensor_tensor(out=O[:, so:so + sl], in0=pt[:, 0:sl],
                                                                  in1=A[:, so:so + sl], op=ALU.mult),
                                 reads=[Bp, BA], writes=[BO])
                        k.dma("sp", ysc_d[i * 128:(i + 1) * 128, o0:o0 + ln], O[:, 0:ln],
                              reads=[BO], write=B_ysc)
                        if i == 0:
                            k.op("act", lambda e: e.activation(out=ssq[:, 0:ln], in_=O[:, 0:ln],
                                                               func=AF.Square),
                                 reads=[BO], writes=[Bssq])
                        else:
                            k.op("act", lambda e: e.activation(out=sqt[:, 0:ln], in_=O[:, 0:ln],
                                                               func=AF.Square),
                                 reads=[BO], writes=[Bsqt])
                            k.op("pool", lambda e: e.tensor_tensor(out=ssq[:, 0:ln], in0=ssq[:, 0:ln],
                                                                   in1=sqt[:, 0:ln], op=ALU.add),
                                 reads=[Bsqt, Bssq], writes=[Bssq])
                    O, BO = orr.get()
                    for (so, sl) in subs:
                        pt, Bp = psum.get()
                        k.op("pe", lambda e: e.matmul(pt[:, 0:sl], lhsT=ones_f[:], rhs=ssq[:, so:so + sl],
                                                      start=True, stop=True),
                             reads=[Bssq, B_c3], writes=[Bp])
                        k.op("act", lambda e: e.activation(out=O[:, so:so + sl], in_=pt[:, 0:sl],
                                                           func=AF.Sqrt, bias=EPS, scale=1.0 / D),
                             reads=[Bp], writes=[BO])
                    k.op("dve", lambda e: e.reciprocal(out=O[:, 0:ln], in_=O[:, 0:ln]),
                         reads=[BO], writes=[BO])
                    k.dma("sp", rstd_sc_d[:, o0:o0 + ln], O[:, 0:ln], reads=[BO], write=B_rstdsc)

                if TPRE > HALO:
                    for (t0, ln, subs) in split_tiles(0, TPRE - HALO, TMAX):
                        run_tile(t0, ln, subs, list(range(0, 40)), False)
                for (t0, ln, subs) in split_tiles(TPRE - HALO, TALL, TMAX):
                    run_tile(t0, ln, subs, list(range(0, 48)), True)

        if "inproj" in phases:
            phase_inproj_fm()
        while vb_pieces:
            vb_piece()

        def phase_zdt():
            k.barrier()
            with ExitStack() as st:
                wdt = sb(st, "zwdt", [128, KC, 64], BF16)
                Bwdt = k.buf()
                k.dma("pool", wdt[:].rearrange("p c n -> p (c n)"), I["w_dt"](), write=Bwdt)
                xr = Ring([(sb(st, f"zx{i}", [128, KC, 512], BF16), k.buf()) for i in range(2)])
                wr = Ring([(sb(st, f"zw{i}", [128, KC, 512], BF16), k.buf()) for i in range(2)])
                orr = Ring([(sb(st, f"zo{i}", [128, 512], F32), k.buf()) for i in range(3)])
                dr = Ring([(sb(st, f"zd{i}", [128, 64], F32), k.buf()) for i in range(2)])
                for t0 in range(0, TALL, 512):
                    xT, BxT = xr.get()
                    k.dma("sp", xT[:], xnT_d[:, :, t0:t0 + 512], reads=[B_xnT], write=BxT)
                    for blk in range(4):
                        pt, Bp = psum.get()
                        for c in range(KC):
                            k.op("pe", lambda e: e.matmul(pt[:, 0:64], lhsT=xT[:, c, blk * 128:(blk + 1) * 128],
                                                          rhs=wdt[:, c, :], start=(c == 0), stop=(c == KC - 1)),
                                 reads=[BxT, Bwdt], writes=[Bp], inc=(c == KC - 1))
                        dd, Bd = dr.get()
                        k.op("act", lambda e: e.copy(out=dd[:], in_=pt[:, 0:64]), reads=[Bp], writes=[Bd])
                        k.dma("sp", dtr_d[t0 + blk * 128:t0 + (blk + 1) * 128, :], dd[:], reads=[Bd], write=B_dtr)
                    if t0 < TPRE:
                        continue
                    o0 = t0 - TPRE
                    for g in range(8):
                        wz, Bwz = wr.get()
                        k.dma("pool", wz[:].rearrange("p c n -> p (c n)"), I["w_z"]()[g], write=Bwz)
                        for blk in range(4):
                            pt, Bp = psum.get()
                            for c in range(KC):
                                k.op("pe", lambda e: e.matmul(pt[:], lhsT=xT[:, c, blk * 128:(blk + 1) * 128],
                                                              rhs=wz[:, c, :], start=(c == 0), stop=(c == KC - 1)),
                                     reads=[BxT, Bwz], writes=[Bp], inc=(c == KC - 1))
                            oo, Bo = orr.get()
                            k.op("act", lambda e: e.activation(out=oo[:], in_=pt[:], func=AF.Silu),
                                 reads=[Bp], writes=[Bo])
                            k.dma("sp", zs_d[o0 + blk * 128:o0 + (blk + 1) * 128, g * 512:(g + 1) * 512], oo[:],
                                  reads=[Bo], write=B_zs)

        if "zdt" in phases:
            phase_zdt()

        def phase_ssd():
            k.barrier()
            with ExitStack() as st:
                triu_f = sb(st, "s_triu", [128, 128], F32)
                negm_f = sb(st, "s_negm", [128, 512], F32)
                gssd = sb(st, "s_gssd", [128, D], F32)
                abc = sb(st, "s_abc", [128, NH], F32)
                dtb = sb(st, "s_dtb", [128, NH], F32)
                dsk = sb(st, "s_dsk", [128, NH], F32)
                flg = sb(st, "s_flag", [128, 1], F32)
                Bc = k.buf()
                k.dma("sp", triu_f[:], I["c_triu"](), write=Bc)
                Bc1 = k.buf()
                k.dma("sp", negm_f[:], I["c_negm"](), write=Bc1)
                Bg = k.buf()
                k.dma("sp", gssd[:], bcast_row(I["g_ssd"](), D), write=Bg)
                Ba = k.buf()
                k.dma("sp", abc[:], bcast_row(I["a_log"](), NH), write=Ba)
                k.op("act", lambda e: e.activation(out=abc[:], in_=abc[:], func=AF.Exp), reads=[Ba], writes=[Ba])
                k.op("dve", lambda e: e.tensor_scalar(out=abc[:], in0=abc[:], scalar1=-1.0, scalar2=None, op0=ALU.mult),
                     reads=[Ba], writes=[Ba])
                Bdb = k.buf()
                k.dma("sp", dtb[:], bcast_row(I["dt_bias"](), NH), write=Bdb)
                Bds = k.buf()
                k.dma("sp", dsk[:], bcast_row(I["d_skip"](), NH), write=Bds)
                Bfl = k.buf()
                k.dma("sp", flg[:], I["flag"](), write=Bfl)

                S = sb(st, "s_S", [128, D], F32)
                S_bf = sb(st, "s_Sbf", [128, D], BF16)
                BS = k.buf()
                BSb = k.buf()
                k.op("dve", lambda e: e.memset(S[:], 0.0), writes=[BS])
                k.op("pool", lambda e: e.memset(S_bf[:], 0.0), writes=[BSb])

                xin = Ring([(sb(st, f"s_xin{i}", [128, 32, 128], F32), k.buf()) for i in range(2)])
                bin_ = Ring([(sb(st, f"s_bin{i}", [128, 8, 128], F32), k.buf()) for i in range(2)])
                cin = Ring([(sb(st, f"s_cin{i}", [128, 8, 128], F32), k.buf()) for i in range(2)])
                dtin = Ring([(sb(st, f"s_dtin{i}", [128, NH], F32), k.buf()) for i in range(2)])
                zin = Ring([(sb(st, f"s_zin{i}", [128, D], F32), k.buf()) for i in range(1)])
                xdt = sb(st, "s_xdt", [128, D], BF16)
                xdtw = sb(st, "s_xdtw", [128, D], BF16)
                xD = sb(st, "s_xD", [128, D], BF16)
                Btm = sb(st, "s_Btm", [128, 1024], BF16)
                BTb = sb(st, "s_BTb", [128, 8, 128], BF16)
                CTb = sb(st, "s_CTb", [128, 8, 128], BF16)
                Bxdt, Bxdtw, BxD, BBtm, BBTb, BCTb = [k.buf() for _ in range(6)]
                sm = sb(st, "s_sm", [128, 10, NH], F32)
                Bsm = [k.buf() for _ in range(10)]
                rhsg = Ring([(sb(st, f"s_rhsg{i}", [128, 8, 128], F32), k.buf()) for i in range(2)])
                Er = Ring([(sb(st, f"s_E{i}", [128, 8, 128], F32), k.buf()) for i in range(2)])
                MTr = Ring([(sb(st, f"s_MT{i}", [128, 8, 128], BF16), k.buf()) for i in range(2)])
                cbr = Ring([(sb(st, f"s_cb{i}", [128, 128], F32), k.buf()) for i in range(2)])
                tmpr = Ring([(sb(st, f"s_tmp{i}", [128, 512], F32), k.buf()) for i in range(2)])
                ygr = Ring([(sb(st, f"s_yg{i}", [128, 512], F32), k.buf()) for i in range(2)])
                sqr = Ring([(sb(st, f"s_sq{i}", [128, 512], F32), k.buf()) for i in range(1)])
                ynr = Ring([(sb(st, f"s_yn{i}", [128, 512], BF16), k.buf()) for i in range(2)])
                ssr = Ring([(sb(st, f"s_ss{i}", [128, 2], F32), k.buf()) for i in range(2)])
                yTs = Ring([(sb(st, f"s_yT{i}", [128, 32, 128], BF16), k.buf()) for i in range(2)])

                xrows = xbcT_d[0:4096, :].rearrange("(c p) t -> p c t", p=128)
                brows = xbcT_d[4096:5120, :].rearrange("(c p) t -> p c t", p=128)
                crows = xbcT_d[5120:6144, :].rearrange("(c p) t -> p c t", p=128)
                yrows = yT_d[0:4096, :].rearrange("(c p) t -> p c t", p=128)

                def h3(ap_, g):
                    return ap_[:, 8 * g:8 * g + 8].unsqueeze(2).to_broadcast([128, 8, 64])

                nchunks = TALL // 128
                for ci in range(nchunks):
                    t0 = ci * 128
                    own = t0 >= TPRE
                    need_sbf = (t0 + 128 >= TPRE) and (ci + 1 < nchunks)
                    xi, Bxi = xin.get()
                    k.dma("sp", xi[:], xrows[:, :, t0:t0 + 128], reads=[B_xbcT], write=Bxi)
                    bi_, Bbi = bin_.get()
                    k.dma("sp", bi_[:], brows[:, :, t0:t0 + 128], reads=[B_xbcT], write=Bbi)
                    dti, Bdti = dtin.get()
                    k.dma("sp", dti[:], dtr_d[t0:t0 + 128, :], reads=[B_dtr], write=Bdti)
                    if own:
                        ci_, Bci = cin.get()
                        k.dma("sp", ci_[:], crows[:, :, t0:t0 + 128], reads=[B_xbcT], write=Bci)
                        zi, Bzi = zin.get()
                        k.dma("sp", zi[:], zs_d[t0 - TPRE:t0 - TPRE + 128, :], reads=[B_zs], write=Bzi)
                    v, ab, mx, dt_, a_ = (sm[:, i, :] for i in range(5))
                    k.op("dve", lambda e: e.tensor_tensor(out=v, in0=dti[:], in1=dtb[:], op=ALU.add),
                         reads=[Bdti, Bdb], writes=[Bsm[0]])
                    k.op("act", lambda e: e.activation(out=ab, in_=v, func=AF.Abs),
                         reads=[Bsm[0]], writes=[Bsm[1]])
                    k.op("act", lambda e: e.activation(out=ab, in_=ab, func=AF.Exp, scale=-1.0),
                         reads=[Bsm[1]], writes=[Bsm[1]])
                    k.op("act", lambda e: e.activation(out=ab, in_=ab, func=AF.Ln, bias=1.0, scale=1.0),
                         reads=[Bsm[1]], writes=[Bsm[1]])
                    k.op("dve", lambda e: e.tensor_scalar_max(out=mx, in0=v, scalar1=0.0),
                         reads=[Bsm[0]], writes=[Bsm[2]])
                    k.op("dve", lambda e: e.tensor_tensor(out=dt_, in0=mx, in1=ab, op=ALU.add),
                         reads=[Bsm[1], Bsm[2]], writes=[Bsm[3]])
                    k.op("dve", lambda e: e.tensor_tensor(out=a_, in0=dt_, in1=abc[:], op=ALU.mult),
                         reads=[Bsm[3], Ba], writes=[Bsm[4]])
                    pA, BpA = psum.get()
                    k.op("pe", lambda e: e.matmul(pA[:, 0:64], lhsT=triu_f[:], rhs=a_, start=True, stop=True),
                         reads=[Bc, Bsm[4]], writes=[BpA])
                    k.op("pe", lambda e: e.matmul(pA[:, 64:128], lhsT=ones_f[:], rhs=a_, start=True, stop=True),
                         reads=[B_c3, Bsm[4]], writes=[BpA])
                    acum, nacum, eacum, toend, dA = (sm[:, i, :] for i in range(5, 10))
                    k.op("act", lambda e: e.copy(out=acum, in_=pA[:, 0:64]), reads=[BpA], writes=[Bsm[5]])
                    k.op("act", lambda e: e.mul(out=nacum, in_=pA[:, 0:64], mul=-1.0), reads=[BpA], writes=[Bsm[6]])
                    if own:
                        k.op("act", lambda e: e.activation(out=eacum, in_=pA[:, 0:64], func=AF.Exp),
                             reads=[BpA], writes=[Bsm[7]])
                    k.op("dve", lambda e: e.tensor_tensor(out=toend, in0=pA[:, 64:128], in1=acum, op=ALU.subtract),
                         reads=[BpA, Bsm[5]], writes=[Bsm[8]])
                    k.op("act", lambda e: e.activation(out=toend, in_=toend, func=AF.Exp),
                         reads=[Bsm[8]], writes=[Bsm[8]])
                    k.op("act", lambda e: e.activation(out=dA, in_=pA[:, 64:128], func=AF.Exp),
                         reads=[BpA], writes=[Bsm[9]])
                    for g in range(NG):
                        pt, Bp = psum.get()
                        for j in range(4):
                            k.op("pe", lambda e: e.transpose(out=pt[:, j * 128:(j + 1) * 128], in_=xi[:, 4 * g + j, :],
                                                             identity=ident_f[:]),
                                 reads=[Bxi, B_const], writes=[Bp], inc=(j == 3))
                        p3 = pt[:].rearrange("p (h d) -> p h d", h=8)
                        k.op("dve", lambda e: e.tensor_tensor(
                            out=xdt[:, 512 * g:512 * (g + 1)].rearrange("p (h d) -> p h d", h=8),
                            in0=p3, in1=h3(dt_, g), op=ALU.mult), reads=[Bp, Bsm[3]], writes=[Bxdt])
                        if own:
                            k.op("dve", lambda e: e.tensor_tensor(
                                out=xD[:, 512 * g:512 * (g + 1)].rearrange("p (h d) -> p h d", h=8),
                                in0=p3, in1=h3(dsk[:], g), op=ALU.mult), reads=[Bp, Bds], writes=[BxD])
                        k.op("pool", lambda e: e.tensor_tensor(
                            out=xdtw[:, 512 * g:512 * (g + 1)].rearrange("p (h d) -> p h d", h=8),
                            in0=xdt[:, 512 * g:512 * (g + 1)].rearrange("p (h d) -> p h d", h=8),
                            in1=h3(toend, g), op=ALU.mult), reads=[Bxdt, Bsm[8]], writes=[Bxdtw])
                    for half in range(2):
                        pt, Bp = psum.get()
                        for j in range(4):
                            k.op("pe", lambda e: e.transpose(out=pt[:, j * 128:(j + 1) * 128], in_=bi_[:, 4 * half + j, :],
                                                             identity=ident_f[:]),
                                 reads=[Bbi, B_const], writes=[Bp], inc=(j == 3))
                        k.op("act", lambda e: e.copy(out=Btm[:, 512 * half:512 * (half + 1)], in_=pt[:]),
                             reads=[Bp], writes=[BBtm])
                    if own:
                        k.op("act", lambda e: e.copy(out=BTb[:], in_=bi_[:]), reads=[Bbi], writes=[BBTb])
                        k.op("pool", lambda e: e.tensor_copy(out=CTb[:], in_=ci_[:]), reads=[Bci], writes=[BCTb])
                        yT, ByT = yTs.get()
                    def front(g):
                        gs = slice(512 * g, 512 * (g + 1))
                        pc, Bpc = psum.get()
                        k.op("pe", lambda e: e.matmul(pc[:, 0:128], lhsT=BTb[:, g, :], rhs=CTb[:, g, :],
                                                      start=True, stop=True),
                             reads=[BBTb, BCTb], writes=[Bpc])
                        cb, Bcb = cbr.get()
                        k.op("act", lambda e: e.copy(out=cb[:], in_=pc[:, 0:128]), reads=[Bpc], writes=[Bcb])
                        rg, Brg = rhsg.get()
                        k.op("dve", lambda e: e.tensor_tensor(
                            out=rg[:], in0=triu_f[:].unsqueeze(1).to_broadcast([128, 8, 128]),
                            in1=a_[:, 8 * g:8 * g + 8].unsqueeze(2).to_broadcast([128, 8, 128]), op=ALU.mult),
                             reads=[Bc, Bsm[4]], writes=[Brg])
                        E, BE = Er.get()
                        for j in range(2):
                            pseg, Bps = psum.get()
                            k.op("pe", lambda e: e.matmul(
                                pseg[:], lhsT=ones_f[:], rhs=rg[:, 4 * j:4 * j + 4, :].rearrange("p h t -> p (h t)"),
                                start=True, stop=False), reads=[B_c3, Brg], writes=[Bps], inc=False)
                            k.op("pe", lambda e: e.matmul(pseg[:], lhsT=ident_f[:], rhs=negm_f[:],
                                                          start=False, stop=True),
                                 reads=[B_const, Bc1], writes=[Bps])
                            for hh in range(4):
                                h = 8 * g + 4 * j + hh
                                k.op("act", lambda e: e.activation(
                                    out=E[:, 4 * j + hh, :], in_=pseg[:, hh * 128:(hh + 1) * 128], func=AF.Exp,
                                    bias=nacum[:, h:h + 1], scale=1.0), reads=[Bps, Bsm[6]], writes=[BE])
                        MT, BMT = MTr.get()
                        k.op("dve", lambda e: e.tensor_tensor(
                            out=MT[:], in0=E[:], in1=cb[:].unsqueeze(1).to_broadcast([128, 8, 128]), op=ALU.mult),
                             reads=[BE, Bcb], writes=[BMT])
                        return MT, BMT

                    def back(g, MT, BMT):
                        gs = slice(512 * g, 512 * (g + 1))
                        if own:
                            py, Bpy = psum.get()
                            k.op("pe", lambda e: e.matmul(py[:], lhsT=ident_b[:], rhs=xD[:, gs], start=True, stop=False),
                                 reads=[B_c2, BxD], writes=[Bpy], inc=False)
                            for hh in range(8):
                                k.op("pe", lambda e: e.matmul(py[:, 64 * hh:64 * (hh + 1)], lhsT=MT[:, hh, :],
                                                              rhs=xdt[:, 512 * g + 64 * hh:512 * g + 64 * (hh + 1)],
                                                              start=False, stop=(hh == 7)),
                                     reads=[BMT, Bxdt], writes=[Bpy], inc=(hh == 7))
                            po, Bpo = psum.get()
                            k.op("pe", lambda e: e.matmul(po[:], lhsT=CTb[:, g, :], rhs=S_bf[:, gs], start=True, stop=True),
                                 reads=[BCTb, BSb], writes=[Bpo])
                            tmp, Btmp = tmpr.get()
                            k.op("dve", lambda e: e.tensor_tensor(
                                out=tmp[:].rearrange("p (h d) -> p h d", h=8),
                                in0=po[:].rearrange("p (h d) -> p h d", h=8), in1=h3(eacum, g), op=ALU.mult),
                                 reads=[Bpo, Bsm[7]], writes=[Btmp])
                            yg, Byg = ygr.get()
                            k.op("dve", lambda e: e.tensor_tensor(out=yg[:], in0=py[:], in1=tmp[:], op=ALU.add),
                                 reads=[Bpy, Btmp], writes=[Byg])
                            k.op("pool", lambda e: e.tensor_tensor(out=yg[:], in0=yg[:], in1=zi[:, gs], op=ALU.mult),
                                 reads=[Byg, Bzi], writes=[Byg])
                            sq, Bsq = sqr.get()
                            ss, Bss = ssr.get()
                            k.op("act", lambda e: e.activation(out=sq[:], in_=yg[:], func=AF.Square, accum_out=ss[:, 0:1]),
                                 reads=[Byg], writes=[Bsq, Bss])
                            k.op("act", lambda e: e.activation(out=ss[:, 1:2], in_=ss[:, 0:1], func=AF.Sqrt,
                                                               bias=EPS, scale=1.0 / 512),
                                 reads=[Bss], writes=[Bss])
                            k.op("dve", lambda e: e.reciprocal(out=ss[:, 1:2], in_=ss[:, 1:2]), reads=[Bss], writes=[Bss])
                            yn, Byn = ynr.get()
                            k.op("dve", lambda e: e.scalar_tensor_tensor(
                                out=yn[:], in0=yg[:], scalar=ss[:, 1:2], in1=gssd[:, gs], op0=ALU.mult, op1=ALU.mult),
                                 reads=[Byg, Bss, Bg], writes=[Byn])
                            ptT, BpT = psum.get()
                            ptb = ptT[:].bitcast(BF16).rearrange("p (a b) -> p a b", a=8)
                            for j in range(4):
                                k.op("pe", lambda e: e.transpose(out=ptb[:, j, :], in_=yn[:, 128 * j:128 * (j + 1)],
                                                                 identity=ident_b[:]),
                                     reads=[Byn, B_c2], writes=[BpT], inc=(j == 3))
                            k.op("act", lambda e: e.copy(out=yT[:, 4 * g:4 * g + 4, :], in_=ptb[:, 0:4, :]),
                                 reads=[BpT], writes=[ByT])
                        pu, Bpu = psum.get()
                        k.op("pe", lambda e: e.matmul(pu[:], lhsT=Btm[:, 128 * g:128 * (g + 1)], rhs=xdtw[:, gs],
                                                      start=True, stop=True),
                             reads=[BBtm, Bxdtw], writes=[Bpu])
                        k.op("pool", lambda e: e.tensor_tensor(
                            out=S[:, gs].rearrange("p (h d) -> p h d", h=8),
                            in0=S[:, gs].rearrange("p (h d) -> p h d", h=8), in1=h3(dA, g), op=ALU.mult),
                             reads=[BS, Bsm[9]], writes=[BS])
                        k.op("dve", lambda e: e.tensor_tensor(out=S[:, gs], in0=S[:, gs], in1=pu[:], op=ALU.add),
                             reads=[BS, Bpu], writes=[BS])

                    nxt = front(0) if own else (None, None)
                    for g in range(NG):
                        cur = nxt
                        if own and g + 1 < NG:
                            nxt = front(g + 1)
                        back(g, *cur)
                    if own:
                        o0 = t0 - TPRE
                        k.dma("sp", yrows[:, :, o0:o0 + 128], yT[:], reads=[ByT], write=B_yT)
                    if t0 + 128 == TPRE:
                        k.op("dve", lambda e: e.tensor_scalar(out=S[:], in0=S[:], scalar1=flg[:, 0:1], scalar2=None,
                                                              op0=ALU.mult), reads=[BS, Bfl], writes=[BS])
                    if need_sbf:
                        k.op("act", lambda e: e.copy(out=S_bf[:], in_=S[:]), reads=[BS], writes=[BSb])

        if "ssd" in phases:
            phase_ssd()

        def phase_scpack():
            k.barrier()
            with ExitStack() as st:
                gsc = sb(st, "p_gsc", [128, 32], F32)
                Bg = k.buf()
                k.dma("sp", gsc[:], I["g_sc"](), write=Bg)
                rs = sb(st, "p_rs", [128, TOWN], F32)
                Brs = k.buf()
                k.dma("sp", rs[:], rstd_sc_d[:, HALO:HALO + TOWN], reads=[B_rstdsc], write=Brs)
                yr = Ring([(sb(st, f"p_y{i}", [128, TOWN], F32), k.buf()) for i in range(2)])
                orr = Ring([(sb(st, f"p_o{i}", [128, TOWN], BF16), k.buf()) for i in range(2)])
                for i in range(32):
                    y, By = yr.get()
                    k.dma("sp", y[:], ysc_d[i * 128:(i + 1) * 128, HALO:HALO + TOWN], reads=[B_ysc], write=By)
                    o, Bo = orr.get()
                    k.op("dve", lambda e: e.scalar_tensor_tensor(out=o[:], in0=y[:], scalar=gsc[:, i:i + 1], in1=rs[:],
                                                                 op0=ALU.mult, op1=ALU.mult),
                         reads=[By, Bg, Brs], writes=[Bo])
                    k.dma("sp", yT_d[D + i * 128:D + (i + 1) * 128, :], o[:], reads=[Bo], write=B_yT)

        if "scpack" in phases:
            phase_scpack()

        def phase_outproj():
            k.barrier()
            with ExitStack() as st:
                yt = sb(st, "o_y", [128, 64, 512], BF16)
                Byt = k.buf()
                wr = Ring([(sb(st, f"o_w{i}", [128, 32, 512], BF16), k.buf()) for i in range(3)])
                xr = Ring([(sb(st, f"o_x{i}", [128, 512], F32), k.buf()) for i in range(3)])
                hr = Ring([(sb(st, f"o_h{i}", [128, 512], F32), k.buf()) for i in range(3)])
                yv = yT_d.rearrange("(c p) t -> p c t", p=128)
                x_all = I["x_all"]()
                for o0 in range(0, TOWN, 512):
                    k.dma("sp", yt[:], yv[:, :, o0:o0 + 512], reads=[B_yT], write=Byt)
                    for s in range(8):
                        accs = [psum.get() for _ in range(4)]
                        for half in range(2):
                            w, Bw = wr.get()
                            k.dma("pool", w[:].rearrange("p c n -> p (c n)"), I["w_out"]()[s, half], write=Bw)
                            for blk in range(4):
                                pt, Bp = accs[blk]
                                for c in range(32):
                                    last = (half == 1 and c == 31)
                                    k.op("pe", lambda e: e.matmul(
                                        pt[:], lhsT=yt[:, half * 32 + c, blk * 128:(blk + 1) * 128], rhs=w[:, c, :],
                                        start=(half == 0 and c == 0), stop=last),
                                         reads=[Byt, Bw], writes=[Bp], inc=(c == 31))
                        for blk in range(4):
                            pt, Bp = accs[blk]
                            r0 = o0 + blk * 128
                            xt, Bx = xr.get()
                            k.dma("sp", xt[:], x_all[TPRE + r0:TPRE + r0 + 128, s * 512:(s + 1) * 512], write=Bx)
                            ht, Bh = hr.get()
                            k.op("dve", lambda e: e.tensor_tensor(out=ht[:], in0=pt[:], in1=xt[:], op=ALU.add),
                                 reads=[Bp, Bx], writes=[Bh])
                            k.dma("sp", h1_d[r0:r0 + 128, s * 512:(s + 1) * 512], ht[:], reads=[Bh], write=B_h1)

        if "outproj" in phases:
            phase_outproj()

        if "norm2" in phases:
            phase_norm(h1_d, TOWN, I["g_ffn"](), xn2T_d, B_xn2T, "n2", src_dep=B_h1)

        def phase_qg():
            k.barrier()
            with ExitStack() as st:
                iota = sb(st, "q_iota", [128, 128], F32)
                Bio = k.buf()
                k.dma("sp", iota[:], I["c_iota"](), write=Bio)
                iota16s = sb(st, "q_iota16s", [128, 16], F32)
                k.op("dve", lambda e: e.tensor_scalar(out=iota16s[:], in0=iota[:, 0:16], scalar1=16.0, scalar2=None,
                                                      op0=ALU.mult), reads=[Bio], writes=[Bio])
                keys = sb(st, "q_keys", [128, 16, 128], F32)
                Bky = k.buf()
                k.dma("sp", keys[:], I["keys_t"]().rearrange("c d n -> d c n"), write=Bky)
                xT = sb(st, "q_xT", [128, KC, 512], BF16)
                BxT = k.buf()
                wr = Ring([(sb(st, f"q_w{i}", [128, KC, 128], BF16), k.buf()) for i in range(2)])
                qT = sb(st, "q_qT", [128, 16, 512], F32)
                BqT = k.buf()
                sc = sb(st, "q_sc", [128, 16, 128], F32)
                sc2 = sb(st, "q_sc2", [128, 16, 128], F32)
                Bsc, Bsc2 = k.buf(), k.buf()
                tops = sb(st, "q_tops", [128, 16, 16], F32)
                topu = sb(st, "q_topu", [128, 16, 16], U32)
                topi = sb(st, "q_topi", [128, 16, 16], F32)
                Btops, Btopu, Btopi = k.buf(), k.buf(), k.buf()
                cand = sc2[:].rearrange("p (h c) n -> p h (c n)", c=2)
                cand2 = sb(st, "q_cand2", [128, 8, 256], F32)
                Bcand, Bcand2 = Bsc2, k.buf()
                bests = sb(st, "q_bests", [128, 8, 16], F32)
                bposu = sb(st, "q_bposu", [128, 8, 16], U32)
                au = sb(st, "q_au", [128, 8, 16], U32)
                bu = sb(st, "q_bu", [128, 8, 16], U32)
                bmod = sb(st, "q_bmod", [128, 8, 16], F32)
                a16 = sb(st, "q_a16", [128, 8, 16], F32)
                Bbests, Bbposu, Bbpos, Bbmod, Ba16 = (k.buf() for _ in range(5))
                oh = cand2[:].rearrange("p h (a b) -> p h a b", a=16)
                Boh = Bcand2
                Iv = sb(st, "q_I", [128, 8, 16], F32)
                Jv = sb(st, "q_J", [128, 8, 16], F32)
                gv = sb(st, "q_g", [128, 8, 16], F32)
                BIv, BJv, Bgv = k.buf(), k.buf(), k.buf()
                gsum = sb(st, "q_gsum", [128, 8], F32)
                Bgsum = k.buf()
                tr = [sb(st, f"q_tr{i}", [128, 128], F32) for i in range(3)]
                Btr = [k.buf() for _ in range(3)]
                Ar = Ring([(sb(st, f"q_A{i}", [128, 16, 128], BF16), k.buf()) for i in range(2)])
                Br = Ring([(sb(st, f"q_B{i}", [128, 16, 128], BF16), k.buf()) for i in range(2)])
                Gr = Ring([(sb(st, f"q_G{i}", [128, 128, 128], BF16), k.buf()) for i in range(2)])
                Gv = G_d.rearrange("i j t -> j i t")

                for o0 in range(0, TOWN, 512):
                    k.dma("sp", xT[:], xn2T_d[:, :, o0:o0 + 512], reads=[B_xn2T], write=BxT)
                    for hc in range(16):
                        w, Bw = wr.get()
                        k.dma("pool", w[:].rearrange("p c n -> p (c n)"), I["w_q"]()[hc], write=Bw)
                        pt, Bp = psum.get()
                        for c in range(KC):
                            k.op("pe", lambda e: e.matmul(pt[:], lhsT=w[:, c, :], rhs=xT[:, c, :],
                                                          start=(c == 0), stop=(c == KC - 1)),
                                 reads=[Bw, BxT], writes=[Bp], inc=(c == KC - 1))
                        k.op("act", lambda e: e.copy(out=qT[:, hc, :], in_=pt[:]), reads=[Bp], writes=[BqT])
                    for blk in range(4):
                        r0 = o0 + blk * 128
                        Gst, BGst = Gr.get()
                        for q4 in range(4):
                            pt, Bp = psum.get()
                            for j in range(4):
                                hc = 4 * q4 + j
                                k.op("pe", lambda e: e.matmul(pt[:, j * 128:(j + 1) * 128],
                                                              lhsT=qT[:, hc, blk * 128:(blk + 1) * 128],
                                                              rhs=keys[:, hc, :], start=True, stop=True),
                                     reads=[BqT, Bky], writes=[Bp], inc=(j == 3))
                            k.op("act", lambda e: e.copy(out=sc[:, 4 * q4:4 * q4 + 4, :].rearrange("p a b -> p (a b)"),
                                                         in_=pt[:]), reads=[Bp], writes=[Bsc])
                        for hc in range(16):
                            k.op("dve", lambda e: e.max(out=tops[:, hc, 0:8], in_=sc[:, hc, :]),
                                 reads=[Bsc], writes=[Btops])
                            k.op("dve", lambda e: e.max_index(out=topu[:, hc, 0:8], in_max=tops[:, hc, 0:8],
                                                              in_values=sc[:, hc, :]),
                                 reads=[Bsc, Btops], writes=[Btopu])
                            k.op("dve", lambda e: e.match_replace(out=sc2[:, hc, :], in_to_replace=tops[:, hc, 0:8],
                                                                  in_values=sc[:, hc, :], imm_value=-1e30),
                                 reads=[Bsc, Btops], writes=[Bsc2])
                            k.op("dve", lambda e: e.max(out=tops[:, hc, 8:16], in_=sc2[:, hc, :]),
                                 reads=[Bsc2], writes=[Btops])
                            k.op("dve", lambda e: e.max_index(out=topu[:, hc, 8:16], in_max=tops[:, hc, 8:16],
                                                              in_values=sc2[:, hc, :]),
                                 reads=[Bsc2, Btops], writes=[Btopu])
                        k.op("dve", lambda e: e.tensor_copy(out=topi[:], in_=topu[:]), reads=[Btopu], writes=[Btopi])
                        t4 = tops[:].rearrange("p (h c) a -> p h c a", c=2)
                        k.op("dve", lambda e: e.tensor_tensor(
                            out=cand.rearrange("p h (a b) -> p h a b", a=16),
                            in0=t4[:, :, 0, :].unsqueeze(3).to_broadcast([128, 8, 16, 16]),
                            in1=t4[:, :, 1, :].unsqueeze(2).to_broadcast([128, 8, 16, 16]), op=ALU.add),
                             reads=[Btops], writes=[Bcand])
                        for h in range(8):
                            k.op("dve", lambda e: e.max(out=bests[:, h, 0:8], in_=cand[:, h, :]),
                                 reads=[Bcand], writes=[Bbests])
                            k.op("dve", lambda e: e.max_index(out=bposu[:, h, 0:8], in_max=bests[:, h, 0:8],
                                                              in_values=cand[:, h, :]),
                                 reads=[Bcand, Bbests], writes=[Bbposu])
                            k.op("dve", lambda e: e.match_replace(out=cand2[:, h, :], in_to_replace=bests[:, h, 0:8],
                                                                  in_values=cand[:, h, :], imm_value=-1e30),
                                 reads=[Bcand, Bbests], writes=[Bcand2])
                            k.op("dve", lambda e: e.max(out=bests[:, h, 8:16], in_=cand2[:, h, :]),
                                 reads=[Bcand2], writes=[Bbests])
                            k.op("dve", lambda e: e.max_index(out=bposu[:, h, 8:16], in_max=bests[:, h, 8:16],
                                                              in_values=cand2[:, h, :]),
                                 reads=[Bcand2, Bbests], writes=[Bbposu])
                        k.op("dve", lambda e: e.tensor_single_scalar(out=au[:], in_=bposu[:], scalar=4,
                                                                     op=ALU.logical_shift_right),
                             reads=[Bbposu], writes=[Bbpos])
                        k.op("dve", lambda e: e.tensor_single_scalar(out=bu[:], in_=bposu[:], scalar=15,
                                                                     op=ALU.bitwise_and),
                             reads=[Bbposu], writes=[Bbmod])
                        k.op("dve", lambda e: e.tensor_copy(out=a16[:], in_=au[:]), reads=[Bbpos], writes=[Ba16])
                        k.op("dve", lambda e: e.tensor_copy(out=bmod[:], in_=bu[:]), reads=[Bbmod], writes=[Bbmod])
                        i4 = topi[:].rearrange("p (h c) a -> p h c a", c=2)
                        io16 = iota[:, 0:16].unsqueeze(1).unsqueeze(1).to_broadcast([128, 8, 16, 16])
                        io16s = iota16s[:].unsqueeze(1).unsqueeze(1).to_broadcast([128, 8, 16, 16])
                        k.op("dve", lambda e: e.tensor_tensor(
                            out=oh, in0=io16, in1=a16[:].unsqueeze(3).to_broadcast([128, 8, 16, 16]),
                            op=ALU.is_equal), reads=[Bio, Ba16], writes=[Boh])
                        k.op("dve", lambda e: e.tensor_tensor(
                            out=oh, in0=oh, in1=i4[:, :, 0, :].unsqueeze(2).to_broadcast([128, 8, 16, 16]),
                            op=ALU.mult), reads=[Boh, Btopi], writes=[Boh])
                        k.op("dve", lambda e: e.tensor_reduce(out=Iv[:], in_=oh, axis=AX.X, op=ALU.add),
                             reads=[Boh], writes=[BIv])
                        k.op("dve", lambda e: e.tensor_tensor(
                            out=oh, in0=io16, in1=bmod[:].unsqueeze(3).to_broadcast([128, 8, 16, 16]),
                            op=ALU.is_equal), reads=[Bio, Bbmod, BIv], writes=[Boh])
                        k.op("dve", lambda e: e.tensor_tensor(
                            out=oh, in0=oh, in1=i4[:, :, 1, :].unsqueeze(2).to_broadcast([128, 8, 16, 16]),
                            op=ALU.mult), reads=[Boh, Btopi], writes=[Boh])
                        k.op("dve", lambda e: e.tensor_reduce(out=Jv[:], in_=oh, axis=AX.X, op=ALU.add),
                             reads=[Boh], writes=[BJv])
                        k.op("dve", lambda e: e.tensor_tensor(
                            out=gv[:], in0=bests[:], in1=bests[:, :, 0:1].to_broadcast([128, 8, 16]), op=ALU.subtract),
                             reads=[Bbests], writes=[Bgv])
                        k.op("act", lambda e: e.activation(out=gv[:], in_=gv[:], func=AF.Exp), reads=[Bgv], writes=[Bgv])
                        k.op("dve", lambda e: e.tensor_reduce(out=gsum[:], in_=gv[:], axis=AX.X, op=ALU.add),
                             reads=[Bgv], writes=[Bgsum])
                        k.op("dve", lambda e: e.reciprocal(out=gsum[:], in_=gsum[:]), reads=[Bgsum], writes=[Bgsum])
                        k.op("dve", lambda e: e.tensor_tensor(
                            out=gv[:], in0=gv[:], in1=gsum[:].unsqueeze(2).to_broadcast([128, 8, 16]), op=ALU.mult),
                             reads=[Bgv, Bgsum], writes=[Bgv])
                        for n_, (src, Bsrc) in enumerate(((Iv, BIv), (Jv, BJv), (gv, Bgv))):
                            pt, Bp = psum.get()
                            k.op("pe", lambda e: e.transpose(out=pt[:, 0:128], in_=src[:].rearrange("p h k -> p (h k)"),
                                                             identity=ident_f[:]),
                                 reads=[Bsrc, B_const], writes=[Bp])
                            k.op("act", lambda e: e.copy(out=tr[n_][:], in_=pt[:, 0:128]), reads=[Bp], writes=[Btr[n_]])
                        IT, JT, gT = tr
                        iob = iota[:].unsqueeze(1).to_broadcast([128, 16, 128])
                        for t16 in range(0, 128, 16):
                            A, BA = Ar.get()
                            Bm, BB = Br.get()
                            k.op("dve", lambda e: e.tensor_tensor(
                                out=A[:], in0=iob, in1=IT[:, t16:t16 + 16].unsqueeze(2).to_broadcast([128, 16, 128]),
                                op=ALU.is_equal), reads=[Bio, Btr[0]], writes=[BA])
                            k.op("dve", lambda e: e.tensor_tensor(
                                out=A[:], in0=A[:], in1=gT[:, t16:t16 + 16].unsqueeze(2).to_broadcast([128, 16, 128]),
                                op=ALU.mult), reads=[BA, Btr[2]], writes=[BA])
                            k.op("dve", lambda e: e.tensor_tensor(
                                out=Bm[:], in0=iob, in1=JT[:, t16:t16 + 16].unsqueeze(2).to_broadcast([128, 16, 128]),
                                op=ALU.is_equal), reads=[Bio, Btr[1]], writes=[BB])
                            for q4 in range(4):
                                pt, Bp = psum.get()
                                for tt in range(4):
                                    k.op("pe", lambda e: e.matmul(pt[:, tt * 128:(tt + 1) * 128], lhsT=Bm[:, 4 * q4 + tt, :],
                                                                  rhs=A[:, 4 * q4 + tt, :], start=True, stop=True),
                                         reads=[BA, BB], writes=[Bp], inc=(tt == 3))
                                tb = t16 + 4 * q4
                                k.op("act", lambda e: e.copy(
                                    out=Gst[:, :, tb:tb + 4].rearrange("p i t -> p t i"),
                                    in_=pt[:].rearrange("p (t i) -> p t i", t=4)), reads=[Bp], writes=[BGst])
                        for i8 in range(8):
                            k.dma("sp", Gv[:, 16 * i8:16 * (i8 + 1), r0:r0 + 128], Gst[:, 16 * i8:16 * (i8 + 1), :],
                                  reads=[BGst], write=B_G)

        if "qg" in phases:
            phase_qg()

        def phase_gemm1():
            k.barrier()
            TB = min(1024, TOWN)
            with ExitStack() as st:
                xT = sb(st, "u_xT", [128, KC, TB], BF16)
                BxT = k.buf()
                wr = Ring([(sb(st, f"u_w{i}", [128, KC, 128], BF16), k.buf()) for i in range(3)])
                hr = Ring([(sb(st, f"u_h{i}", [128, TB], F32), k.buf()) for i in range(2)])
                gr = Ring([(sb(st, f"u_g{i}", [128, TB], BF16), k.buf()) for i in range(3)])
                orr = Ring([(sb(st, f"u_o{i}", [128, TB], BF16), k.buf()) for i in range(3)])
                for o0 in range(0, TOWN, TB):
                    k.dma("sp", xT[:], xn2T_d[:, :, o0:o0 + TB], reads=[B_xn2T], write=BxT)
                    for i in range(128):
                        w, Bw = wr.get()
                        k.dma("pool", w[:].rearrange("p c n -> p (c n)"), I["u_t"]()[i], write=Bw)
                        g, Bg = gr.get()
                        k.dma("sp", g[:], G_d[i, :, o0:o0 + TB], reads=[B_G], write=Bg)
                        hsb, Bh = hr.get()
                        for n0 in range(0, TB, 512):
                            pt, Bp = psum.get()
                            for c in range(KC):
                                k.op("pe", lambda e: e.matmul(pt[:], lhsT=w[:, c, :], rhs=xT[:, c, n0:n0 + 512],
                                                              start=(c == 0), stop=(c == KC - 1)),
                                     reads=[Bw, BxT], writes=[Bp], inc=(c == KC - 1))
                            k.op("act", lambda e: e.activation(out=hsb[:, n0:n0 + 512], in_=pt[:], func=AF.Gelu),
                                 reads=[Bp], writes=[Bh])
                        o, Bo = orr.get()
                        k.op("dve", lambda e: e.tensor_tensor(out=o[:], in0=hsb[:], in1=g[:], op=ALU.mult),
                             reads=[Bh, Bg], writes=[Bo])
                        k.dma("sp", HG_d[i * 128:(i + 1) * 128, o0:o0 + TB], o[:], reads=[Bo], write=B_HG)

        if "gemm1" in phases:
            phase_gemm1()

        def phase_gemm2():
            k.barrier()
            TB = min(1024, TOWN)
            nb = TB // 128
            with ExitStack() as st:
                hgr = Ring([(sb(st, f"v_hg{i}", [128, TB], BF16), k.buf()) for i in range(4)])
                vr = Ring([(sb(st, f"v_v{i}", [128, 512], BF16), k.buf()) for i in range(4)])
                xr = Ring([(sb(st, f"v_x{i}", [128, 512], F32), k.buf()) for i in range(3)])
                orr = Ring([(sb(st, f"v_o{i}", [128, 512], F32), k.buf()) for i in range(3)])
                for o0 in range(0, TOWN, TB):
                    for s in range(8):
                        accs = [psum.get() for _ in range(nb)]
                        for i in range(128):
                            hg, Bhg = hgr.get()
                            k.dma("sp", hg[:], HG_d[i * 128:(i + 1) * 128, o0:o0 + TB], reads=[B_HG], write=Bhg)
                            v, Bv = vr.get()
                            k.dma("sp", v[:], vb_d[i * 128:(i + 1) * 128, s * 512:(s + 1) * 512], reads=[B_vb], write=Bv)
                            for blk in range(nb):
                                pt, Bp = accs[blk]
                                k.op("pe", lambda e: e.matmul(pt[:], lhsT=hg[:, blk * 128:(blk + 1) * 128], rhs=v[:],
                                                              start=(i == 0), stop=(i == 127)),
                                     reads=[Bhg, Bv], writes=[Bp], inc=(blk == nb - 1))
                        for blk in range(nb):
                            pt, Bp = accs[blk]
                            r0 = o0 + blk * 128
                            xt, Bx = xr.get()
                            k.dma("sp", xt[:], h1_d[r0:r0 + 128, s * 512:(s + 1) * 512], reads=[B_h1], write=Bx)
                            ot, Bo = orr.get()
                            k.op("dve", lambda e: e.tensor_tensor(out=ot[:], in0=pt[:], in1=xt[:], op=ALU.add),
                                 reads=[Bp, Bx], writes=[Bo])
                            k.dma("sp", h2_d[r0:r0 + 128, s * 512:(s + 1) * 512], ot[:], reads=[Bo], write=B_h2)

        if "gemm2" in phases:
            phase_gemm2()

        def phase_final():
            k.barrier()
            with ExitStack() as st:
                gain = sb(st, "f_gain", [128, D], F32)
                Bg = k.buf()
                k.dma("sp", gain[:], bcast_row(I["g_fin"](), D), write=Bg)
                xr = Ring([(sb(st, f"f_x{i}", [128, D], F32), k.buf()) for i in range(2)])
                sq = sb(st, "f_sq", [128, D], F32)
                Bsq = k.buf()
                orr = Ring([(sb(st, f"f_o{i}", [128, D], F32), k.buf()) for i in range(2)])
                smr = Ring([(sb(st, f"f_sm{i}", [128, 2], F32), k.buf()) for i in range(2)])
                for r0 in range(0, TOWN, 128):
                    xt, Bx = xr.get()
                    k.dma("sp", xt[:], h2_d[r0:r0 + 128, :], reads=[B_h2], write=Bx)
                    sm, Bsm = smr.get()
                    k.op("act", lambda e: e.activation(out=sq[:], in_=xt[:], func=AF.Square, accum_out=sm[:, 0:1]),
                         reads=[Bx], writes=[Bsq, Bsm])
                    k.op("act", lambda e: e.activation(out=sm[:, 1:2], in_=sm[:, 0:1], func=AF.Sqrt, bias=EPS,
                                                       scale=1.0 / D), reads=[Bsm], writes=[Bsm])
                    k.op("dve", lambda e: e.reciprocal(out=sm[:, 1:2], in_=sm[:, 1:2]), reads=[Bsm], writes=[Bsm])
                    ot, Bo = orr.get()
                    k.op("dve", lambda e: e.scalar_tensor_tensor(out=ot[:], in0=xt[:], scalar=sm[:, 1:2], in1=gain[:],
                                                                 op0=ALU.mult, op1=ALU.mult),
                         reads=[Bx, Bsm, Bg], writes=[Bo])
                    k.dma("sp", out[r0:r0 + 128, :], ot[:], reads=[Bo], write=B_out, is_output=True)

        if "final" in phases:
            phase_final()

        fin = list(k.out_tokens)
        for b in (B_xnT, B_xbcT, B_ysc, B_rstdsc, B_zs, B_dtr, B_out, B_yT, B_h1, B_xn2T, B_G, B_HG, B_h2, B_vb):
            fin += b.w
        k._wait("sp", fin)
    return nc


def prep_weights(inp):
    W = np.asarray(inp["w_in"])[0]
    o_z, o_x, o_B, o_C, o_dt, o_b, o_c, o_h = 0, 4096, 8192, 9216, 10240, 10304, 14400, 18496
    cols = np.concatenate([np.arange(o_x, o_x + 4096), np.arange(o_B, o_B + 1024), np.arange(o_C, o_C + 1024),
                           np.arange(o_b, o_b + 4096), np.arange(o_c, o_c + 4096), np.arange(o_h, o_h + 4096)])
    wf = W[:, cols]
    w_fm = np.ascontiguousarray(wf.reshape(KC, 128, 144, 128).transpose(2, 1, 0, 3)).reshape(144, 128, KC * 128)
    w_z = np.ascontiguousarray(W[:, 0:4096].reshape(KC, 128, 8, 512).transpose(2, 1, 0, 3)).reshape(8, 128, KC * 512)
    w_dt = np.ascontiguousarray(W[:, o_dt:o_dt + 64].reshape(KC, 128, 64).transpose(1, 0, 2)).reshape(128, KC * 64)
    cw = np.asarray(inp["ssd_conv_w"])[0]
    cw_xbc = np.ascontiguousarray(cw.reshape(4, 48, 128).transpose(2, 1, 0))
    cb_xbc = np.ascontiguousarray(np.asarray(inp["ssd_conv_b"])[0].reshape(48, 128).T)
    cs = np.asarray(inp["sc_conv_w"])[0]
    cw_sc = np.ascontiguousarray(cs.reshape(3, 32, 128).transpose(2, 1, 0))
    g_sc = np.ascontiguousarray(np.asarray(inp["sc_norm_w"])[0].reshape(32, 128).T)
    Wo = np.asarray(inp["w_out"])[0]
    w_out = np.ascontiguousarray(Wo.reshape(2, 32, 128, 8, 512).transpose(3, 0, 2, 1, 4)).reshape(8, 2, 128, 32 * 512)
    Wq = np.asarray(inp["peer_w_query"])[0]
    w_q = np.ascontiguousarray(Wq.reshape(KC, 128, 16, 128).transpose(2, 1, 0, 3)).reshape(16, 128, KC * 128)
    keys = np.asarray(inp["peer_sub_keys"])[0]
    keys_t = np.ascontiguousarray(keys.reshape(16, 128, 128).transpose(0, 2, 1))
    U = np.asarray(inp["peer_u"])[0]
    u_t = np.ascontiguousarray(U.reshape(128, 128, KC, 128).transpose(0, 3, 2, 1)).reshape(128, 128, KC * 128)
    V = np.asarray(inp["peer_v"])[0]
    u = np.arange(128)
    triu = (u[:, None] <= u[None, :]).astype(np.float32)
    negm = np.where(u[:, None] > u[None, :], np.float32(-30000.0), np.float32(0.0)).astype(np.float32)
    return dict(
        w_fm=w_fm, w_z=w_z, w_dt=w_dt, cw_xbc=cw_xbc, cb_xbc=cb_xbc, cw_sc=cw_sc, g_sc=g_sc,
        g_mix=np.asarray(inp["norm_mix_w"])[0], g_ffn=np.asarray(inp["norm_ffn_w"])[0],
        g_fin=np.asarray(inp["norm_final_w"]), g_ssd=np.asarray(inp["ssd_norm_w"])[0],
        dt_bias=np.asarray(inp["ssd_dt_bias"])[0], a_log=np.asarray(inp["ssd_a_log"])[0],
        d_skip=np.asarray(inp["ssd_d"])[0], w_out=w_out, w_q=w_q, keys_t=keys_t, u_t=u_t, v_nat=V,
        c_ident=np.eye(128, dtype=np.float32), c_triu=triu, c_negm=np.tile(negm, (1, 4)),
        c_iota=np.tile(np.arange(128, dtype=np.float32), (128, 1)),
    )


_NC_CACHE = {}


def kernel(**inputs):
    n_cores = 8
    TPRE = TOWN = 2048
    wts = prep_weights(inputs)
    x = np.asarray(inputs["x"])
    if "nc" not in _NC_CACHE:
        _NC_CACHE["nc"] = build(TPRE, TOWN)
    nc = _NC_CACHE["nc"]
    zeros = np.zeros((TPRE, D), np.float32)
    in_maps = []
    for c in range(n_cores):
        b, half = c // 2, c % 2
        own = x[b, half * TOWN:(half + 1) * TOWN]
        pre = x[b, 0:TPRE] if half == 1 else zeros
        m = dict(wts)
        m["x_all"] = np.ascontiguousarray(np.concatenate([pre, own], axis=0))
        m["flag"] = np.full((128, 1), float(half), np.float32)
        in_maps.append(m)
    res = run_bass_kernel_spmd(nc, in_maps, core_ids=list(range(n_cores)))
    out = np.empty((4, 2 * TOWN, D), np.float32)
    for c in range(n_cores):
        b, half = c // 2, c % 2
        out[b, half * TOWN:(half + 1) * TOWN] = res.results[c]["out"]
    return out
```

```python
import numpy as np
from contextlib import ExitStack
import concourse.bass as bass
import concourse.mybir as mybir
from concourse.bass_utils import run_bass_kernel_spmd

F32 = mybir.dt.float32
BF16 = mybir.dt.bfloat16
U32 = mybir.dt.uint32
AF = mybir.ActivationFunctionType
ALU = mybir.AluOpType
AX = mybir.AxisListType

D = 4096
KC = D // 128
NH = 64
NG = 8
NFM = 144
HALO = 64
EPS = 1e-6
NEG = -30000.0


class Buf:
    __slots__ = ("name", "w", "r", "dsem", "dcnt", "multi")

    def __init__(self, name, multi=False):
        self.name = name
        self.w = []
        self.r = []
        self.dsem = None
        self.dcnt = 0
        self.multi = multi


class K:
    def __init__(self, nc, stack):
        self.nc = nc
        self.stack = stack
        self.E = {"pe": nc.tensor, "act": nc.scalar, "dve": nc.vector,
                  "pool": nc.gpsimd, "sp": nc.sync}
        self.sem = {}
        self.cnt = {}
        self.seen = {}
        for e in self.E:
            self.sem[e] = stack.enter_context(nc.semaphore("s_" + e))
            self.cnt[e] = 0
            self.seen[e] = {}
        self.nsem = len(self.E)
        self.out_tokens = []
        self.nbuf = 0
        self.bufs = []

    def buf(self, name=None, multi=False):
        self.nbuf += 1
        b = Buf(name or f"b{self.nbuf}", multi)
        self.bufs.append(b)
        return b

    def barrier(self):
        toks = [(self.sem[e], self.cnt[e]) for e in self.E if self.cnt[e] > 0]
        for b in self.bufs:
            toks += b.w
            toks += b.r
        toks = self._compress(toks)
        for e in self.E:
            self._wait(e, toks)
        self.bufs = [b for b in self.bufs if b.multi or b.name.startswith("const") or b.name.startswith("bank")]

    def _wait(self, e, toks, hazard=()):
        own = self.sem.get(e)
        best = {}
        for (s, v) in toks:
            if e == "pe" and s is own:
                continue
            k = id(s)
            if k not in best or best[k][1] < v:
                best[k] = (s, v)
        for (s, v) in hazard:
            if s is own:
                continue
            k = id(s)
            if k not in best or best[k][1] < v:
                best[k] = (s, v)
        seen = self.seen[e]
        for k, (s, v) in best.items():
            if seen.get(k, 0) >= v:
                continue
            self.E[e].wait_ge(s, v)
            seen[k] = v

    @staticmethod
    def _compress(toks):
        best = {}
        for (s, v) in toks:
            k = id(s)
            if k not in best or best[k][1] < v:
                best[k] = (s, v)
        return list(best.values())

    def _deps(self, reads, writes):
        raw, haz = [], []
        for b in reads:
            raw += b.w
        for b in writes:
            haz += b.w
            haz += b.r
        return raw, haz

    def op(self, e, fn, reads=(), writes=(), inc=True):
        raw, haz = self._deps(reads, writes)
        self._wait(e, raw, haz)
        ins = fn(self.E[e])
        tok = (self.sem[e], self.cnt[e] + 1)
        if inc:
            ins.then_inc(self.sem[e], 1)
            self.cnt[e] += 1
        for b in reads:
            b.r.append(tok)
            if len(b.r) > 16:
                b.r = self._compress(b.r)
        for b in writes:
            b.w = [tok]
            b.r = []
        return ins

    def _dsem(self, b):
        if b.dsem is None:
            b.dsem = self.stack.enter_context(self.nc.semaphore(f"d{self.nsem}"))
            b.dcnt = 0
            self.nsem += 1
        return b.dsem

    def dma(self, q, out, in_, reads=(), write=None, is_output=False, **kw):
        raw, haz = self._deps(reads, [] if write.multi else [write])
        if write.multi:
            haz = haz + write.r
        self._wait(q, raw, haz)
        s = self._dsem(write)
        write.dcnt += 16
        ins = self.E[q].dma_start(out=out, in_=in_, **kw)
        ins.then_inc(s, 16)
        tok = (s, write.dcnt)
        for b in reads:
            b.r.append(tok)
            if len(b.r) > 16:
                b.r = self._compress(b.r)
        write.w = [tok]
        if not write.multi:
            write.r = []
        if is_output:
            self.out_tokens.append(tok)
        return ins

    def finish(self):
        self._wait("sp", self.out_tokens)


class Ring:
    def __init__(self, items):
        self.items = items
        self.i = 0

    def get(self):
        it = self.items[self.i % len(self.items)]
        self.i += 1
        return it


def split_tiles(lo, hi, tmax, smax=512):
    n = hi - lo
    nt = (n + tmax - 1) // tmax
    base = (n + nt - 1) // nt
    base = ((base + 31) // 32) * 32
    tiles = []
    p = lo
    while p < hi:
        ln = min(base, hi - p)
        subs = []
        q = 0
        while q < ln:
            sl = min(smax, ln - q)
            subs.append((q, sl))
            q += sl
        tiles.append((p, ln, subs))
        p += ln
    return tiles


def build(TPRE, TOWN, debug=False, phases=None, scratch_in=()):
    TALL = TPRE + TOWN
    nc = bass.Bass("TRN2", target_bir_lowering=False)
    ALLP = ["norm1", "inproj", "zdt", "ssd", "scpack", "outproj", "norm2", "qg", "gemm1", "gemm2", "final"]
    phases = set(ALLP) if phases is None else set(phases)

    _din = {}

    def din(name, shape, dt=F32):
        if name not in _din:
            _din[name] = nc.dram_tensor(name, list(shape), dt, kind="ExternalInput").ap()
        return _din[name]

    def dscr(name, shape, dt=F32):
        if name in scratch_in:
            kind = "ExternalInput"
        else:
            kind = "ExternalOutput" if debug else "Internal"
        return nc.dram_tensor(name, list(shape), dt, kind=kind).ap()

    class _Lazy:
        def __init__(self, name, shape, dt=F32):
            self.a = (name, shape, dt)

        def __call__(self):
            return din(*self.a)

    I = dict(
        x_all=_Lazy("x_all", [TALL, D]), flag=_Lazy("flag", [128, 1]),
        w_fm=_Lazy("w_fm", [NFM, 128, KC * 128]), w_z=_Lazy("w_z", [8, 128, KC * 512]),
        w_dt=_Lazy("w_dt", [128, KC * 64]), cw_xbc=_Lazy("cw_xbc", [128, 48, 4]),
        cb_xbc=_Lazy("cb_xbc", [128, 48]), cw_sc=_Lazy("cw_sc", [128, 32, 3]),
        g_mix=_Lazy("g_mix", [D]), g_ffn=_Lazy("g_ffn", [D]), g_fin=_Lazy("g_fin", [D]),
        g_ssd=_Lazy("g_ssd", [D]), g_sc=_Lazy("g_sc", [128, 32]),
        dt_bias=_Lazy("dt_bias", [NH]), a_log=_Lazy("a_log", [NH]), d_skip=_Lazy("d_skip", [NH]),
        w_out=_Lazy("w_out", [8, 2, 128, 32 * 512]), w_q=_Lazy("w_q", [16, 128, KC * 128]),
        keys_t=_Lazy("keys_t", [16, 128, 128]), u_t=_Lazy("u_t", [128, 128, KC * 128]),
        v_nat=_Lazy("v_nat", [128 * 128, D]), c_ident=_Lazy("c_ident", [128, 128]),
        c_triu=_Lazy("c_triu", [128, 128]), c_negm=_Lazy("c_negm", [128, 512]),
        c_iota=_Lazy("c_iota", [128, 128]),
    )

    out = nc.dram_tensor("out", [TOWN, D], F32, kind="ExternalOutput").ap()

    xnT_d = dscr("xnT_d", [128, KC, TALL], BF16)
    xbcT_d = dscr("xbcT_d", [48 * 128, TALL])
    ysc_d = dscr("ysc_d", [32 * 128, TOWN + HALO])
    rstd_sc_d = dscr("rstd_sc_d", [128, TOWN + HALO])
    zs_d = dscr("zs_d", [TOWN, D])
    dtr_d = dscr("dtr_d", [TALL, NH])
    yT_d = dscr("yT_d", [2 * D, TOWN], BF16)
    h1_d = dscr("h1_d", [TOWN, D])
    xn2T_d = dscr("xn2T_d", [128, KC, TOWN], BF16)
    G_d = dscr("G_d", [128, 128, TOWN], BF16)
    HG_d = dscr("HG_d", [128 * 128, TOWN], BF16)
    h2_d = dscr("h2_d", [TOWN, D])
    vb_d = dscr("vb_d", [128 * 128, D], BF16)

    with ExitStack() as top:
        k = K(nc, top)
        B_xnT = k.buf("xnT_d", multi=True)
        B_xbcT = k.buf("xbcT_d", multi=True)
        B_ysc = k.buf("ysc_d", multi=True)
        B_rstdsc = k.buf("rstd_sc_d", multi=True)
        B_zs = k.buf("zs_d", multi=True)
        B_dtr = k.buf("dtr_d", multi=True)
        B_out = k.buf("out", multi=True)
        B_yT = k.buf("yT_d", multi=True)
        B_h1 = k.buf("h1_d", multi=True)
        B_xn2T = k.buf("xn2T_d", multi=True)
        B_G = k.buf("G_d", multi=True)
        B_HG = k.buf("HG_d", multi=True)
        B_h2 = k.buf("h2_d", multi=True)
        B_vb = k.buf("vb_d", multi=True)

        def bcast_row(vec_ap, n):
            return bass.AP(vec_ap.tensor, vec_ap.offset, [[0, 128], [1, n]])

        def sb(st, name, shape, dt):
            return st.enter_context(nc.sbuf_tensor(name, list(shape), dt))

        ident_f = sb(top, "ident_f", [128, 128], F32)
        ident_b = sb(top, "ident_b", [128, 128], BF16)
        ones_f = sb(top, "ones_f", [128, 128], F32)
        B_const = k.buf("const")
        k.dma("sp", ident_f[:], I["c_ident"](), write=B_const)
        B_c2 = k.buf("const2")
        k.dma("pool", ident_b[:], I["c_ident"](), write=B_c2)
        B_c3 = k.buf("const3")
        k.op("dve", lambda e: e.memset(ones_f[:], 1.0), writes=[B_c3])
        CONST = [B_const, B_c2, B_c3]

        vb_pieces = [r_ for r_ in range(0, 128 * 128, 256)] if "gemm2" in phases else []

        def vb_piece():
            if vb_pieces:
                r_ = vb_pieces.pop(0)
                k.dma("pool", vb_d[r_:r_ + 256, :], I["v_nat"]()[r_:r_ + 256, :], write=B_vb)

        if "inproj" not in phases:
            while vb_pieces:
                vb_piece()

        banks = []
        for i in range(8):
            t = top.enter_context(nc.psum_tensor(f"bank{i}", [128, 512], F32))
            banks.append((t, k.buf(f"bank{i}")))
        psum = Ring(banks)

        def phase_norm(src_d, nrows, gain_d, dstT_d, B_dst, tag, src_dep=None):
            k.barrier()
            with ExitStack() as st:
                gain = sb(st, tag + "gain", [128, D], F32)
                Bg = k.buf()
                k.dma("sp", gain[:], bcast_row(gain_d, D), write=Bg)
                xr = Ring([(sb(st, f"{tag}x{i}", [128, D], F32), k.buf()) for i in range(3)])
                sqr = Ring([(sb(st, f"{tag}sq{i}", [128, D], F32), k.buf()) for i in range(1)])
                xnr = Ring([(sb(st, f"{tag}xn{i}", [128, D], BF16), k.buf()) for i in range(3)])
                str_ = Ring([(sb(st, f"{tag}st{i}", [128, KC, 512], BF16), k.buf()) for i in range(2)])
                smr = Ring([(sb(st, f"{tag}sm{i}", [128, 2], F32), k.buf()) for i in range(3)])
                for t0 in range(0, nrows, 512):
                    stg, Bst = str_.get()
                    for bi in range(4):
                        r0 = t0 + bi * 128
                        xt, Bx = xr.get()
                        k.dma("sp", xt[:], src_d[r0:r0 + 128, :], reads=([src_dep] if src_dep is not None else []), write=Bx)
                        sq, Bsq = sqr.get()
                        sm, Bsm = smr.get()
                        k.op("act", lambda e: e.activation(out=sq[:], in_=xt[:], func=AF.Square,
                                                           accum_out=sm[:, 0:1]),
                             reads=[Bx], writes=[Bsq, Bsm])
                        k.op("act", lambda e: e.activation(out=sm[:, 1:2], in_=sm[:, 0:1], func=AF.Sqrt,
                                                           bias=EPS, scale=1.0 / D),
                             reads=[Bsm], writes=[Bsm])
                        k.op("dve", lambda e: e.reciprocal(out=sm[:, 1:2], in_=sm[:, 1:2]),
                             reads=[Bsm], writes=[Bsm])
                        xn, Bxn = xnr.get()
                        k.op("dve", lambda e: e.scalar_tensor_tensor(
                            out=xn[:], in0=xt[:], scalar=sm[:, 1:2], in1=gain[:],
                            op0=ALU.mult, op1=ALU.mult), reads=[Bx, Bsm, Bg], writes=[Bxn])
                        for grp in range(4):
                            pt, Bp = psum.get()
                            ptb = pt[:].bitcast(BF16).rearrange("p (a b) -> p a b", a=8)
                            for j in range(8):
                                c = grp * 8 + j
                                k.op("pe", lambda e: e.transpose(out=ptb[:, j, :],
                                                                 in_=xn[:, c * 128:(c + 1) * 128],
                                                                 identity=ident_b[:]),
                                     reads=[Bxn, B_c2], writes=[Bp], inc=(j == 7))
                            k.op("act", lambda e: e.copy(
                                out=stg[:, grp * 8:(grp + 1) * 8, bi * 128:(bi + 1) * 128], in_=ptb),
                                 reads=[Bp], writes=[Bst])
                    k.dma("sp", dstT_d[:, :, t0:t0 + 512], stg[:], reads=[Bst], write=B_dst)

        if "norm1" in phases:
            phase_norm(I["x_all"](), TALL, I["g_mix"](), xnT_d, B_xnT, "n1")

        def phase_inproj_fm():
            k.barrier()
            with ExitStack() as st:
                TMAX = 1056
                cwx = sb(st, "cwx", [128, 48, 4], F32)
                cbx = sb(st, "cbx", [128, 48], F32)
                cws = sb(st, "cws", [128, 32, 3], F32)
                Bcw = k.buf()
                k.dma("sp", cwx[:], I["cw_xbc"](), write=Bcw)
                Bcb = k.buf()
                k.dma("sp", cbx[:], I["cb_xbc"](), write=Bcb)
                Bcs = k.buf()
                k.dma("sp", cws[:], I["cw_sc"](), write=Bcs)
                hs = sb(st, "hs", [128, 112, 4], F32)
                Bhs = k.buf()
                k.op("dve", lambda e: e.memset(hs[:], 0.0), writes=[Bhs])
                xT = sb(st, "ipxT", [128, KC, TMAX], BF16)
                BxT = k.buf()
                wr = Ring([(sb(st, f"ipw{i}", [128, KC, 128], BF16), k.buf()) for i in range(3)])
                pr = Ring([(sb(st, f"ipP{i}", [128, 4 + TMAX], F32), k.buf()) for i in range(2)])
                ar = Ring([(sb(st, f"ipA{i}", [128, TMAX], F32), k.buf()) for i in range(2)])
                orr = Ring([(sb(st, f"ipO{i}", [128, TMAX], F32), k.buf()) for i in range(2)])
                csr = Ring([(sb(st, f"ipC{i}", [128, TMAX], F32), k.buf()) for i in range(1)])
                ssq = sb(st, "ipssq", [128, TMAX], F32)
                Bssq = k.buf()
                sqt = sb(st, "ipsqt", [128, TMAX], F32)
                Bsqt = k.buf()

                ncall = [0]

                def gemm_chunk(j, ln, subs):
                    wt, Bw = wr.get()
                    k.dma("pool", wt[:].rearrange("p c n -> p (c n)"), I["w_fm"]()[j], write=Bw)
                    ncall[0] += 1
                    if ncall[0] % 4 == 0:
                        vb_piece()
                    res = []
                    for (so, sl) in subs:
                        pt, Bp = psum.get()
                        for c in range(KC):
                            k.op("pe", lambda e: e.matmul(pt[:, 0:sl], lhsT=wt[:, c, :],
                                                          rhs=xT[:, c, so:so + sl],
                                                          start=(c == 0), stop=(c == KC - 1)),
                                 reads=[Bw, BxT], writes=[Bp], inc=(c == KC - 1))
                        res.append((pt, Bp, so, sl))
                    return res

                def conv(P, BP, A, BA, ln, taps, wtile, Bwt, ci, eng):
                    base = 4 - (taps - 1)
                    k.op(eng, lambda e: e.tensor_scalar(out=A[:, 0:ln], in0=P[:, base:base + ln],
                                                        scalar1=wtile[:, ci, 0:1], scalar2=None,
                                                        op0=ALU.mult),
                         reads=[BP, Bwt], writes=[BA])
                    for t in range(1, taps):
                        k.op(eng, lambda e: e.scalar_tensor_tensor(
                            out=A[:, 0:ln], in0=P[:, base + t:base + t + ln], scalar=wtile[:, ci, t:t + 1],
                            in1=A[:, 0:ln], op0=ALU.mult, op1=ALU.add),
                             reads=[BP, Bwt, BA], writes=[BA])

                def run_tile(t0, ln, subs, chunks, own):
                    k.dma("sp", xT[:, :, 0:ln], xnT_d[:, :, t0:t0 + ln], reads=[B_xnT], write=BxT)
                    for j in chunks:
                        res = gemm_chunk(j, ln, subs)
                        P, BP = pr.get()
                        k.op("dve", lambda e: e.tensor_copy(out=P[:, 0:4], in_=hs[:, j, :]),
                             reads=[Bhs], writes=[BP])
                        for (pt, Bp, so, sl) in res:
                            k.op("act", lambda e: e.copy(out=P[:, 4 + so:4 + so + sl], in_=pt[:, 0:sl]),
                                 reads=[Bp], writes=[BP])
                        k.op("dve", lambda e: e.tensor_copy(out=hs[:, j, :], in_=P[:, ln:ln + 4]),
                             reads=[BP], writes=[Bhs])
                        A, BA = ar.get()
                        conv(P, BP, A, BA, ln, 4, cwx, Bcw, j, "dve")
                        O, BO = orr.get()
                        k.op("act", lambda e: e.activation(out=O[:, 0:ln], in_=A[:, 0:ln], func=AF.Silu,
                                                           bias=cbx[:, j:j + 1], scale=1.0),
                             reads=[BA, Bcb], writes=[BO])
                        k.dma("sp", xbcT_d[j * 128:(j + 1) * 128, t0:t0 + ln], O[:, 0:ln],
                              reads=[BO], write=B_xbcT)
                    if not own:
                        return
                    o0 = t0 - (TPRE - HALO)
                    for i in range(32):
                        res_c = gemm_chunk(48 + 32 + i, ln, subs)
                        Cs, BCs = csr.get()
                        for (pt, Bp, so, sl) in res_c:
                            k.op("act", lambda e: e.copy(out=Cs[:, so:so + sl], in_=pt[:, 0:sl]),
                                 reads=[Bp], writes=[BCs])
                        res_h = gemm_chunk(48 + 64 + i, ln, subs)
                        P, BP = pr.get()
                        k.op("dve", lambda e: e.tensor_copy(out=P[:, 0:4], in_=hs[:, 48 + i, :]),
                             reads=[Bhs], writes=[BP])
                        for (pt, Bp, so, sl) in res_h:
                            k.op("dve", lambda e: e.tensor_tensor(out=P[:, 4 + so:4 + so + sl],
                                                                  in0=pt[:, 0:sl], in1=Cs[:, so:so + sl],
                                                                  op=ALU.mult),
                                 reads=[Bp, BCs], writes=[BP])
                        k.op("dve", lambda e: e.tensor_copy(out=hs[:, 48 + i, :], in_=P[:, ln:ln + 4]),
                             reads=[BP], writes=[Bhs])
                        A, BA = ar.get()
                        conv(P, BP, A, BA, ln, 3, cws, Bcs, i, "dve")
                        res_b = gemm_chunk(48 + i, ln, subs)
                        O, BO = orr.get()
                        for (pt, Bp, so, sl) in res_b:
                            k.op("dve", lambda e: e.tensor_tensor(out=O[:, so:so + sl], in0=pt[:, 0:sl],
                                                                  in1=A[:, so:so + sl], op=ALU.mult),
                                 reads=[Bp, BA], writes=[BO])
                        k.dma("sp", ysc_d[i * 128:(i + 1) * 128, o0:o0 + ln], O[:, 0:ln],
                              reads=[BO], write=B_ysc)
                        if i == 0:
                            k.op("act", lambda e: e.activation(out=ssq[:, 0:ln], in_=O[:, 0:ln],
                                                               func=AF.Square),
                                 reads=[BO], writes=[Bssq])
                        else:
                            k.op("act", lambda e: e.activation(out=sqt[:, 0:ln], in_=O[:, 0:ln],
                                                               func=AF.Square),
                                 reads=[BO], writes=[Bsqt])
                            k.op("pool", lambda e: e.tensor_tensor(out=ssq[:, 0:ln], in0=ssq[:, 0:ln],
                                                                   in1=sqt[:, 0:ln], op=ALU.add),
                                 reads=[Bsqt, Bssq], writes=[Bssq])
                    O, BO = orr.get()
                    for (so, sl) in subs:
                        pt, Bp = psum.get()
                        k.op("pe", lambda e: e.matmul(pt[:, 0:sl], lhsT=ones_f[:], rhs=ssq[:, so:so + sl],
                                                      start=True, stop=True),
                             reads=[Bssq, B_c3], writes=[Bp])
                        k.op("act", lambda e: e.activation(out=O[:, so:so + sl], in_=pt[:, 0:sl],
                                                           func=AF.Sqrt, bias=EPS, scale=1.0 / D),
                             reads=[Bp], writes=[BO])
                    k.op("dve", lambda e: e.reciprocal(out=O[:, 0:ln], in_=O[:, 0:ln]),
                         reads=[BO], writes=[BO])
                    k.dma("sp", rstd_sc_d[:, o0:o0 + ln], O[:, 0:ln], reads=[BO], write=B_rstdsc)

                if TPRE > HALO:
                    for (t0, ln, subs) in split_tiles(0, TPRE - HALO, TMAX):
                        run_tile(t0, ln, subs, list(range(0, 40)), False)
                for (t0, ln, subs) in split_tiles(TPRE - HALO, TALL, TMAX):
                    run_tile(t0, ln, subs, list(range(0, 48)), True)

        if "inproj" in phases:
            phase_inproj_fm()
        while vb_pieces:
            vb_piece()

        def phase_zdt():
            k.barrier()
            with ExitStack() as st:
                wdt = sb(st, "zwdt", [128, KC, 64], BF16)
                Bwdt = k.buf()
                k.dma("pool", wdt[:].rearrange("p c n -> p (c n)"), I["w_dt"](), write=Bwdt)
                xr = Ring([(sb(st, f"zx{i}", [128, KC, 512], BF16), k.buf()) for i in range(2)])
                wr = Ring([(sb(st, f"zw{i}", [128, KC, 512], BF16), k.buf()) for i in range(2)])
                orr = Ring([(sb(st, f"zo{i}", [128, 512], F32), k.buf()) for i in range(3)])
                dr = Ring([(sb(st, f"zd{i}", [128, 64], F32), k.buf()) for i in range(2)])
                for t0 in range(0, TALL, 512):
                    xT, BxT = xr.get()
                    k.dma("sp", xT[:], xnT_d[:, :, t0:t0 + 512], reads=[B_xnT], write=BxT)
                    for blk in range(4):
                        pt, Bp = psum.get()
                        for c in range(KC):
                            k.op("pe", lambda e: e.matmul(pt[:, 0:64], lhsT=xT[:, c, blk * 128:(blk + 1) * 128],
                                                          rhs=wdt[:, c, :], start=(c == 0), stop=(c == KC - 1)),
                                 reads=[BxT, Bwdt], writes=[Bp], inc=(c == KC - 1))
                        dd, Bd = dr.get()
                        k.op("act", lambda e: e.copy(out=dd[:], in_=pt[:, 0:64]), reads=[Bp], writes=[Bd])
                        k.dma("sp", dtr_d[t0 + blk * 128:t0 + (blk + 1) * 128, :], dd[:], reads=[Bd], write=B_dtr)
                    if t0 < TPRE:
                        continue
                    o0 = t0 - TPRE
                    for g in range(8):
                        wz, Bwz = wr.get()
                        k.dma("pool", wz[:].rearrange("p c n -> p (c n)"), I["w_z"]()[g], write=Bwz)
                        for blk in range(4):
                            pt, Bp = psum.get()
                            for c in range(KC):
                                k.op("pe", lambda e: e.matmul(pt[:], lhsT=xT[:, c, blk * 128:(blk + 1) * 128],
                                                              rhs=wz[:, c, :], start=(c == 0), stop=(c == KC - 1)),
                                     reads=[BxT, Bwz], writes=[Bp], inc=(c == KC - 1))
                            oo, Bo = orr.get()
                            k.op("act", lambda e: e.activation(out=oo[:], in_=pt[:], func=AF.Silu),
                                 reads=[Bp], writes=[Bo])
                            k.dma("sp", zs_d[o0 + blk * 128:o0 + (blk + 1) * 128, g * 512:(g + 1) * 512], oo[:],
                                  reads=[Bo], write=B_zs)

        if "zdt" in phases:
            phase_zdt()

        def phase_ssd():
            k.barrier()
            with ExitStack() as st:
                triu_f = sb(st, "s_triu", [128, 128], F32)
                negm_f = sb(st, "s_negm", [128, 512], F32)
                gssd = sb(st, "s_gssd", [128, D], F32)
                abc = sb(st, "s_abc", [128, NH], F32)
                dtb = sb(st, "s_dtb", [128, NH], F32)
                dsk = sb(st, "s_dsk", [128, NH], F32)
                flg = sb(st, "s_flag", [128, 1], F32)
                Bc = k.buf()
                k.dma("sp", triu_f[:], I["c_triu"](), write=Bc)
                Bc1 = k.buf()
                k.dma("sp", negm_f[:], I["c_negm"](), write=Bc1)
                Bg = k.buf()
                k.dma("sp", gssd[:], bcast_row(I["g_ssd"](), D), write=Bg)
                Ba = k.buf()
                k.dma("sp", abc[:], bcast_row(I["a_log"](), NH), write=Ba)
                k.op("act", lambda e: e.activation(out=abc[:], in_=abc[:], func=AF.Exp), reads=[Ba], writes=[Ba])
                k.op("dve", lambda e: e.tensor_scalar(out=abc[:], in0=abc[:], scalar1=-1.0, scalar2=None, op0=ALU.mult),
                     reads=[Ba], writes=[Ba])
                Bdb = k.buf()
                k.dma("sp", dtb[:], bcast_row(I["dt_bias"](), NH), write=Bdb)
                Bds = k.buf()
                k.dma("sp", dsk[:], bcast_row(I["d_skip"](), NH), write=Bds)
                Bfl = k.buf()
                k.dma("sp", flg[:], I["flag"](), write=Bfl)

                S = sb(st, "s_S", [128, D], F32)
                S_bf = sb(st, "s_Sbf", [128, D], BF16)
                BS = k.buf()
                BSb = k.buf()
                k.op("dve", lambda e: e.memset(S[:], 0.0), writes=[BS])
                k.op("pool", lambda e: e.memset(S_bf[:], 0.0), writes=[BSb])

                xin = Ring([(sb(st, f"s_xin{i}", [128, 32, 128], F32), k.buf()) for i in range(1)])
                bin_ = Ring([(sb(st, f"s_bin{i}", [128, 8, 128], F32), k.buf()) for i in range(1)])
                cin = Ring([(sb(st, f"s_cin{i}", [128, 8, 128], F32), k.buf()) for i in range(1)])
                dtin = Ring([(sb(st, f"s_dtin{i}", [128, NH], F32), k.buf()) for i in range(2)])
                zin = Ring([(sb(st, f"s_zin{i}", [128, D], F32), k.buf()) for i in range(2)])
                xdt_r = Ring([(sb(st, f"s_xdt{i}", [128, D], BF16), k.buf()) for i in range(2)])
                xdtw_r = Ring([(sb(st, f"s_xdtw{i}", [128, D], BF16), k.buf()) for i in range(2)])
                xD_r = Ring([(sb(st, f"s_xD{i}", [128, D], BF16), k.buf()) for i in range(2)])
                Btm_r = Ring([(sb(st, f"s_Btm{i}", [128, 1024], BF16), k.buf()) for i in range(2)])
                BTb_r = Ring([(sb(st, f"s_BTb{i}", [128, 8, 128], BF16), k.buf()) for i in range(2)])
                CTb_r = Ring([(sb(st, f"s_CTb{i}", [128, 8, 128], BF16), k.buf()) for i in range(2)])
                sm_r = Ring([(sb(st, f"s_sm{i}", [128, 10, NH], F32), [k.buf() for _ in range(10)]) for i in range(2)])
                rhsg = Ring([(sb(st, f"s_rhsg{i}", [128, 8, 128], F32), k.buf()) for i in range(2)])
                Er = Ring([(sb(st, f"s_E{i}", [128, 8, 128], F32), k.buf()) for i in range(2)])
                MTr = Ring([(sb(st, f"s_MT{i}", [128, 8, 128], BF16), k.buf()) for i in range(2)])
                cbr = Ring([(sb(st, f"s_cb{i}", [128, 128], F32), k.buf()) for i in range(2)])
                tmpr = Ring([(sb(st, f"s_tmp{i}", [128, 512], F32), k.buf()) for i in range(2)])
                ygr = Ring([(sb(st, f"s_yg{i}", [128, 512], F32), k.buf()) for i in range(2)])
                sqr = Ring([(sb(st, f"s_sq{i}", [128, 512], F32), k.buf()) for i in range(1)])
                ynr = Ring([(sb(st, f"s_yn{i}", [128, 512], BF16), k.buf()) for i in range(2)])
                ssr = Ring([(sb(st, f"s_ss{i}", [128, 2], F32), k.buf()) for i in range(2)])
                yTs = Ring([(sb(st, f"s_yT{i}", [128, 32, 128], BF16), k.buf()) for i in range(1)])

                xrows = xbcT_d[0:4096, :].rearrange("(c p) t -> p c t", p=128)
                brows = xbcT_d[4096:5120, :].rearrange("(c p) t -> p c t", p=128)
                crows = xbcT_d[5120:6144, :].rearrange("(c p) t -> p c t", p=128)
                yrows = yT_d[0:4096, :].rearrange("(c p) t -> p c t", p=128)

                def h3(ap_, g):
                    return ap_[:, 8 * g:8 * g + 8].unsqueeze(2).to_broadcast([128, 8, 64])

                nchunks = TALL // 128
                def chunk_gen(ci):
                    xdt, Bxdt = xdt_r.get()
                    xdtw, Bxdtw = xdtw_r.get()
                    xD, BxD = xD_r.get()
                    Btm, BBtm = Btm_r.get()
                    BTb, BBTb = BTb_r.get()
                    CTb, BCTb = CTb_r.get()
                    sm, Bsm = sm_r.get()
                    t0 = ci * 128
                    own = t0 >= TPRE
                    need_sbf = (t0 + 128 >= TPRE) and (ci + 1 < nchunks)
                    xi, Bxi = xin.get()
                    k.dma("sp", xi[:], xrows[:, :, t0:t0 + 128], reads=[B_xbcT], write=Bxi)
                    bi_, Bbi = bin_.get()
                    k.dma("sp", bi_[:], brows[:, :, t0:t0 + 128], reads=[B_xbcT], write=Bbi)
                    dti, Bdti = dtin.get()
                    k.dma("sp", dti[:], dtr_d[t0:t0 + 128, :], reads=[B_dtr], write=Bdti)
                    if own:
                        ci_, Bci = cin.get()
                        k.dma("sp", ci_[:], crows[:, :, t0:t0 + 128], reads=[B_xbcT], write=Bci)
                        zi, Bzi = zin.get()
                        k.dma("sp", zi[:], zs_d[t0 - TPRE:t0 - TPRE + 128, :], reads=[B_zs], write=Bzi)
                    v, ab, mx, dt_, a_ = (sm[:, i, :] for i in range(5))
                    k.op("dve", lambda e: e.tensor_tensor(out=v, in0=dti[:], in1=dtb[:], op=ALU.add),
                         reads=[Bdti, Bdb], writes=[Bsm[0]])
                    k.op("act", lambda e: e.activation(out=ab, in_=v, func=AF.Abs),
                         reads=[Bsm[0]], writes=[Bsm[1]])
                    k.op("act", lambda e: e.activation(out=ab, in_=ab, func=AF.Exp, scale=-1.0),
                         reads=[Bsm[1]], writes=[Bsm[1]])
                    k.op("act", lambda e: e.activation(out=ab, in_=ab, func=AF.Ln, bias=1.0, scale=1.0),
                         reads=[Bsm[1]], writes=[Bsm[1]])
                    k.op("dve", lambda e: e.tensor_scalar_max(out=mx, in0=v, scalar1=0.0),
                         reads=[Bsm[0]], writes=[Bsm[2]])
                    k.op("dve", lambda e: e.tensor_tensor(out=dt_, in0=mx, in1=ab, op=ALU.add),
                         reads=[Bsm[1], Bsm[2]], writes=[Bsm[3]])
                    k.op("dve", lambda e: e.tensor_tensor(out=a_, in0=dt_, in1=abc[:], op=ALU.mult),
                         reads=[Bsm[3], Ba], writes=[Bsm[4]])
                    pA, BpA = psum.get()
                    k.op("pe", lambda e: e.matmul(pA[:, 0:64], lhsT=triu_f[:], rhs=a_, start=True, stop=True),
                         reads=[Bc, Bsm[4]], writes=[BpA])
                    k.op("pe", lambda e: e.matmul(pA[:, 64:128], lhsT=ones_f[:], rhs=a_, start=True, stop=True),
                         reads=[B_c3, Bsm[4]], writes=[BpA])
                    acum, nacum, eacum, toend, dA = (sm[:, i, :] for i in range(5, 10))
                    k.op("act", lambda e: e.copy(out=acum, in_=pA[:, 0:64]), reads=[BpA], writes=[Bsm[5]])
                    k.op("act", lambda e: e.mul(out=nacum, in_=pA[:, 0:64], mul=-1.0), reads=[BpA], writes=[Bsm[6]])
                    if own:
                        k.op("act", lambda e: e.activation(out=eacum, in_=pA[:, 0:64], func=AF.Exp),
                             reads=[BpA], writes=[Bsm[7]])
                    k.op("dve", lambda e: e.tensor_tensor(out=toend, in0=pA[:, 64:128], in1=acum, op=ALU.subtract),
                         reads=[BpA, Bsm[5]], writes=[Bsm[8]])
                    k.op("act", lambda e: e.activation(out=toend, in_=toend, func=AF.Exp),
                         reads=[Bsm[8]], writes=[Bsm[8]])
                    k.op("act", lambda e: e.activation(out=dA, in_=pA[:, 64:128], func=AF.Exp),
                         reads=[BpA], writes=[Bsm[9]])
                    for g in range(NG):
                        pt, Bp = psum.get()
                        for j in range(4):
                            k.op("pe", lambda e: e.transpose(out=pt[:, j * 128:(j + 1) * 128], in_=xi[:, 4 * g + j, :],
                                                             identity=ident_f[:]),
                                 reads=[Bxi, B_const], writes=[Bp], inc=(j == 3))
                        p3 = pt[:].rearrange("p (h d) -> p h d", h=8)
                        k.op("dve", lambda e: e.tensor_tensor(
                            out=xdt[:, 512 * g:512 * (g + 1)].rearrange("p (h d) -> p h d", h=8),
                            in0=p3, in1=h3(dt_, g), op=ALU.mult), reads=[Bp, Bsm[3]], writes=[Bxdt])
                        if own:
                            k.op("dve", lambda e: e.tensor_tensor(
                                out=xD[:, 512 * g:512 * (g + 1)].rearrange("p (h d) -> p h d", h=8),
                                in0=p3, in1=h3(dsk[:], g), op=ALU.mult), reads=[Bp, Bds], writes=[BxD])
                        k.op("pool", lambda e: e.tensor_tensor(
                            out=xdtw[:, 512 * g:512 * (g + 1)].rearrange("p (h d) -> p h d", h=8),
                            in0=xdt[:, 512 * g:512 * (g + 1)].rearrange("p (h d) -> p h d", h=8),
                            in1=h3(toend, g), op=ALU.mult), reads=[Bxdt, Bsm[8]], writes=[Bxdtw])
                    for half in range(2):
                        pt, Bp = psum.get()
                        for j in range(4):
                            k.op("pe", lambda e: e.transpose(out=pt[:, j * 128:(j + 1) * 128], in_=bi_[:, 4 * half + j, :],
                                                             identity=ident_f[:]),
                                 reads=[Bbi, B_const], writes=[Bp], inc=(j == 3))
                        k.op("act", lambda e: e.copy(out=Btm[:, 512 * half:512 * (half + 1)], in_=pt[:]),
                             reads=[Bp], writes=[BBtm])
                    if own:
                        k.op("act", lambda e: e.copy(out=BTb[:], in_=bi_[:]), reads=[Bbi], writes=[BBTb])
                        k.op("pool", lambda e: e.tensor_copy(out=CTb[:], in_=ci_[:]), reads=[Bci], writes=[BCTb])
                        yT, ByT = yTs.get()
                    yield

                    def front(g):
                        gs = slice(512 * g, 512 * (g + 1))
                        pc, Bpc = psum.get()
                        k.op("pe", lambda e: e.matmul(pc[:, 0:128], lhsT=BTb[:, g, :], rhs=CTb[:, g, :],
                                                      start=True, stop=True),
                             reads=[BBTb, BCTb], writes=[Bpc])
                        cb, Bcb = cbr.get()
                        k.op("act", lambda e: e.copy(out=cb[:], in_=pc[:, 0:128]), reads=[Bpc], writes=[Bcb])
                        rg, Brg = rhsg.get()
                        k.op("dve", lambda e: e.tensor_tensor(
                            out=rg[:], in0=triu_f[:].unsqueeze(1).to_broadcast([128, 8, 128]),
                            in1=a_[:, 8 * g:8 * g + 8].unsqueeze(2).to_broadcast([128, 8, 128]), op=ALU.mult),
                             reads=[Bc, Bsm[4]], writes=[Brg])
                        E, BE = Er.get()
                        for j in range(2):
                            pseg, Bps = psum.get()
                            k.op("pe", lambda e: e.matmul(
                                pseg[:], lhsT=ones_f[:], rhs=rg[:, 4 * j:4 * j + 4, :].rearrange("p h t -> p (h t)"),
                                start=True, stop=False), reads=[B_c3, Brg], writes=[Bps], inc=False)
                            k.op("pe", lambda e: e.matmul(pseg[:], lhsT=ident_f[:], rhs=negm_f[:],
                                                          start=False, stop=True),
                                 reads=[B_const, Bc1], writes=[Bps])
                            for hh in range(4):
                                h = 8 * g + 4 * j + hh
                                k.op("act", lambda e: e.activation(
                                    out=E[:, 4 * j + hh, :], in_=pseg[:, hh * 128:(hh + 1) * 128], func=AF.Exp,
                                    bias=nacum[:, h:h + 1], scale=1.0), reads=[Bps, Bsm[6]], writes=[BE])
                        MT, BMT = MTr.get()
                        k.op("dve", lambda e: e.tensor_tensor(
                            out=MT[:], in0=E[:], in1=cb[:].unsqueeze(1).to_broadcast([128, 8, 128]), op=ALU.mult),
                             reads=[BE, Bcb], writes=[BMT])
                        return MT, BMT

                    def back(g, MT, BMT):
                        gs = slice(512 * g, 512 * (g + 1))
                        if own:
                            py, Bpy = psum.get()
                            k.op("pe", lambda e: e.matmul(py[:], lhsT=ident_b[:], rhs=xD[:, gs], start=True, stop=False),
                                 reads=[B_c2, BxD], writes=[Bpy], inc=False)
                            for hh in range(8):
                                k.op("pe", lambda e: e.matmul(py[:, 64 * hh:64 * (hh + 1)], lhsT=MT[:, hh, :],
                                                              rhs=xdt[:, 512 * g + 64 * hh:512 * g + 64 * (hh + 1)],
                                                              start=False, stop=(hh == 7)),
                                     reads=[BMT, Bxdt], writes=[Bpy], inc=(hh == 7))
                            po, Bpo = psum.get()
                            k.op("pe", lambda e: e.matmul(po[:], lhsT=CTb[:, g, :], rhs=S_bf[:, gs], start=True, stop=True),
                                 reads=[BCTb, BSb], writes=[Bpo])
                            tmp, Btmp = tmpr.get()
                            k.op("dve", lambda e: e.tensor_tensor(
                                out=tmp[:].rearrange("p (h d) -> p h d", h=8),
                                in0=po[:].rearrange("p (h d) -> p h d", h=8), in1=h3(eacum, g), op=ALU.mult),
                                 reads=[Bpo, Bsm[7]], writes=[Btmp])
                            yg, Byg = ygr.get()
                            k.op("dve", lambda e: e.tensor_tensor(out=yg[:], in0=py[:], in1=tmp[:], op=ALU.add),
                                 reads=[Bpy, Btmp], writes=[Byg])
                            k.op("pool", lambda e: e.tensor_tensor(out=yg[:], in0=yg[:], in1=zi[:, gs], op=ALU.mult),
                                 reads=[Byg, Bzi], writes=[Byg])
                            sq, Bsq = sqr.get()
                            ss, Bss = ssr.get()
                            k.op("act", lambda e: e.activation(out=sq[:], in_=yg[:], func=AF.Square, accum_out=ss[:, 0:1]),
                                 reads=[Byg], writes=[Bsq, Bss])
                            k.op("act", lambda e: e.activation(out=ss[:, 1:2], in_=ss[:, 0:1], func=AF.Sqrt,
                                                               bias=EPS, scale=1.0 / 512),
                                 reads=[Bss], writes=[Bss])
                            k.op("dve", lambda e: e.reciprocal(out=ss[:, 1:2], in_=ss[:, 1:2]), reads=[Bss], writes=[Bss])
                            yn, Byn = ynr.get()
                            k.op("dve", lambda e: e.scalar_tensor_tensor(
                                out=yn[:], in0=yg[:], scalar=ss[:, 1:2], in1=gssd[:, gs], op0=ALU.mult, op1=ALU.mult),
                                 reads=[Byg, Bss, Bg], writes=[Byn])
                            ptT, BpT = psum.get()
                            ptb = ptT[:].bitcast(BF16).rearrange("p (a b) -> p a b", a=8)
                            for j in range(4):
                                k.op("pe", lambda e: e.transpose(out=ptb[:, j, :], in_=yn[:, 128 * j:128 * (j + 1)],
                                                                 identity=ident_b[:]),
                                     reads=[Byn, B_c2], writes=[BpT], inc=(j == 3))
                            k.op("act", lambda e: e.copy(out=yT[:, 4 * g:4 * g + 4, :], in_=ptb[:, 0:4, :]),
                                 reads=[BpT], writes=[ByT])
                        pu, Bpu = psum.get()
                        k.op("pe", lambda e: e.matmul(pu[:], lhsT=Btm[:, 128 * g:128 * (g + 1)], rhs=xdtw[:, gs],
                                                      start=True, stop=True),
                             reads=[BBtm, Bxdtw], writes=[Bpu])
                        k.op("pool", lambda e: e.tensor_tensor(
                            out=S[:, gs].rearrange("p (h d) -> p h d", h=8),
                            in0=S[:, gs].rearrange("p (h d) -> p h d", h=8), in1=h3(dA, g), op=ALU.mult),
                             reads=[BS, Bsm[9]], writes=[BS])
                        k.op("dve", lambda e: e.tensor_tensor(out=S[:, gs], in0=S[:, gs], in1=pu[:], op=ALU.add),
                             reads=[BS, Bpu], writes=[BS])

                    nxt = front(0) if own else (None, None)
                    for g in range(NG):
                        cur = nxt
                        if own and g + 1 < NG:
                            nxt = front(g + 1)
                        back(g, *cur)
                    if own:
                        o0 = t0 - TPRE
                        k.dma("sp", yrows[:, :, o0:o0 + 128], yT[:], reads=[ByT], write=B_yT)
                    if t0 + 128 == TPRE:
                        k.op("dve", lambda e: e.tensor_scalar(out=S[:], in0=S[:], scalar1=flg[:, 0:1], scalar2=None,
                                                              op0=ALU.mult), reads=[BS, Bfl], writes=[BS])
                    if need_sbf:
                        k.op("act", lambda e: e.copy(out=S_bf[:], in_=S[:]), reads=[BS], writes=[BSb])
                gens = [chunk_gen(ci) for ci in range(nchunks)]
                next(gens[0])
                for ci in range(nchunks):
                    if ci + 1 < nchunks:
                        next(gens[ci + 1])
                    for _ in gens[ci]:
                        pass

        if "ssd" in phases:
            phase_ssd()

        def phase_scpack():
            k.barrier()
            with ExitStack() as st:
                gsc = sb(st, "p_gsc", [128, 32], F32)
                Bg = k.buf()
                k.dma("sp", gsc[:], I["g_sc"](), write=Bg)
                rs = sb(st, "p_rs", [128, TOWN], F32)
                Brs = k.buf()
                k.dma("sp", rs[:], rstd_sc_d[:, HALO:HALO + TOWN], reads=[B_rstdsc], write=Brs)
                yr = Ring([(sb(st, f"p_y{i}", [128, TOWN], F32), k.buf()) for i in range(2)])
                orr = Ring([(sb(st, f"p_o{i}", [128, TOWN], BF16), k.buf()) for i in range(2)])
                for i in range(32):
                    y, By = yr.get()
                    k.dma("sp", y[:], ysc_d[i * 128:(i + 1) * 128, HALO:HALO + TOWN], reads=[B_ysc], write=By)
                    o, Bo = orr.get()
                    k.op("dve", lambda e: e.scalar_tensor_tensor(out=o[:], in0=y[:], scalar=gsc[:, i:i + 1], in1=rs[:],
                                                                 op0=ALU.mult, op1=ALU.mult),
                         reads=[By, Bg, Brs], writes=[Bo])
                    k.dma("sp", yT_d[D + i * 128:D + (i + 1) * 128, :], o[:], reads=[Bo], write=B_yT)

        if "scpack" in phases:
            phase_scpack()

        def phase_outproj():
            k.barrier()
            with ExitStack() as st:
                yt = sb(st, "o_y", [128, 64, 512], BF16)
                Byt = k.buf()
                wr = Ring([(sb(st, f"o_w{i}", [128, 32, 512], BF16), k.buf()) for i in range(3)])
                xr = Ring([(sb(st, f"o_x{i}", [128, 512], F32), k.buf()) for i in range(3)])
                hr = Ring([(sb(st, f"o_h{i}", [128, 512], F32), k.buf()) for i in range(3)])
                yv = yT_d.rearrange("(c p) t -> p c t", p=128)
                x_all = I["x_all"]()
                for o0 in range(0, TOWN, 512):
                    k.dma("sp", yt[:], yv[:, :, o0:o0 + 512], reads=[B_yT], write=Byt)
                    for s in range(8):
                        accs = [psum.get() for _ in range(4)]
                        for half in range(2):
                            w, Bw = wr.get()
                            k.dma("pool", w[:].rearrange("p c n -> p (c n)"), I["w_out"]()[s, half], write=Bw)
                            for blk in range(4):
                                pt, Bp = accs[blk]
                                for c in range(32):
                                    last = (half == 1 and c == 31)
                                    k.op("pe", lambda e: e.matmul(
                                        pt[:], lhsT=yt[:, half * 32 + c, blk * 128:(blk + 1) * 128], rhs=w[:, c, :],
                                        start=(half == 0 and c == 0), stop=last),
                                         reads=[Byt, Bw], writes=[Bp], inc=(c == 31))
                        for blk in range(4):
                            pt, Bp = accs[blk]
                            r0 = o0 + blk * 128
                            xt, Bx = xr.get()
                            k.dma("sp", xt[:], x_all[TPRE + r0:TPRE + r0 + 128, s * 512:(s + 1) * 512], write=Bx)
                            ht, Bh = hr.get()
                            k.op("dve", lambda e: e.tensor_tensor(out=ht[:], in0=pt[:], in1=xt[:], op=ALU.add),
                                 reads=[Bp, Bx], writes=[Bh])
                            k.dma("sp", h1_d[r0:r0 + 128, s * 512:(s + 1) * 512], ht[:], reads=[Bh], write=B_h1)

        if "outproj" in phases:
            phase_outproj()

        if "norm2" in phases:
            phase_norm(h1_d, TOWN, I["g_ffn"](), xn2T_d, B_xn2T, "n2", src_dep=B_h1)

        def phase_qg():
            k.barrier()
            with ExitStack() as st:
                iota = sb(st, "q_iota", [128, 128], F32)
                Bio = k.buf()
                k.dma("sp", iota[:], I["c_iota"](), write=Bio)
                iota16s = sb(st, "q_iota16s", [128, 16], F32)
                k.op("dve", lambda e: e.tensor_scalar(out=iota16s[:], in0=iota[:, 0:16], scalar1=16.0, scalar2=None,
                                                      op0=ALU.mult), reads=[Bio], writes=[Bio])
                keys = sb(st, "q_keys", [128, 16, 128], F32)
                Bky = k.buf()
                k.dma("sp", keys[:], I["keys_t"]().rearrange("c d n -> d c n"), write=Bky)
                xT = sb(st, "q_xT", [128, KC, 512], BF16)
                BxT = k.buf()
                wr = Ring([(sb(st, f"q_w{i}", [128, KC, 128], BF16), k.buf()) for i in range(2)])
                qT = sb(st, "q_qT", [128, 16, 512], F32)
                BqT = k.buf()
                sc = sb(st, "q_sc", [128, 16, 128], F32)
                sc2 = sb(st, "q_sc2", [128, 16, 128], F32)
                Bsc, Bsc2 = k.buf(), k.buf()
                tops = sb(st, "q_tops", [128, 16, 16], F32)
                topu = sb(st, "q_topu", [128, 16, 16], U32)
                topi = sb(st, "q_topi", [128, 16, 16], F32)
                Btops, Btopu, Btopi = k.buf(), k.buf(), k.buf()
                cand = sc2[:].rearrange("p (h c) n -> p h (c n)", c=2)
                cand2 = sb(st, "q_cand2", [128, 8, 256], F32)
                Bcand, Bcand2 = Bsc2, k.buf()
                bests = sb(st, "q_bests", [128, 8, 16], F32)
                bposu = sb(st, "q_bposu", [128, 8, 16], U32)
                au = sb(st, "q_au", [128, 8, 16], U32)
                bu = sb(st, "q_bu", [128, 8, 16], U32)
                bmod = sb(st, "q_bmod", [128, 8, 16], F32)
                a16 = sb(st, "q_a16", [128, 8, 16], F32)
                Bbests, Bbposu, Bbpos, Bbmod, Ba16 = (k.buf() for _ in range(5))
                oh = cand2[:].rearrange("p h (a b) -> p h a b", a=16)
                Boh = Bcand2
                Iv = sb(st, "q_I", [128, 8, 16], F32)
                Jv = sb(st, "q_J", [128, 8, 16], F32)
                gv = sb(st, "q_g", [128, 8, 16], F32)
                BIv, BJv, Bgv = k.buf(), k.buf(), k.buf()
                gsum = sb(st, "q_gsum", [128, 8], F32)
                Bgsum = k.buf()
                trr = Ring([([sb(st, f"q_tr{i}_{j}", [128, 128], F32) for i in range(3)], [k.buf() for _ in range(3)])
                            for j in range(2)])
                Ar = Ring([(sb(st, f"q_A{i}", [128, 16, 128], BF16), k.buf()) for i in range(2)])
                Br = Ring([(sb(st, f"q_B{i}", [128, 16, 128], BF16), k.buf()) for i in range(2)])
                Gr = Ring([(sb(st, f"q_G{i}", [128, 128, 128], BF16), k.buf()) for i in range(2)])
                Gv = G_d.rearrange("i j t -> j i t")

                for o0 in range(0, TOWN, 512):
                    k.dma("sp", xT[:], xn2T_d[:, :, o0:o0 + 512], reads=[B_xn2T], write=BxT)
                    for hc in range(16):
                        w, Bw = wr.get()
                        k.dma("pool", w[:].rearrange("p c n -> p (c n)"), I["w_q"]()[hc], write=Bw)
                        pt, Bp = psum.get()
                        for c in range(KC):
                            k.op("pe", lambda e: e.matmul(pt[:], lhsT=w[:, c, :], rhs=xT[:, c, :],
                                                          start=(c == 0), stop=(c == KC - 1)),
                                 reads=[Bw, BxT], writes=[Bp], inc=(c == KC - 1))
                        k.op("act", lambda e: e.copy(out=qT[:, hc, :], in_=pt[:]), reads=[Bp], writes=[BqT])
                    def q_front(blk):
                        r0 = o0 + blk * 128
                        tr, Btr = trr.get()
                        for q4 in range(4):
                            pt, Bp = psum.get()
                            for j in range(4):
                                hc = 4 * q4 + j
                                k.op("pe", lambda e: e.matmul(pt[:, j * 128:(j + 1) * 128],
                                                              lhsT=qT[:, hc, blk * 128:(blk + 1) * 128],
                                                              rhs=keys[:, hc, :], start=True, stop=True),
                                     reads=[BqT, Bky], writes=[Bp], inc=(j == 3))
                            k.op("act", lambda e: e.copy(out=sc[:, 4 * q4:4 * q4 + 4, :].rearrange("p a b -> p (a b)"),
                                                         in_=pt[:]), reads=[Bp], writes=[Bsc])
                        for hc in range(16):
                            k.op("dve", lambda e: e.max(out=tops[:, hc, 0:8], in_=sc[:, hc, :]),
                                 reads=[Bsc], writes=[Btops])
                            k.op("dve", lambda e: e.max_index(out=topu[:, hc, 0:8], in_max=tops[:, hc, 0:8],
                                                              in_values=sc[:, hc, :]),
                                 reads=[Bsc, Btops], writes=[Btopu])
                            k.op("dve", lambda e: e.match_replace(out=sc2[:, hc, :], in_to_replace=tops[:, hc, 0:8],
                                                                  in_values=sc[:, hc, :], imm_value=-1e30),
                                 reads=[Bsc, Btops], writes=[Bsc2])
                            k.op("dve", lambda e: e.max(out=tops[:, hc, 8:16], in_=sc2[:, hc, :]),
                                 reads=[Bsc2], writes=[Btops])
                            k.op("dve", lambda e: e.max_index(out=topu[:, hc, 8:16], in_max=tops[:, hc, 8:16],
                                                              in_values=sc2[:, hc, :]),
                                 reads=[Bsc2, Btops], writes=[Btopu])
                        k.op("dve", lambda e: e.tensor_copy(out=topi[:], in_=topu[:]), reads=[Btopu], writes=[Btopi])
                        t4 = tops[:].rearrange("p (h c) a -> p h c a", c=2)
                        k.op("dve", lambda e: e.tensor_tensor(
                            out=cand.rearrange("p h (a b) -> p h a b", a=16),
                            in0=t4[:, :, 0, :].unsqueeze(3).to_broadcast([128, 8, 16, 16]),
                            in1=t4[:, :, 1, :].unsqueeze(2).to_broadcast([128, 8, 16, 16]), op=ALU.add),
                             reads=[Btops], writes=[Bcand])
                        for h in range(8):
                            k.op("dve", lambda e: e.max(out=bests[:, h, 0:8], in_=cand[:, h, :]),
                                 reads=[Bcand], writes=[Bbests])
                            k.op("dve", lambda e: e.max_index(out=bposu[:, h, 0:8], in_max=bests[:, h, 0:8],
                                                              in_values=cand[:, h, :]),
                                 reads=[Bcand, Bbests], writes=[Bbposu])
                            k.op("dve", lambda e: e.match_replace(out=cand2[:, h, :], in_to_replace=bests[:, h, 0:8],
                                                                  in_values=cand[:, h, :], imm_value=-1e30),
                                 reads=[Bcand, Bbests], writes=[Bcand2])
                            k.op("dve", lambda e: e.max(out=bests[:, h, 8:16], in_=cand2[:, h, :]),
                                 reads=[Bcand2], writes=[Bbests])
                            k.op("dve", lambda e: e.max_index(out=bposu[:, h, 8:16], in_max=bests[:, h, 8:16],
                                                              in_values=cand2[:, h, :]),
                                 reads=[Bcand2, Bbests], writes=[Bbposu])
                        k.op("dve", lambda e: e.tensor_single_scalar(out=au[:], in_=bposu[:], scalar=4,
                                                                     op=ALU.logical_shift_right),
                             reads=[Bbposu], writes=[Bbpos])
                        k.op("dve", lambda e: e.tensor_single_scalar(out=bu[:], in_=bposu[:], scalar=15,
                                                                     op=ALU.bitwise_and),
                             reads=[Bbposu], writes=[Bbmod])
                        k.op("dve", lambda e: e.tensor_copy(out=a16[:], in_=au[:]), reads=[Bbpos], writes=[Ba16])
                        k.op("dve", lambda e: e.tensor_copy(out=bmod[:], in_=bu[:]), reads=[Bbmod], writes=[Bbmod])
                        i4 = topi[:].rearrange("p (h c) a -> p h c a", c=2)
                        io16 = iota[:, 0:16].unsqueeze(1).unsqueeze(1).to_broadcast([128, 8, 16, 16])
                        io16s = iota16s[:].unsqueeze(1).unsqueeze(1).to_broadcast([128, 8, 16, 16])
                        k.op("dve", lambda e: e.tensor_tensor(
                            out=oh, in0=io16, in1=a16[:].unsqueeze(3).to_broadcast([128, 8, 16, 16]),
                            op=ALU.is_equal), reads=[Bio, Ba16], writes=[Boh])
                        k.op("dve", lambda e: e.tensor_tensor(
                            out=oh, in0=oh, in1=i4[:, :, 0, :].unsqueeze(2).to_broadcast([128, 8, 16, 16]),
                            op=ALU.mult), reads=[Boh, Btopi], writes=[Boh])
                        k.op("dve", lambda e: e.tensor_reduce(out=Iv[:], in_=oh, axis=AX.X, op=ALU.add),
                             reads=[Boh], writes=[BIv])
                        k.op("dve", lambda e: e.tensor_tensor(
                            out=oh, in0=io16, in1=bmod[:].unsqueeze(3).to_broadcast([128, 8, 16, 16]),
                            op=ALU.is_equal), reads=[Bio, Bbmod, BIv], writes=[Boh])
                        k.op("dve", lambda e: e.tensor_tensor(
                            out=oh, in0=oh, in1=i4[:, :, 1, :].unsqueeze(2).to_broadcast([128, 8, 16, 16]),
                            op=ALU.mult), reads=[Boh, Btopi], writes=[Boh])
                        k.op("dve", lambda e: e.tensor_reduce(out=Jv[:], in_=oh, axis=AX.X, op=ALU.add),
                             reads=[Boh], writes=[BJv])
                        k.op("dve", lambda e: e.tensor_tensor(
                            out=gv[:], in0=bests[:], in1=bests[:, :, 0:1].to_broadcast([128, 8, 16]), op=ALU.subtract),
                             reads=[Bbests], writes=[Bgv])
                        k.op("act", lambda e: e.activation(out=gv[:], in_=gv[:], func=AF.Exp), reads=[Bgv], writes=[Bgv])
                        k.op("dve", lambda e: e.tensor_reduce(out=gsum[:], in_=gv[:], axis=AX.X, op=ALU.add),
                             reads=[Bgv], writes=[Bgsum])
                        k.op("dve", lambda e: e.reciprocal(out=gsum[:], in_=gsum[:]), reads=[Bgsum], writes=[Bgsum])
                        k.op("dve", lambda e: e.tensor_tensor(
                            out=gv[:], in0=gv[:], in1=gsum[:].unsqueeze(2).to_broadcast([128, 8, 16]), op=ALU.mult),
                             reads=[Bgv, Bgsum], writes=[Bgv])
                        for n_, (src, Bsrc) in enumerate(((Iv, BIv), (Jv, BJv), (gv, Bgv))):
                            pt, Bp = psum.get()
                            k.op("pe", lambda e: e.transpose(out=pt[:, 0:128], in_=src[:].rearrange("p h k -> p (h k)"),
                                                             identity=ident_f[:]),
                                 reads=[Bsrc, B_const], writes=[Bp])
                            k.op("act", lambda e: e.copy(out=tr[n_][:], in_=pt[:, 0:128]), reads=[Bp], writes=[Btr[n_]])
                        return tr, Btr

                    def q_back(blk, tr, Btr):
                        r0 = o0 + blk * 128
                        Gst, BGst = Gr.get()
                        IT, JT, gT = tr
                        iob = iota[:].unsqueeze(1).to_broadcast([128, 16, 128])
                        for t16 in range(0, 128, 16):
                            A, BA = Ar.get()
                            Bm, BB = Br.get()
                            k.op("dve", lambda e: e.tensor_tensor(
                                out=A[:], in0=iob, in1=IT[:, t16:t16 + 16].unsqueeze(2).to_broadcast([128, 16, 128]),
                                op=ALU.is_equal), reads=[Bio, Btr[0]], writes=[BA])
                            k.op("dve", lambda e: e.tensor_tensor(
                                out=A[:], in0=A[:], in1=gT[:, t16:t16 + 16].unsqueeze(2).to_broadcast([128, 16, 128]),
                                op=ALU.mult), reads=[BA, Btr[2]], writes=[BA])
                            k.op("dve", lambda e: e.tensor_tensor(
                                out=Bm[:], in0=iob, in1=JT[:, t16:t16 + 16].unsqueeze(2).to_broadcast([128, 16, 128]),
                                op=ALU.is_equal), reads=[Bio, Btr[1]], writes=[BB])
                            for q4 in range(4):
                                pt, Bp = psum.get()
                                for tt in range(4):
                                    k.op("pe", lambda e: e.matmul(pt[:, tt * 128:(tt + 1) * 128], lhsT=Bm[:, 4 * q4 + tt, :],
                                                                  rhs=A[:, 4 * q4 + tt, :], start=True, stop=True),
                                         reads=[BA, BB], writes=[Bp], inc=(tt == 3))
                                tb = t16 + 4 * q4
                                k.op("act", lambda e: e.copy(
                                    out=Gst[:, :, tb:tb + 4].rearrange("p i t -> p t i"),
                                    in_=pt[:].rearrange("p (t i) -> p t i", t=4)), reads=[Bp], writes=[BGst])
                        for i8 in range(8):
                            k.dma("sp", Gv[:, 16 * i8:16 * (i8 + 1), r0:r0 + 128], Gst[:, 16 * i8:16 * (i8 + 1), :],
                                  reads=[BGst], write=B_G)
                    nxt = q_front(0)
                    for blk in range(4):
                        cur = nxt
                        if blk + 1 < 4:
                            nxt = q_front(blk + 1)
                        q_back(blk, *cur)

        if "qg" in phases:
            phase_qg()

        def phase_gemm1():
            k.barrier()
            TB = min(1024, TOWN)
            with ExitStack() as st:
                xT = sb(st, "u_xT", [128, KC, TB], BF16)
                BxT = k.buf()
                wr = Ring([(sb(st, f"u_w{i}", [128, KC, 128], BF16), k.buf()) for i in range(3)])
                hr = Ring([(sb(st, f"u_h{i}", [128, TB], F32), k.buf()) for i in range(2)])
                gr = Ring([(sb(st, f"u_g{i}", [128, TB], BF16), k.buf()) for i in range(3)])
                orr = Ring([(sb(st, f"u_o{i}", [128, TB], BF16), k.buf()) for i in range(3)])
                for o0 in range(0, TOWN, TB):
                    k.dma("sp", xT[:], xn2T_d[:, :, o0:o0 + TB], reads=[B_xn2T], write=BxT)
                    for i in range(128):
                        w, Bw = wr.get()
                        k.dma("pool", w[:].rearrange("p c n -> p (c n)"), I["u_t"]()[i], write=Bw)
                        g, Bg = gr.get()
                        k.dma("sp", g[:], G_d[i, :, o0:o0 + TB], reads=[B_G], write=Bg)
                        hsb, Bh = hr.get()
                        for n0 in range(0, TB, 512):
                            pt, Bp = psum.get()
                            for c in range(KC):
                                k.op("pe", lambda e: e.matmul(pt[:], lhsT=w[:, c, :], rhs=xT[:, c, n0:n0 + 512],
                                                              start=(c == 0), stop=(c == KC - 1)),
                                     reads=[Bw, BxT], writes=[Bp], inc=(c == KC - 1))
                            k.op("act", lambda e: e.activation(out=hsb[:, n0:n0 + 512], in_=pt[:], func=AF.Gelu),
                                 reads=[Bp], writes=[Bh])
                        o, Bo = orr.get()
                        k.op("dve", lambda e: e.tensor_tensor(out=o[:], in0=hsb[:], in1=g[:], op=ALU.mult),
                             reads=[Bh, Bg], writes=[Bo])
                        k.dma("sp", HG_d[i * 128:(i + 1) * 128, o0:o0 + TB], o[:], reads=[Bo], write=B_HG)

        if "gemm1" in phases:
            phase_gemm1()

        def phase_gemm2():
            k.barrier()
            TB = min(1024, TOWN)
            nb = TB // 128
            with ExitStack() as st:
                hgr = Ring([(sb(st, f"v_hg{i}", [128, 2, TB], BF16), k.buf()) for i in range(3)])
                vr = Ring([(sb(st, f"v_v{i}", [128, 2, 512], BF16), k.buf()) for i in range(3)])
                xr = Ring([(sb(st, f"v_x{i}", [128, 512], F32), k.buf()) for i in range(3)])
                orr = Ring([(sb(st, f"v_o{i}", [128, 512], F32), k.buf()) for i in range(3)])
                for o0 in range(0, TOWN, TB):
                    for s in range(8):
                        accs = [psum.get() for _ in range(nb)]
                        for i2 in range(64):
                            hg, Bhg = hgr.get()
                            k.dma("sp", hg[:], HG_d[i2 * 256:(i2 + 1) * 256, o0:o0 + TB].rearrange("(a p) t -> p a t", p=128),
                                  reads=[B_HG], write=Bhg)
                            v, Bv = vr.get()
                            k.dma("sp", v[:], vb_d[i2 * 256:(i2 + 1) * 256, s * 512:(s + 1) * 512].rearrange(
                                "(a p) n -> p a n", p=128), reads=[B_vb], write=Bv)
                            for a in range(2):
                                i = 2 * i2 + a
                                for blk in range(nb):
                                    pt, Bp = accs[blk]
                                    k.op("pe", lambda e: e.matmul(pt[:], lhsT=hg[:, a, blk * 128:(blk + 1) * 128],
                                                                  rhs=v[:, a, :], start=(i == 0), stop=(i == 127)),
                                         reads=[Bhg, Bv], writes=[Bp], inc=(blk == nb - 1))
                        for blk in range(nb):
                            pt, Bp = accs[blk]
                            r0 = o0 + blk * 128
                            xt, Bx = xr.get()
                            k.dma("sp", xt[:], h1_d[r0:r0 + 128, s * 512:(s + 1) * 512], reads=[B_h1], write=Bx)
                            ot, Bo = orr.get()
                            k.op("dve", lambda e: e.tensor_tensor(out=ot[:], in0=pt[:], in1=xt[:], op=ALU.add),
                                 reads=[Bp, Bx], writes=[Bo])
                            k.dma("sp", h2_d[r0:r0 + 128, s * 512:(s + 1) * 512], ot[:], reads=[Bo], write=B_h2)

        if "gemm2" in phases:
            phase_gemm2()

        def phase_final():
            k.barrier()
            with ExitStack() as st:
                gain = sb(st, "f_gain", [128, D], F32)
                Bg = k.buf()
                k.dma("sp", gain[:], bcast_row(I["g_fin"](), D), write=Bg)
                xr = Ring([(sb(st, f"f_x{i}", [128, D], F32), k.buf()) for i in range(2)])
                sq = sb(st, "f_sq", [128, D], F32)
                Bsq = k.buf()
                orr = Ring([(sb(st, f"f_o{i}", [128, D], F32), k.buf()) for i in range(2)])
                smr = Ring([(sb(st, f"f_sm{i}", [128, 2], F32), k.buf()) for i in range(2)])
                for r0 in range(0, TOWN, 128):
                    xt, Bx = xr.get()
                    k.dma("sp", xt[:], h2_d[r0:r0 + 128, :], reads=[B_h2], write=Bx)
                    sm, Bsm = smr.get()
                    k.op("act", lambda e: e.activation(out=sq[:], in_=xt[:], func=AF.Square, accum_out=sm[:, 0:1]),
                         reads=[Bx], writes=[Bsq, Bsm])
                    k.op("act", lambda e: e.activation(out=sm[:, 1:2], in_=sm[:, 0:1], func=AF.Sqrt, bias=EPS,
                                                       scale=1.0 / D), reads=[Bsm], writes=[Bsm])
                    k.op("dve", lambda e: e.reciprocal(out=sm[:, 1:2], in_=sm[:, 1:2]), reads=[Bsm], writes=[Bsm])
                    ot, Bo = orr.get()
                    k.op("dve", lambda e: e.scalar_tensor_tensor(out=ot[:], in0=xt[:], scalar=sm[:, 1:2], in1=gain[:],
                                                                 op0=ALU.mult, op1=ALU.mult),
                         reads=[Bx, Bsm, Bg], writes=[Bo])
                    k.dma("sp", out[r0:r0 + 128, :], ot[:], reads=[Bo], write=B_out, is_output=True)

        if "final" in phases:
            phase_final()

        fin = list(k.out_tokens)
        for b in (B_xnT, B_xbcT, B_ysc, B_rstdsc, B_zs, B_dtr, B_out, B_yT, B_h1, B_xn2T, B_G, B_HG, B_h2, B_vb):
            fin += b.w
        k._wait("sp", fin)
    return nc


def prep_weights(inp):
    W = np.asarray(inp["w_in"])[0]
    o_z, o_x, o_B, o_C, o_dt, o_b, o_c, o_h = 0, 4096, 8192, 9216, 10240, 10304, 14400, 18496
    cols = np.concatenate([np.arange(o_x, o_x + 4096), np.arange(o_B, o_B + 1024), np.arange(o_C, o_C + 1024),
                           np.arange(o_b, o_b + 4096), np.arange(o_c, o_c + 4096), np.arange(o_h, o_h + 4096)])
    wf = W[:, cols]
    w_fm = np.ascontiguousarray(wf.reshape(KC, 128, 144, 128).transpose(2, 1, 0, 3)).reshape(144, 128, KC * 128)
    w_z = np.ascontiguousarray(W[:, 0:4096].reshape(KC, 128, 8, 512).transpose(2, 1, 0, 3)).reshape(8, 128, KC * 512)
    w_dt = np.ascontiguousarray(W[:, o_dt:o_dt + 64].reshape(KC, 128, 64).transpose(1, 0, 2)).reshape(128, KC * 64)
    cw = np.asarray(inp["ssd_conv_w"])[0]
    cw_xbc = np.ascontiguousarray(cw.reshape(4, 48, 128).transpose(2, 1, 0))
    cb_xbc = np.ascontiguousarray(np.asarray(inp["ssd_conv_b"])[0].reshape(48, 128).T)
    cs = np.asarray(inp["sc_conv_w"])[0]
    cw_sc = np.ascontiguousarray(cs.reshape(3, 32, 128).transpose(2, 1, 0))
    g_sc = np.ascontiguousarray(np.asarray(inp["sc_norm_w"])[0].reshape(32, 128).T)
    Wo = np.asarray(inp["w_out"])[0]
    w_out = np.ascontiguousarray(Wo.reshape(2, 32, 128, 8, 512).transpose(3, 0, 2, 1, 4)).reshape(8, 2, 128, 32 * 512)
    Wq = np.asarray(inp["peer_w_query"])[0]
    w_q = np.ascontiguousarray(Wq.reshape(KC, 128, 16, 128).transpose(2, 1, 0, 3)).reshape(16, 128, KC * 128)
    keys = np.asarray(inp["peer_sub_keys"])[0]
    keys_t = np.ascontiguousarray(keys.reshape(16, 128, 128).transpose(0, 2, 1))
    U = np.asarray(inp["peer_u"])[0]
    u_t = np.ascontiguousarray(U.reshape(128, 128, KC, 128).transpose(0, 3, 2, 1)).reshape(128, 128, KC * 128)
    V = np.asarray(inp["peer_v"])[0]
    u = np.arange(128)
    triu = (u[:, None] <= u[None, :]).astype(np.float32)
    negm = np.where(u[:, None] > u[None, :], np.float32(-30000.0), np.float32(0.0)).astype(np.float32)
    return dict(
        w_fm=w_fm, w_z=w_z, w_dt=w_dt, cw_xbc=cw_xbc, cb_xbc=cb_xbc, cw_sc=cw_sc, g_sc=g_sc,
        g_mix=np.asarray(inp["norm_mix_w"])[0], g_ffn=np.asarray(inp["norm_ffn_w"])[0],
        g_fin=np.asarray(inp["norm_final_w"]), g_ssd=np.asarray(inp["ssd_norm_w"])[0],
        dt_bias=np.asarray(inp["ssd_dt_bias"])[0], a_log=np.asarray(inp["ssd_a_log"])[0],
        d_skip=np.asarray(inp["ssd_d"])[0], w_out=w_out, w_q=w_q, keys_t=keys_t, u_t=u_t, v_nat=V,
        c_ident=np.eye(128, dtype=np.float32), c_triu=triu, c_negm=np.tile(negm, (1, 4)),
        c_iota=np.tile(np.arange(128, dtype=np.float32), (128, 1)),
    )


_NC_CACHE = {}


def kernel(**inputs):
    n_cores = 8
    TPRE = TOWN = 2048
    wts = prep_weights(inputs)
    x = np.asarray(inputs["x"])
    if "nc" not in _NC_CACHE:
        _NC_CACHE["nc"] = build(TPRE, TOWN)
    nc = _NC_CACHE["nc"]
    zeros = np.zeros((TPRE, D), np.float32)
    in_maps = []
    for c in range(n_cores):
        b, half = c // 2, c % 2
        own = x[b, half * TOWN:(half + 1) * TOWN]
        pre = x[b, 0:TPRE] if half == 1 else zeros
        m = dict(wts)
        m["x_all"] = np.ascontiguousarray(np.concatenate([pre, own], axis=0))
        m["flag"] = np.full((128, 1), float(half), np.float32)
        in_maps.append(m)
    res = run_bass_kernel_spmd(nc, in_maps, core_ids=list(range(n_cores)))
    out = np.empty((4, 2 * TOWN, D), np.float32)
    for c in range(n_cores):
        b, half = c // 2, c % 2
        out[b, half * TOWN:(half + 1) * TOWN] = res.results[c]["out"]
    return out
```

```python
import numpy as np
from contextlib import ExitStack
import concourse.bass as bass
import concourse.mybir as mybir
from concourse.bass_utils import run_bass_kernel_spmd

F32 = mybir.dt.float32
BF16 = mybir.dt.bfloat16
U32 = mybir.dt.uint32
AF = mybir.ActivationFunctionType
ALU = mybir.AluOpType
AX = mybir.AxisListType

D = 4096
KC = D // 128
NH = 64
NG = 8
NFM = 144
HALO = 64
EPS = 1e-6
NEG = -30000.0


class Buf:
    __slots__ = ("name", "w", "r", "dsem", "dcnt", "multi")

    def __init__(self, name, multi=False):
        self.name = name
        self.w = []
        self.r = []
        self.dsem = None
        self.dcnt = 0
        self.multi = multi


class K:
    def __init__(self, nc, stack):
        self.nc = nc
        self.stack = stack
        self.E = {"pe": nc.tensor, "act": nc.scalar, "dve": nc.vector,
                  "pool": nc.gpsimd, "sp": nc.sync}
        self.sem = {}
        self.cnt = {}
        self.seen = {}
        for e in self.E:
            self.sem[e] = stack.enter_context(nc.semaphore("s_" + e))
            self.cnt[e] = 0
            self.seen[e] = {}
        self.nsem = len(self.E)
        self.out_tokens = []
        self.nbuf = 0
        self.bufs = []

    def buf(self, name=None, multi=False):
        self.nbuf += 1
        b = Buf(name or f"b{self.nbuf}", multi)
        self.bufs.append(b)
        return b

    def barrier(self):
        toks = [(self.sem[e], self.cnt[e]) for e in self.E if self.cnt[e] > 0]
        for b in self.bufs:
            toks += b.w
            toks += b.r
        toks = self._compress(toks)
        for e in self.E:
            self._wait(e, toks)
        self.bufs = [b for b in self.bufs if b.multi or b.name.startswith("const") or b.name.startswith("bank")]

    def _wait(self, e, toks, hazard=()):
        own = self.sem.get(e)
        best = {}
        for (s, v) in toks:
            if e == "pe" and s is own:
                continue
            k = id(s)
            if k not in best or best[k][1] < v:
                best[k] = (s, v)
        for (s, v) in hazard:
            if s is own:
                continue
            k = id(s)
            if k not in best or best[k][1] < v:
                best[k] = (s, v)
        seen = self.seen[e]
        for k, (s, v) in best.items():
            if seen.get(k, 0) >= v:
                continue
            self.E[e].wait_ge(s, v)
            seen[k] = v

    @staticmethod
    def _compress(toks):
        best = {}
        for (s, v) in toks:
            k = id(s)
            if k not in best or best[k][1] < v:
                best[k] = (s, v)
        return list(best.values())

    def _deps(self, reads, writes):
        raw, haz = [], []
        for b in reads:
            raw += b.w
        for b in writes:
            haz += b.w
            haz += b.r
        return raw, haz

    def op(self, e, fn, reads=(), writes=(), inc=True):
        raw, haz = self._deps(reads, writes)
        self._wait(e, raw, haz)
        ins = fn(self.E[e])
        tok = (self.sem[e], self.cnt[e] + 1)
        if inc:
            ins.then_inc(self.sem[e], 1)
            self.cnt[e] += 1
        for b in reads:
            b.r.append(tok)
            if len(b.r) > 16:
                b.r = self._compress(b.r)
        for b in writes:
            b.w = [tok]
            b.r = []
        return ins

    def _dsem(self, b):
        if b.dsem is None:
            b.dsem = self.stack.enter_context(self.nc.semaphore(f"d{self.nsem}"))
            b.dcnt = 0
            self.nsem += 1
        return b.dsem

    def dma(self, q, out, in_, reads=(), write=None, is_output=False, **kw):
        raw, haz = self._deps(reads, [] if write.multi else [write])
        if write.multi:
            haz = haz + write.r
        self._wait(q, raw, haz)
        s = self._dsem(write)
        write.dcnt += 16
        ins = self.E[q].dma_start(out=out, in_=in_, **kw)
        ins.then_inc(s, 16)
        tok = (s, write.dcnt)
        for b in reads:
            b.r.append(tok)
            if len(b.r) > 16:
                b.r = self._compress(b.r)
        write.w = [tok]
        if not write.multi:
            write.r = []
        if is_output:
            self.out_tokens.append(tok)
        return ins

    def finish(self):
        self._wait("sp", self.out_tokens)


class Ring:
    def __init__(self, items):
        self.items = items
        self.i = 0

    def get(self):
        it = self.items[self.i % len(self.items)]
        self.i += 1
        return it


def split_tiles(lo, hi, tmax, smax=512):
    n = hi - lo
    nt = (n + tmax - 1) // tmax
    base = (n + nt - 1) // nt
    base = ((base + 31) // 32) * 32
    tiles = []
    p = lo
    while p < hi:
        ln = min(base, hi - p)
        subs = []
        q = 0
        while q < ln:
            sl = min(smax, ln - q)
            subs.append((q, sl))
            q += sl
        tiles.append((p, ln, subs))
        p += ln
    return tiles


def build(TPRE, TOWN, debug=False, phases=None, scratch_in=()):
    TALL = TPRE + TOWN
    nc = bass.Bass("TRN2", target_bir_lowering=False)
    ALLP = ["norm1", "inproj", "zdt", "ssd", "scpack", "outproj", "norm2", "qg", "gemm1", "gemm2", "final"]
    phases = set(ALLP) if phases is None else set(phases)

    _din = {}

    def din(name, shape, dt=F32):
        if name not in _din:
            _din[name] = nc.dram_tensor(name, list(shape), dt, kind="ExternalInput").ap()
        return _din[name]

    def dscr(name, shape, dt=F32):
        if name in scratch_in:
            kind = "ExternalInput"
        else:
            kind = "ExternalOutput" if debug else "Internal"
        return nc.dram_tensor(name, list(shape), dt, kind=kind).ap()

    class _Lazy:
        def __init__(self, name, shape, dt=F32):
            self.a = (name, shape, dt)

        def __call__(self):
            return din(*self.a)

    I = dict(
        x_all=_Lazy("x_all", [TALL, D]), flag=_Lazy("flag", [128, 1]),
        w_fm=_Lazy("w_fm", [NFM, 128, KC * 128]), w_z=_Lazy("w_z", [8, 128, KC * 512]),
        w_dt=_Lazy("w_dt", [128, KC * 64]), cw_xbc=_Lazy("cw_xbc", [128, 48, 4]),
        cb_xbc=_Lazy("cb_xbc", [128, 48]), cw_sc=_Lazy("cw_sc", [128, 32, 3]),
        g_mix=_Lazy("g_mix", [D]), g_ffn=_Lazy("g_ffn", [D]), g_fin=_Lazy("g_fin", [D]),
        g_ssd=_Lazy("g_ssd", [D]), g_sc=_Lazy("g_sc", [128, 32]),
        dt_bias=_Lazy("dt_bias", [NH]), a_log=_Lazy("a_log", [NH]), d_skip=_Lazy("d_skip", [NH]),
        w_out=_Lazy("w_out", [8, 2, 128, 32 * 512]), w_q=_Lazy("w_q", [16, 128, KC * 128]),
        keys_t=_Lazy("keys_t", [16, 128, 128]), u_t=_Lazy("u_t", [128, 128, KC * 128]),
        v_nat=_Lazy("v_nat", [128 * 128, D]), c_ident=_Lazy("c_ident", [128, 128]),
        c_triu=_Lazy("c_triu", [128, 128]), c_negm=_Lazy("c_negm", [128, 512]),
        c_iota=_Lazy("c_iota", [128, 128]),
    )

    out = nc.dram_tensor("out", [TOWN, D], F32, kind="ExternalOutput").ap()

    xnT_d = dscr("xnT_d", [128, KC, TALL], BF16)
    xbcT_d = dscr("xbcT_d", [48 * 128, TALL])
    ysc_d = dscr("ysc_d", [32 * 128, TOWN + HALO])
    rstd_sc_d = dscr("rstd_sc_d", [128, TOWN + HALO])
    zs_d = dscr("zs_d", [TOWN, D])
    dtr_d = dscr("dtr_d", [TALL, NH])
    yT_d = dscr("yT_d", [2 * D, TOWN], BF16)
    h1_d = dscr("h1_d", [TOWN, D])
    xn2T_d = dscr("xn2T_d", [128, KC, TOWN], BF16)
    G_d = dscr("G_d", [128, 128, TOWN], BF16)
    HG_d = dscr("HG_d", [128 * 128, TOWN], BF16)
    h2_d = dscr("h2_d", [TOWN, D])
    vb_d = dscr("vb_d", [128 * 128, D], BF16)

    with ExitStack() as top:
        k = K(nc, top)
        B_xnT = k.buf("xnT_d", multi=True)
        B_xbcT = k.buf("xbcT_d", multi=True)
        B_ysc = k.buf("ysc_d", multi=True)
        B_rstdsc = k.buf("rstd_sc_d", multi=True)
        B_zs = k.buf("zs_d", multi=True)
        B_dtr = k.buf("dtr_d", multi=True)
        B_out = k.buf("out", multi=True)
        B_yT = k.buf("yT_d", multi=True)
        B_h1 = k.buf("h1_d", multi=True)
        B_xn2T = k.buf("xn2T_d", multi=True)
        B_G = k.buf("G_d", multi=True)
        B_HG = k.buf("HG_d", multi=True)
        B_h2 = k.buf("h2_d", multi=True)
        B_vb = k.buf("vb_d", multi=True)

        def bcast_row(vec_ap, n):
            return bass.AP(vec_ap.tensor, vec_ap.offset, [[0, 128], [1, n]])

        def sb(st, name, shape, dt):
            return st.enter_context(nc.sbuf_tensor(name, list(shape), dt))

        ident_f = sb(top, "ident_f", [128, 128], F32)
        ident_b = sb(top, "ident_b", [128, 128], BF16)
        ones_f = sb(top, "ones_f", [128, 128], F32)
        B_const = k.buf("const")
        k.dma("sp", ident_f[:], I["c_ident"](), write=B_const)
        B_c2 = k.buf("const2")
        k.dma("pool", ident_b[:], I["c_ident"](), write=B_c2)
        B_c3 = k.buf("const3")
        k.op("dve", lambda e: e.memset(ones_f[:], 1.0), writes=[B_c3])
        CONST = [B_const, B_c2, B_c3]

        vb_pieces = [r_ for r_ in range(0, 128 * 128, 256)] if "gemm2" in phases else []

        def vb_piece():
            if vb_pieces:
                r_ = vb_pieces.pop(0)
                k.dma("pool", vb_d[r_:r_ + 256, :], I["v_nat"]()[r_:r_ + 256, :], write=B_vb)

        if "inproj" not in phases:
            while vb_pieces:
                vb_piece()

        banks = []
        for i in range(8):
            t = top.enter_context(nc.psum_tensor(f"bank{i}", [128, 512], F32))
            banks.append((t, k.buf(f"bank{i}")))
        psum = Ring(banks)

        def phase_norm(src_d, nrows, gain_d, dstT_d, B_dst, tag, src_dep=None):
            k.barrier()
            with ExitStack() as st:
                gain = sb(st, tag + "gain", [128, D], F32)
                Bg = k.buf()
                k.dma("sp", gain[:], bcast_row(gain_d, D), write=Bg)
                xr = Ring([(sb(st, f"{tag}x{i}", [128, D], F32), k.buf()) for i in range(3)])
                sqr = Ring([(sb(st, f"{tag}sq{i}", [128, D], F32), k.buf()) for i in range(1)])
                xnr = Ring([(sb(st, f"{tag}xn{i}", [128, D], BF16), k.buf()) for i in range(3)])
                str_ = Ring([(sb(st, f"{tag}st{i}", [128, KC, 512], BF16), k.buf()) for i in range(2)])
                smr = Ring([(sb(st, f"{tag}sm{i}", [128, 2], F32), k.buf()) for i in range(3)])
                for t0 in range(0, nrows, 512):
                    stg, Bst = str_.get()
                    for bi in range(4):
                        r0 = t0 + bi * 128
                        xt, Bx = xr.get()
                        k.dma("sp", xt[:], src_d[r0:r0 + 128, :], reads=([src_dep] if src_dep is not None else []), write=Bx)
                        sq, Bsq = sqr.get()
                        sm, Bsm = smr.get()
                        k.op("act", lambda e: e.activation(out=sq[:], in_=xt[:], func=AF.Square,
                                                           accum_out=sm[:, 0:1]),
                             reads=[Bx], writes=[Bsq, Bsm])
                        k.op("act", lambda e: e.activation(out=sm[:, 1:2], in_=sm[:, 0:1], func=AF.Sqrt,
                                                           bias=EPS, scale=1.0 / D),
                             reads=[Bsm], writes=[Bsm])
                        k.op("dve", lambda e: e.reciprocal(out=sm[:, 1:2], in_=sm[:, 1:2]),
                             reads=[Bsm], writes=[Bsm])
                        xn, Bxn = xnr.get()
                        k.op("dve", lambda e: e.scalar_tensor_tensor(
                            out=xn[:], in0=xt[:], scalar=sm[:, 1:2], in1=gain[:],
                            op0=ALU.mult, op1=ALU.mult), reads=[Bx, Bsm, Bg], writes=[Bxn])
                        for grp in range(4):
                            pt, Bp = psum.get()
                            ptb = pt[:].bitcast(BF16).rearrange("p (a b) -> p a b", a=8)
                            for j in range(8):
                                c = grp * 8 + j
                                k.op("pe", lambda e: e.transpose(out=ptb[:, j, :],
                                                                 in_=xn[:, c * 128:(c + 1) * 128],
                                                                 identity=ident_b[:]),
                                     reads=[Bxn, B_c2], writes=[Bp], inc=(j == 7))
                            k.op("act", lambda e: e.copy(
                                out=stg[:, grp * 8:(grp + 1) * 8, bi * 128:(bi + 1) * 128], in_=ptb),
                                 reads=[Bp], writes=[Bst])
                    k.dma("sp", dstT_d[:, :, t0:t0 + 512], stg[:], reads=[Bst], write=B_dst)

        if "norm1" in phases:
            phase_norm(I["x_all"](), TALL, I["g_mix"](), xnT_d, B_xnT, "n1")

        def phase_inproj_fm():
            k.barrier()
            with ExitStack() as st:
                TMAX = 1056
                cwx = sb(st, "cwx", [128, 48, 4], F32)
                cbx = sb(st, "cbx", [128, 48], F32)
                cws = sb(st, "cws", [128, 32, 3], F32)
                Bcw = k.buf()
                k.dma("sp", cwx[:], I["cw_xbc"](), write=Bcw)
                Bcb = k.buf()
                k.dma("sp", cbx[:], I["cb_xbc"](), write=Bcb)
                Bcs = k.buf()
                k.dma("sp", cws[:], I["cw_sc"](), write=Bcs)
                hs = sb(st, "hs", [128, 112, 4], F32)
                Bhs = k.buf()
                k.op("dve", lambda e: e.memset(hs[:], 0.0), writes=[Bhs])
                xT = sb(st, "ipxT", [128, KC, TMAX], BF16)
                BxT = k.buf()
                wr = Ring([(sb(st, f"ipw{i}", [128, KC, 128], BF16), k.buf()) for i in range(3)])
                pr = Ring([(sb(st, f"ipP{i}", [128, 4 + TMAX], F32), k.buf()) for i in range(2)])
                ar = Ring([(sb(st, f"ipA{i}", [128, TMAX], F32), k.buf()) for i in range(2)])
                orr = Ring([(sb(st, f"ipO{i}", [128, TMAX], F32), k.buf()) for i in range(2)])
                csr = Ring([(sb(st, f"ipC{i}", [128, TMAX], F32), k.buf()) for i in range(1)])
                ssq = sb(st, "ipssq", [128, TMAX], F32)
                Bssq = k.buf()
                sqt = sb(st, "ipsqt", [128, TMAX], F32)
                Bsqt = k.buf()

                ncall = [0]

                def gemm_chunk(j, ln, subs):
                    wt, Bw = wr.get()
                    k.dma("pool", wt[:].rearrange("p c n -> p (c n)"), I["w_fm"]()[j], write=Bw)
                    ncall[0] += 1
                    if ncall[0] % 4 == 0:
                        vb_piece()
                    res = []
                    for (so, sl) in subs:
                        pt, Bp = psum.get()
                        for c in range(KC):
                            k.op("pe", lambda e: e.matmul(pt[:, 0:sl], lhsT=wt[:, c, :],
                                                          rhs=xT[:, c, so:so + sl],
                                                          start=(c == 0), stop=(c == KC - 1)),
                                 reads=[Bw, BxT], writes=[Bp], inc=(c == KC - 1))
                        res.append((pt, Bp, so, sl))
                    return res

                def conv(P, BP, A, BA, ln, taps, wtile, Bwt, ci, eng):
                    base = 4 - (taps - 1)
                    k.op(eng, lambda e: e.tensor_scalar(out=A[:, 0:ln], in0=P[:, base:base + ln],
                                                        scalar1=wtile[:, ci, 0:1], scalar2=None,
                                                        op0=ALU.mult),
                         reads=[BP, Bwt], writes=[BA])
                    for t in range(1, taps):
                        k.op(eng, lambda e: e.scalar_tensor_tensor(
                            out=A[:, 0:ln], in0=P[:, base + t:base + t + ln], scalar=wtile[:, ci, t:t + 1],
                            in1=A[:, 0:ln], op0=ALU.mult, op1=ALU.add),
                             reads=[BP, Bwt, BA], writes=[BA])

                def run_tile(t0, ln, subs, chunks, own):
                    k.dma("sp", xT[:, :, 0:ln], xnT_d[:, :, t0:t0 + ln], reads=[B_xnT], write=BxT)
                    for j in chunks:
                        res = gemm_chunk(j, ln, subs)
                        P, BP = pr.get()
                        k.op("dve", lambda e: e.tensor_copy(out=P[:, 0:4], in_=hs[:, j, :]),
                             reads=[Bhs], writes=[BP])
                        for (pt, Bp, so, sl) in res:
                            k.op("act", lambda e: e.copy(out=P[:, 4 + so:4 + so + sl], in_=pt[:, 0:sl]),
                                 reads=[Bp], writes=[BP])
                        k.op("dve", lambda e: e.tensor_copy(out=hs[:, j, :], in_=P[:, ln:ln + 4]),
                             reads=[BP], writes=[Bhs])
                        A, BA = ar.get()
                        conv(P, BP, A, BA, ln, 4, cwx, Bcw, j, "dve")
                        O, BO = orr.get()
                        k.op("act", lambda e: e.activation(out=O[:, 0:ln], in_=A[:, 0:ln], func=AF.Silu,
                                                           bias=cbx[:, j:j + 1], scale=1.0),
                             reads=[BA, Bcb], writes=[BO])
                        k.dma("sp", xbcT_d[j * 128:(j + 1) * 128, t0:t0 + ln], O[:, 0:ln],
                              reads=[BO], write=B_xbcT)
                    if not own:
                        return
                    o0 = t0 - (TPRE - HALO)
                    for i in range(32):
                        res_c = gemm_chunk(48 + 32 + i, ln, subs)
                        Cs, BCs = csr.get()
                        for (pt, Bp, so, sl) in res_c:
                            k.op("act", lambda e: e.copy(out=Cs[:, so:so + sl], in_=pt[:, 0:sl]),
                                 reads=[Bp], writes=[BCs])
                        res_h = gemm_chunk(48 + 64 + i, ln, subs)
                        P, BP = pr.get()
                        k.op("dve", lambda e: e.tensor_copy(out=P[:, 0:4], in_=hs[:, 48 + i, :]),
                             reads=[Bhs], writes=[BP])
                        for (pt, Bp, so, sl) in res_h:
                            k.op("dve", lambda e: e.tensor_tensor(out=P[:, 4 + so:4 + so + sl],
                                                                  in0=pt[:, 0:sl], in1=Cs[:, so:so + sl],
                                                                  op=ALU.mult),
                                 reads=[Bp, BCs], writes=[BP])
                        k.op("dve", lambda e: e.tensor_copy(out=hs[:, 48 + i, :], in_=P[:, ln:ln + 4]),
                             reads=[BP], writes=[Bhs])
                        A, BA = ar.get()
                        conv(P, BP, A, BA, ln, 3, cws, Bcs, i, "dve")
                        res_b = gemm_chunk(48 + i, ln, subs)
                        O, BO = orr.get()
                        for (pt, Bp, so, sl) in res_b:
                            k.op("dve", lambda e: e.tensor_tensor(out=O[:, so:so + sl], in0=pt[:, 0:sl],
                                                                  in1=A[:, so:so + sl], op=ALU.mult),
                                 reads=[Bp, BA], writes=[BO])
                        k.dma("sp", ysc_d[i * 128:(i + 1) * 128, o0:o0 + ln], O[:, 0:ln],
                              reads=[BO], write=B_ysc)
                        if i == 0:
                            k.op("act", lambda e: e.activation(out=ssq[:, 0:ln], in_=O[:, 0:ln],
                                                               func=AF.Square),
                                 reads=[BO], writes=[Bssq])
                        else:
                            k.op("act", lambda e: e.activation(out=sqt[:, 0:ln], in_=O[:, 0:ln],
                                                               func=AF.Square),
                                 reads=[BO], writes=[Bsqt])
                            k.op("pool", lambda e: e.tensor_tensor(out=ssq[:, 0:ln], in0=ssq[:, 0:ln],
                                                                   in1=sqt[:, 0:ln], op=ALU.add),
                                 reads=[Bsqt, Bssq], writes=[Bssq])
                    O, BO = orr.get()
                    for (so, sl) in subs:
                        pt, Bp = psum.get()
                        k.op("pe", lambda e: e.matmul(pt[:, 0:sl], lhsT=ones_f[:], rhs=ssq[:, so:so + sl],
                                                      start=True, stop=True),
                             reads=[Bssq, B_c3], writes=[Bp])
                        k.op("act", lambda e: e.activation(out=O[:, so:so + sl], in_=pt[:, 0:sl],
                                                           func=AF.Sqrt, bias=EPS, scale=1.0 / D),
                             reads=[Bp], writes=[BO])
                    k.op("dve", lambda e: e.reciprocal(out=O[:, 0:ln], in_=O[:, 0:ln]),
                         reads=[BO], writes=[BO])
                    k.dma("sp", rstd_sc_d[:, o0:o0 + ln], O[:, 0:ln], reads=[BO], write=B_rstdsc)

                if TPRE > HALO:
                    for (t0, ln, subs) in split_tiles(0, TPRE - HALO, TMAX):
                        run_tile(t0, ln, subs, list(range(0, 40)), False)
                for (t0, ln, subs) in split_tiles(TPRE - HALO, TALL, TMAX):
                    run_tile(t0, ln, subs, list(range(0, 48)), True)

        if "inproj" in phases:
            phase_inproj_fm()
        while vb_pieces:
            vb_piece()

        def phase_zdt():
            k.barrier()
            with ExitStack() as st:
                wdt = sb(st, "zwdt", [128, KC, 64], BF16)
                Bwdt = k.buf()
                k.dma("pool", wdt[:].rearrange("p c n -> p (c n)"), I["w_dt"](), write=Bwdt)
                xr = Ring([(sb(st, f"zx{i}", [128, KC, 512], BF16), k.buf()) for i in range(2)])
                wr = Ring([(sb(st, f"zw{i}", [128, KC, 512], BF16), k.buf()) for i in range(2)])
                orr = Ring([(sb(st, f"zo{i}", [128, 512], F32), k.buf()) for i in range(3)])
                dr = Ring([(sb(st, f"zd{i}", [128, 64], F32), k.buf()) for i in range(2)])
                for t0 in range(0, TALL, 512):
                    xT, BxT = xr.get()
                    k.dma("sp", xT[:], xnT_d[:, :, t0:t0 + 512], reads=[B_xnT], write=BxT)
                    for blk in range(4):
                        pt, Bp = psum.get()
                        for c in range(KC):
                            k.op("pe", lambda e: e.matmul(pt[:, 0:64], lhsT=xT[:, c, blk * 128:(blk + 1) * 128],
                                                          rhs=wdt[:, c, :], start=(c == 0), stop=(c == KC - 1)),
                                 reads=[BxT, Bwdt], writes=[Bp], inc=(c == KC - 1))
                        dd, Bd = dr.get()
                        k.op("act", lambda e: e.copy(out=dd[:], in_=pt[:, 0:64]), reads=[Bp], writes=[Bd])
                        k.dma("sp", dtr_d[t0 + blk * 128:t0 + (blk + 1) * 128, :], dd[:], reads=[Bd], write=B_dtr)
                    if t0 < TPRE:
                        continue
                    o0 = t0 - TPRE
                    for g in range(8):
                        wz, Bwz = wr.get()
                        k.dma("pool", wz[:].rearrange("p c n -> p (c n)"), I["w_z"]()[g], write=Bwz)
                        for blk in range(4):
                            pt, Bp = psum.get()
                            for c in range(KC):
                                k.op("pe", lambda e: e.matmul(pt[:], lhsT=xT[:, c, blk * 128:(blk + 1) * 128],
                                                              rhs=wz[:, c, :], start=(c == 0), stop=(c == KC - 1)),
                                     reads=[BxT, Bwz], writes=[Bp], inc=(c == KC - 1))
                            oo, Bo = orr.get()
                            k.op("act", lambda e: e.activation(out=oo[:], in_=pt[:], func=AF.Silu),
                                 reads=[Bp], writes=[Bo])
                            k.dma("sp", zs_d[o0 + blk * 128:o0 + (blk + 1) * 128, g * 512:(g + 1) * 512], oo[:],
                                  reads=[Bo], write=B_zs)

        if "zdt" in phases:
            phase_zdt()

        def phase_ssd():
            k.barrier()
            with ExitStack() as st:
                triu_f = sb(st, "s_triu", [128, 128], F32)
                negm_f = sb(st, "s_negm", [128, 512], F32)
                gssd = sb(st, "s_gssd", [128, D], F32)
                abc = sb(st, "s_abc", [128, NH], F32)
                dtb = sb(st, "s_dtb", [128, NH], F32)
                dsk = sb(st, "s_dsk", [128, NH], F32)
                flg = sb(st, "s_flag", [128, 1], F32)
                Bc = k.buf()
                k.dma("sp", triu_f[:], I["c_triu"](), write=Bc)
                Bc1 = k.buf()
                k.dma("sp", negm_f[:], I["c_negm"](), write=Bc1)
                Bg = k.buf()
                k.dma("sp", gssd[:], bcast_row(I["g_ssd"](), D), write=Bg)
                Ba = k.buf()
                k.dma("sp", abc[:], bcast_row(I["a_log"](), NH), write=Ba)
                k.op("act", lambda e: e.activation(out=abc[:], in_=abc[:], func=AF.Exp), reads=[Ba], writes=[Ba])
                k.op("dve", lambda e: e.tensor_scalar(out=abc[:], in0=abc[:], scalar1=-1.0, scalar2=None, op0=ALU.mult),
                     reads=[Ba], writes=[Ba])
                Bdb = k.buf()
                k.dma("sp", dtb[:], bcast_row(I["dt_bias"](), NH), write=Bdb)
                Bds = k.buf()
                k.dma("sp", dsk[:], bcast_row(I["d_skip"](), NH), write=Bds)
                Bfl = k.buf()
                k.dma("sp", flg[:], I["flag"](), write=Bfl)

                S = sb(st, "s_S", [128, D], F32)
                S_bf = sb(st, "s_Sbf", [128, D], BF16)
                BS = k.buf()
                BSb = k.buf()
                k.op("dve", lambda e: e.memset(S[:], 0.0), writes=[BS])
                k.op("pool", lambda e: e.memset(S_bf[:], 0.0), writes=[BSb])

                xin = Ring([(sb(st, f"s_xin{i}", [128, 32, 128], F32), k.buf()) for i in range(1)])
                bin_ = Ring([(sb(st, f"s_bin{i}", [128, 8, 128], F32), k.buf()) for i in range(1)])
                cin = Ring([(sb(st, f"s_cin{i}", [128, 8, 128], F32), k.buf()) for i in range(1)])
                dtin = Ring([(sb(st, f"s_dtin{i}", [128, NH], F32), k.buf()) for i in range(2)])
                zin = Ring([(sb(st, f"s_zin{i}", [128, D], F32), k.buf()) for i in range(2)])
                xdt_r = Ring([(sb(st, f"s_xdt{i}", [128, D], BF16), k.buf()) for i in range(2)])
                xdtw_r = Ring([(sb(st, f"s_xdtw{i}", [128, D], BF16), k.buf()) for i in range(2)])
                xD_r = Ring([(sb(st, f"s_xD{i}", [128, D], BF16), k.buf()) for i in range(2)])
                Btm_r = Ring([(sb(st, f"s_Btm{i}", [128, 1024], BF16), k.buf()) for i in range(2)])
                BTb_r = Ring([(sb(st, f"s_BTb{i}", [128, 8, 128], BF16), k.buf()) for i in range(2)])
                CTb_r = Ring([(sb(st, f"s_CTb{i}", [128, 8, 128], BF16), k.buf()) for i in range(2)])
                sm_r = Ring([(sb(st, f"s_sm{i}", [128, 10, NH], F32), [k.buf() for _ in range(10)]) for i in range(2)])
                rhsg = Ring([(sb(st, f"s_rhsg{i}", [128, 8, 128], F32), k.buf()) for i in range(2)])
                Er = Ring([(sb(st, f"s_E{i}", [128, 8, 128], F32), k.buf()) for i in range(2)])
                MTr = Ring([(sb(st, f"s_MT{i}", [128, 8, 128], BF16), k.buf()) for i in range(2)])
                cbr = Ring([(sb(st, f"s_cb{i}", [128, 128], F32), k.buf()) for i in range(2)])
                tmpr = Ring([(sb(st, f"s_tmp{i}", [128, 512], F32), k.buf()) for i in range(2)])
                ygr = Ring([(sb(st, f"s_yg{i}", [128, 512], F32), k.buf()) for i in range(2)])
                sqr = Ring([(sb(st, f"s_sq{i}", [128, 512], F32), k.buf()) for i in range(1)])
                ynr = Ring([(sb(st, f"s_yn{i}", [128, 512], BF16), k.buf()) for i in range(2)])
                ssr = Ring([(sb(st, f"s_ss{i}", [128, 2], F32), k.buf()) for i in range(2)])
                yTs = Ring([(sb(st, f"s_yT{i}", [128, 32, 128], BF16), k.buf()) for i in range(1)])

                xrows = xbcT_d[0:4096, :].rearrange("(c p) t -> p c t", p=128)
                brows = xbcT_d[4096:5120, :].rearrange("(c p) t -> p c t", p=128)
                crows = xbcT_d[5120:6144, :].rearrange("(c p) t -> p c t", p=128)
                yrows = yT_d[0:4096, :].rearrange("(c p) t -> p c t", p=128)

                def h3(ap_, g):
                    return ap_[:, 8 * g:8 * g + 8].unsqueeze(2).to_broadcast([128, 8, 64])

                nchunks = TALL // 128
                def chunk_gen(ci):
                    xdt, Bxdt = xdt_r.get()
                    xdtw, Bxdtw = xdtw_r.get()
                    xD, BxD = xD_r.get()
                    Btm, BBtm = Btm_r.get()
                    BTb, BBTb = BTb_r.get()
                    CTb, BCTb = CTb_r.get()
                    sm, Bsm = sm_r.get()
                    t0 = ci * 128
                    own = t0 >= TPRE
                    need_sbf = (t0 + 128 >= TPRE) and (ci + 1 < nchunks)
                    xi, Bxi = xin.get()
                    k.dma("sp", xi[:], xrows[:, :, t0:t0 + 128], reads=[B_xbcT], write=Bxi)
                    bi_, Bbi = bin_.get()
                    k.dma("sp", bi_[:], brows[:, :, t0:t0 + 128], reads=[B_xbcT], write=Bbi)
                    dti, Bdti = dtin.get()
                    k.dma("sp", dti[:], dtr_d[t0:t0 + 128, :], reads=[B_dtr], write=Bdti)
                    if own:
                        ci_, Bci = cin.get()
                        k.dma("sp", ci_[:], crows[:, :, t0:t0 + 128], reads=[B_xbcT], write=Bci)
                        zi, Bzi = zin.get()
                        k.dma("sp", zi[:], zs_d[t0 - TPRE:t0 - TPRE + 128, :], reads=[B_zs], write=Bzi)
                    v, ab, mx, dt_, a_ = (sm[:, i, :] for i in range(5))
                    k.op("dve", lambda e: e.tensor_tensor(out=v, in0=dti[:], in1=dtb[:], op=ALU.add),
                         reads=[Bdti, Bdb], writes=[Bsm[0]])
                    k.op("act", lambda e: e.activation(out=ab, in_=v, func=AF.Abs),
                         reads=[Bsm[0]], writes=[Bsm[1]])
                    k.op("act", lambda e: e.activation(out=ab, in_=ab, func=AF.Exp, scale=-1.0),
                         reads=[Bsm[1]], writes=[Bsm[1]])
                    k.op("act", lambda e: e.activation(out=ab, in_=ab, func=AF.Ln, bias=1.0, scale=1.0),
                         reads=[Bsm[1]], writes=[Bsm[1]])
                    k.op("dve", lambda e: e.tensor_scalar_max(out=mx, in0=v, scalar1=0.0),
                         reads=[Bsm[0]], writes=[Bsm[2]])
                    k.op("dve", lambda e: e.tensor_tensor(out=dt_, in0=mx, in1=ab, op=ALU.add),
                         reads=[Bsm[1], Bsm[2]], writes=[Bsm[3]])
                    k.op("dve", lambda e: e.tensor_tensor(out=a_, in0=dt_, in1=abc[:], op=ALU.mult),
                         reads=[Bsm[3], Ba], writes=[Bsm[4]])
                    pA, BpA = psum.get()
                    k.op("pe", lambda e: e.matmul(pA[:, 0:64], lhsT=triu_f[:], rhs=a_, start=True, stop=True),
                         reads=[Bc, Bsm[4]], writes=[BpA])
                    k.op("pe", lambda e: e.matmul(pA[:, 64:128], lhsT=ones_f[:], rhs=a_, start=True, stop=True),
                         reads=[B_c3, Bsm[4]], writes=[BpA])
                    acum, nacum, eacum, toend, dA = (sm[:, i, :] for i in range(5, 10))
                    k.op("act", lambda e: e.copy(out=acum, in_=pA[:, 0:64]), reads=[BpA], writes=[Bsm[5]])
                    k.op("act", lambda e: e.mul(out=nacum, in_=pA[:, 0:64], mul=-1.0), reads=[BpA], writes=[Bsm[6]])
                    if own:
                        k.op("act", lambda e: e.activation(out=eacum, in_=pA[:, 0:64], func=AF.Exp),
                             reads=[BpA], writes=[Bsm[7]])
                    k.op("dve", lambda e: e.tensor_tensor(out=toend, in0=pA[:, 64:128], in1=acum, op=ALU.subtract),
                         reads=[BpA, Bsm[5]], writes=[Bsm[8]])
                    k.op("act", lambda e: e.activation(out=toend, in_=toend, func=AF.Exp),
                         reads=[Bsm[8]], writes=[Bsm[8]])
                    k.op("act", lambda e: e.activation(out=dA, in_=pA[:, 64:128], func=AF.Exp),
                         reads=[BpA], writes=[Bsm[9]])
                    for g in range(NG):
                        pt, Bp = psum.get()
                        for j in range(4):
                            k.op("pe", lambda e: e.transpose(out=pt[:, j * 128:(j + 1) * 128], in_=xi[:, 4 * g + j, :],
                                                             identity=ident_f[:]),
                                 reads=[Bxi, B_const], writes=[Bp], inc=(j == 3))
                        p3 = pt[:].rearrange("p (h d) -> p h d", h=8)
                        k.op("dve", lambda e: e.tensor_tensor(
                            out=xdt[:, 512 * g:512 * (g + 1)].rearrange("p (h d) -> p h d", h=8),
                            in0=p3, in1=h3(dt_, g), op=ALU.mult), reads=[Bp, Bsm[3]], writes=[Bxdt])
                        if own:
                            k.op("dve", lambda e: e.tensor_tensor(
                                out=xD[:, 512 * g:512 * (g + 1)].rearrange("p (h d) -> p h d", h=8),
                                in0=p3, in1=h3(dsk[:], g), op=ALU.mult), reads=[Bp, Bds], writes=[BxD])
                        k.op("pool", lambda e: e.tensor_tensor(
                            out=xdtw[:, 512 * g:512 * (g + 1)].rearrange("p (h d) -> p h d", h=8),
                            in0=xdt[:, 512 * g:512 * (g + 1)].rearrange("p (h d) -> p h d", h=8),
                            in1=h3(toend, g), op=ALU.mult), reads=[Bxdt, Bsm[8]], writes=[Bxdtw])
                    for half in range(2):
                        pt, Bp = psum.get()
                        for j in range(4):
                            k.op("pe", lambda e: e.transpose(out=pt[:, j * 128:(j + 1) * 128], in_=bi_[:, 4 * half + j, :],
                                                             identity=ident_f[:]),
                                 reads=[Bbi, B_const], writes=[Bp], inc=(j == 3))
                        k.op("act", lambda e: e.copy(out=Btm[:, 512 * half:512 * (half + 1)], in_=pt[:]),
                             reads=[Bp], writes=[BBtm])
                    if own:
                        k.op("act", lambda e: e.copy(out=BTb[:], in_=bi_[:]), reads=[Bbi], writes=[BBTb])
                        k.op("pool", lambda e: e.tensor_copy(out=CTb[:], in_=ci_[:]), reads=[Bci], writes=[BCTb])
                        yT, ByT = yTs.get()
                    yield

                    def front(g):
                        gs = slice(512 * g, 512 * (g + 1))
                        pc, Bpc = psum.get()
                        k.op("pe", lambda e: e.matmul(pc[:, 0:128], lhsT=BTb[:, g, :], rhs=CTb[:, g, :],
                                                      start=True, stop=True),
                             reads=[BBTb, BCTb], writes=[Bpc])
                        cb, Bcb = cbr.get()
                        k.op("act", lambda e: e.copy(out=cb[:], in_=pc[:, 0:128]), reads=[Bpc], writes=[Bcb])
                        rg, Brg = rhsg.get()
                        k.op("dve", lambda e: e.tensor_tensor(
                            out=rg[:], in0=triu_f[:].unsqueeze(1).to_broadcast([128, 8, 128]),
                            in1=a_[:, 8 * g:8 * g + 8].unsqueeze(2).to_broadcast([128, 8, 128]), op=ALU.mult),
                             reads=[Bc, Bsm[4]], writes=[Brg])
                        E, BE = Er.get()
                        for j in range(2):
                            pseg, Bps = psum.get()
                            k.op("pe", lambda e: e.matmul(
                                pseg[:], lhsT=ones_f[:], rhs=rg[:, 4 * j:4 * j + 4, :].rearrange("p h t -> p (h t)"),
                                start=True, stop=False), reads=[B_c3, Brg], writes=[Bps], inc=False)
                            k.op("pe", lambda e: e.matmul(pseg[:], lhsT=ident_f[:], rhs=negm_f[:],
                                                          start=False, stop=True),
                                 reads=[B_const, Bc1], writes=[Bps])
                            for hh in range(4):
                                h = 8 * g + 4 * j + hh
                                k.op("act", lambda e: e.activation(
                                    out=E[:, 4 * j + hh, :], in_=pseg[:, hh * 128:(hh + 1) * 128], func=AF.Exp,
                                    bias=nacum[:, h:h + 1], scale=1.0), reads=[Bps, Bsm[6]], writes=[BE])
                        MT, BMT = MTr.get()
                        k.op("dve", lambda e: e.tensor_tensor(
                            out=MT[:], in0=E[:], in1=cb[:].unsqueeze(1).to_broadcast([128, 8, 128]), op=ALU.mult),
                             reads=[BE, Bcb], writes=[BMT])
                        return MT, BMT

                    def back(g, MT, BMT):
                        gs = slice(512 * g, 512 * (g + 1))
                        if own:
                            py, Bpy = psum.get()
                            k.op("pe", lambda e: e.matmul(py[:], lhsT=ident_b[:], rhs=xD[:, gs], start=True, stop=False),
                                 reads=[B_c2, BxD], writes=[Bpy], inc=False)
                            for hh in range(8):
                                k.op("pe", lambda e: e.matmul(py[:, 64 * hh:64 * (hh + 1)], lhsT=MT[:, hh, :],
                                                              rhs=xdt[:, 512 * g + 64 * hh:512 * g + 64 * (hh + 1)],
                                                              start=False, stop=(hh == 7)),
                                     reads=[BMT, Bxdt], writes=[Bpy], inc=(hh == 7))
                            po, Bpo = psum.get()
                            k.op("pe", lambda e: e.matmul(po[:], lhsT=CTb[:, g, :], rhs=S_bf[:, gs], start=True, stop=True),
                                 reads=[BCTb, BSb], writes=[Bpo])
                            tmp, Btmp = tmpr.get()
                            k.op("dve", lambda e: e.tensor_tensor(
                                out=tmp[:].rearrange("p (h d) -> p h d", h=8),
                                in0=po[:].rearrange("p (h d) -> p h d", h=8), in1=h3(eacum, g), op=ALU.mult),
                                 reads=[Bpo, Bsm[7]], writes=[Btmp])
                            yg, Byg = ygr.get()
                            k.op("dve", lambda e: e.tensor_tensor(out=yg[:], in0=py[:], in1=tmp[:], op=ALU.add),
                                 reads=[Bpy, Btmp], writes=[Byg])
                            k.op("pool", lambda e: e.tensor_tensor(out=yg[:], in0=yg[:], in1=zi[:, gs], op=ALU.mult),
                                 reads=[Byg, Bzi], writes=[Byg])
                            sq, Bsq = sqr.get()
                            ss, Bss = ssr.get()
                            k.op("act", lambda e: e.activation(out=sq[:], in_=yg[:], func=AF.Square, accum_out=ss[:, 0:1]),
                                 reads=[Byg], writes=[Bsq, Bss])
                            k.op("act", lambda e: e.activation(out=ss[:, 1:2], in_=ss[:, 0:1], func=AF.Sqrt,
                                                               bias=EPS, scale=1.0 / 512),
                                 reads=[Bss], writes=[Bss])
                            k.op("dve", lambda e: e.reciprocal(out=ss[:, 1:2], in_=ss[:, 1:2]), reads=[Bss], writes=[Bss])
                            yn, Byn = ynr.get()
                            k.op("dve", lambda e: e.scalar_tensor_tensor(
                                out=yn[:], in0=yg[:], scalar=ss[:, 1:2], in1=gssd[:, gs], op0=ALU.mult, op1=ALU.mult),
                                 reads=[Byg, Bss, Bg], writes=[Byn])
                            ptT, BpT = psum.get()
                            ptb = ptT[:].bitcast(BF16).rearrange("p (a b) -> p a b", a=8)
                            for j in range(4):
                                k.op("pe", lambda e: e.transpose(out=ptb[:, j, :], in_=yn[:, 128 * j:128 * (j + 1)],
                                                                 identity=ident_b[:]),
                                     reads=[Byn, B_c2], writes=[BpT], inc=(j == 3))
                            k.op("act", lambda e: e.copy(out=yT[:, 4 * g:4 * g + 4, :], in_=ptb[:, 0:4, :]),
                                 reads=[BpT], writes=[ByT])
                        pu, Bpu = psum.get()
                        k.op("pe", lambda e: e.matmul(pu[:], lhsT=Btm[:, 128 * g:128 * (g + 1)], rhs=xdtw[:, gs],
                                                      start=True, stop=True),
                             reads=[BBtm, Bxdtw], writes=[Bpu])
                        k.op("pool", lambda e: e.tensor_tensor(
                            out=S[:, gs].rearrange("p (h d) -> p h d", h=8),
                            in0=S[:, gs].rearrange("p (h d) -> p h d", h=8), in1=h3(dA, g), op=ALU.mult),
                             reads=[BS, Bsm[9]], writes=[BS])
                        k.op("dve", lambda e: e.tensor_tensor(out=S[:, gs], in0=S[:, gs], in1=pu[:], op=ALU.add),
                             reads=[BS, Bpu], writes=[BS])

                    nxt = front(0) if own else (None, None)
                    for g in range(NG):
                        cur = nxt
                        if own and g + 1 < NG:
                            nxt = front(g + 1)
                        back(g, *cur)
                    if own:
                        o0 = t0 - TPRE
                        k.dma("sp", yrows[:, :, o0:o0 + 128], yT[:], reads=[ByT], write=B_yT)
                    if t0 + 128 == TPRE:
                        k.op("dve", lambda e: e.tensor_scalar(out=S[:], in0=S[:], scalar1=flg[:, 0:1], scalar2=None,
                                                              op0=ALU.mult), reads=[BS, Bfl], writes=[BS])
                    if need_sbf:
                        k.op("act", lambda e: e.copy(out=S_bf[:], in_=S[:]), reads=[BS], writes=[BSb])
                gens = [chunk_gen(ci) for ci in range(nchunks)]
                next(gens[0])
                for ci in range(nchunks):
                    if ci + 1 < nchunks:
                        next(gens[ci + 1])
                    for _ in gens[ci]:
                        pass

        if "ssd" in phases:
            phase_ssd()

        def phase_scpack():
            k.barrier()
            with ExitStack() as st:
                gsc = sb(st, "p_gsc", [128, 32], F32)
                Bg = k.buf()
                k.dma("sp", gsc[:], I["g_sc"](), write=Bg)
                rs = sb(st, "p_rs", [128, TOWN], F32)
                Brs = k.buf()
                k.dma("sp", rs[:], rstd_sc_d[:, HALO:HALO + TOWN], reads=[B_rstdsc], write=Brs)
                yr = Ring([(sb(st, f"p_y{i}", [128, TOWN], F32), k.buf()) for i in range(2)])
                orr = Ring([(sb(st, f"p_o{i}", [128, TOWN], BF16), k.buf()) for i in range(2)])
                for i in range(32):
                    y, By = yr.get()
                    k.dma("sp", y[:], ysc_d[i * 128:(i + 1) * 128, HALO:HALO + TOWN], reads=[B_ysc], write=By)
                    o, Bo = orr.get()
                    k.op("dve", lambda e: e.scalar_tensor_tensor(out=o[:], in0=y[:], scalar=gsc[:, i:i + 1], in1=rs[:],
                                                                 op0=ALU.mult, op1=ALU.mult),
                         reads=[By, Bg, Brs], writes=[Bo])
                    k.dma("sp", yT_d[D + i * 128:D + (i + 1) * 128, :], o[:], reads=[Bo], write=B_yT)

        if "scpack" in phases:
            phase_scpack()

        def phase_outproj():
            k.barrier()
            with ExitStack() as st:
                yt = sb(st, "o_y", [128, 64, 512], BF16)
                Byt = k.buf()
                wr = Ring([(sb(st, f"o_w{i}", [128, 32, 512], BF16), k.buf()) for i in range(3)])
                xr = Ring([(sb(st, f"o_x{i}", [128, 512], F32), k.buf()) for i in range(3)])
                hr = Ring([(sb(st, f"o_h{i}", [128, 512], F32), k.buf()) for i in range(3)])
                yv = yT_d.rearrange("(c p) t -> p c t", p=128)
                x_all = I["x_all"]()
                for o0 in range(0, TOWN, 512):
                    k.dma("sp", yt[:], yv[:, :, o0:o0 + 512], reads=[B_yT], write=Byt)
                    for s in range(8):
                        accs = [psum.get() for _ in range(4)]
                        for half in range(2):
                            w, Bw = wr.get()
                            k.dma("pool", w[:].rearrange("p c n -> p (c n)"), I["w_out"]()[s, half], write=Bw)
                            for blk in range(4):
                                pt, Bp = accs[blk]
                                for c in range(32):
                                    last = (half == 1 and c == 31)
                                    k.op("pe", lambda e: e.matmul(
                                        pt[:], lhsT=yt[:, half * 32 + c, blk * 128:(blk + 1) * 128], rhs=w[:, c, :],
                                        start=(half == 0 and c == 0), stop=last),
                                         reads=[Byt, Bw], writes=[Bp], inc=(c == 31))
                        for blk in range(4):
                            pt, Bp = accs[blk]
                            r0 = o0 + blk * 128
                            xt, Bx = xr.get()
                            k.dma("sp", xt[:], x_all[TPRE + r0:TPRE + r0 + 128, s * 512:(s + 1) * 512], write=Bx)
                            ht, Bh = hr.get()
                            k.op("dve", lambda e: e.tensor_tensor(out=ht[:], in0=pt[:], in1=xt[:], op=ALU.add),
                                 reads=[Bp, Bx], writes=[Bh])
                            k.dma("sp", h1_d[r0:r0 + 128, s * 512:(s + 1) * 512], ht[:], reads=[Bh], write=B_h1)

        if "outproj" in phases:
            phase_outproj()

        if "norm2" in phases:
            phase_norm(h1_d, TOWN, I["g_ffn"](), xn2T_d, B_xn2T, "n2", src_dep=B_h1)

        def phase_qg():
            k.barrier()
            with ExitStack() as st:
                iota = sb(st, "q_iota", [128, 128], F32)
                Bio = k.buf()
                k.dma("sp", iota[:], I["c_iota"](), write=Bio)
                iota16s = sb(st, "q_iota16s", [128, 16], F32)
                k.op("dve", lambda e: e.tensor_scalar(out=iota16s[:], in0=iota[:, 0:16], scalar1=16.0, scalar2=None,
                                                      op0=ALU.mult), reads=[Bio], writes=[Bio])
                keys = sb(st, "q_keys", [128, 16, 128], F32)
                Bky = k.buf()
                k.dma("sp", keys[:], I["keys_t"]().rearrange("c d n -> d c n"), write=Bky)
                xT = sb(st, "q_xT", [128, KC, 512], BF16)
                BxT = k.buf()
                wr = Ring([(sb(st, f"q_w{i}", [128, KC, 128], BF16), k.buf()) for i in range(2)])
                qT = sb(st, "q_qT", [128, 16, 512], F32)
                BqT = k.buf()
                sc = sb(st, "q_sc", [128, 16, 128], F32)
                sc2 = sb(st, "q_sc2", [128, 16, 128], F32)
                Bsc, Bsc2 = k.buf(), k.buf()
                tops = sb(st, "q_tops", [128, 16, 16], F32)
                topu = sb(st, "q_topu", [128, 16, 16], U32)
                topi = sb(st, "q_topi", [128, 16, 16], F32)
                Btops, Btopu, Btopi = k.buf(), k.buf(), k.buf()
                cand = sc2[:].rearrange("p (h c) n -> p h (c n)", c=2)
                cand2 = sb(st, "q_cand2", [128, 8, 256], F32)
                Bcand, Bcand2 = Bsc2, k.buf()
                bests = sb(st, "q_bests", [128, 8, 16], F32)
                bposu = sb(st, "q_bposu", [128, 8, 16], U32)
                au = sb(st, "q_au", [128, 8, 16], U32)
                bu = sb(st, "q_bu", [128, 8, 16], U32)
                bmod = sb(st, "q_bmod", [128, 8, 16], F32)
                a16 = sb(st, "q_a16", [128, 8, 16], F32)
                Bbests, Bbposu, Bbpos, Bbmod, Ba16 = (k.buf() for _ in range(5))
                oh = cand2[:].rearrange("p h (a b) -> p h a b", a=16)
                Boh = Bcand2
                Iv = sb(st, "q_I", [128, 8, 16], F32)
                Jv = sb(st, "q_J", [128, 8, 16], F32)
                gv = sb(st, "q_g", [128, 8, 16], F32)
                BIv, BJv, Bgv = k.buf(), k.buf(), k.buf()
                gsum = sb(st, "q_gsum", [128, 8], F32)
                Bgsum = k.buf()
                trr = Ring([([sb(st, f"q_tr{i}_{j}", [128, 128], F32) for i in range(3)], [k.buf() for _ in range(3)])
                            for j in range(2)])
                Ar = Ring([(sb(st, f"q_A{i}", [128, 16, 128], BF16), k.buf()) for i in range(2)])
                Br = Ring([(sb(st, f"q_B{i}", [128, 16, 128], BF16), k.buf()) for i in range(2)])
                Gr = Ring([(sb(st, f"q_G{i}", [128, 128, 128], BF16), k.buf()) for i in range(2)])
                Gv = G_d.rearrange("i j t -> j i t")

                for o0 in range(0, TOWN, 512):
                    k.dma("sp", xT[:], xn2T_d[:, :, o0:o0 + 512], reads=[B_xn2T], write=BxT)
                    for hc in range(16):
                        w, Bw = wr.get()
                        k.dma("pool", w[:].rearrange("p c n -> p (c n)"), I["w_q"]()[hc], write=Bw)
                        pt, Bp = psum.get()
                        for c in range(KC):
                            k.op("pe", lambda e: e.matmul(pt[:], lhsT=w[:, c, :], rhs=xT[:, c, :],
                                                          start=(c == 0), stop=(c == KC - 1)),
                                 reads=[Bw, BxT], writes=[Bp], inc=(c == KC - 1))
                        k.op("act", lambda e: e.copy(out=qT[:, hc, :], in_=pt[:]), reads=[Bp], writes=[BqT])
                    def q_front(blk):
                        r0 = o0 + blk * 128
                        tr, Btr = trr.get()
                        for q4 in range(4):
                            pt, Bp = psum.get()
                            for j in range(4):
                                hc = 4 * q4 + j
                                k.op("pe", lambda e: e.matmul(pt[:, j * 128:(j + 1) * 128],
                                                              lhsT=qT[:, hc, blk * 128:(blk + 1) * 128],
                                                              rhs=keys[:, hc, :], start=True, stop=True),
                                     reads=[BqT, Bky], writes=[Bp], inc=(j == 3))
                            k.op("act", lambda e: e.copy(out=sc[:, 4 * q4:4 * q4 + 4, :].rearrange("p a b -> p (a b)"),
                                                         in_=pt[:]), reads=[Bp], writes=[Bsc])
                        for hc in range(16):
                            k.op("dve", lambda e: e.max(out=tops[:, hc, 0:8], in_=sc[:, hc, :]),
                                 reads=[Bsc], writes=[Btops])
                            k.op("dve", lambda e: e.max_index(out=topu[:, hc, 0:8], in_max=tops[:, hc, 0:8],
                                                              in_values=sc[:, hc, :]),
                                 reads=[Bsc, Btops], writes=[Btopu])
                            k.op("dve", lambda e: e.match_replace(out=sc2[:, hc, :], in_to_replace=tops[:, hc, 0:8],
                                                                  in_values=sc[:, hc, :], imm_value=-1e30),
                                 reads=[Bsc, Btops], writes=[Bsc2])
                            k.op("dve", lambda e: e.max(out=tops[:, hc, 8:16], in_=sc2[:, hc, :]),
                                 reads=[Bsc2], writes=[Btops])
                            k.op("dve", lambda e: e.max_index(out=topu[:, hc, 8:16], in_max=tops[:, hc, 8:16],
                                                              in_values=sc2[:, hc, :]),
                                 reads=[Bsc2, Btops], writes=[Btopu])
                        k.op("dve", lambda e: e.tensor_copy(out=topi[:], in_=topu[:]), reads=[Btopu], writes=[Btopi])
                        t4 = tops[:].rearrange("p (h c) a -> p h c a", c=2)
                        k.op("dve", lambda e: e.tensor_tensor(
                            out=cand.rearrange("p h (a b) -> p h a b", a=16),
                            in0=t4[:, :, 0, :].unsqueeze(3).to_broadcast([128, 8, 16, 16]),
                            in1=t4[:, :, 1, :].unsqueeze(2).to_broadcast([128, 8, 16, 16]), op=ALU.add),
                             reads=[Btops], writes=[Bcand])
                        for h in range(8):
                            k.op("dve", lambda e: e.max(out=bests[:, h, 0:8], in_=cand[:, h, :]),
                                 reads=[Bcand], writes=[Bbests])
                            k.op("dve", lambda e: e.max_index(out=bposu[:, h, 0:8], in_max=bests[:, h, 0:8],
                                                              in_values=cand[:, h, :]),
                                 reads=[Bcand, Bbests], writes=[Bbposu])
                            k.op("dve", lambda e: e.match_replace(out=cand2[:, h, :], in_to_replace=bests[:, h, 0:8],
                                                                  in_values=cand[:, h, :], imm_value=-1e30),
                                 reads=[Bcand, Bbests], writes=[Bcand2])
                            k.op("dve", lambda e: e.max(out=bests[:, h, 8:16], in_=cand2[:, h, :]),
                                 reads=[Bcand2], writes=[Bbests])
                            k.op("dve", lambda e: e.max_index(out=bposu[:, h, 8:16], in_max=bests[:, h, 8:16],
                                                              in_values=cand2[:, h, :]),
                                 reads=[Bcand2, Bbests], writes=[Bbposu])
                        k.op("dve", lambda e: e.tensor_single_scalar(out=au[:], in_=bposu[:], scalar=4,
                                                                     op=ALU.logical_shift_right),
                             reads=[Bbposu], writes=[Bbpos])
                        k.op("dve", lambda e: e.tensor_single_scalar(out=bu[:], in_=bposu[:], scalar=15,
                                                                     op=ALU.bitwise_and),
                             reads=[Bbposu], writes=[Bbmod])
                        k.op("dve", lambda e: e.tensor_copy(out=a16[:], in_=au[:]), reads=[Bbpos], writes=[Ba16])
                        k.op("dve", lambda e: e.tensor_copy(out=bmod[:], in_=bu[:]), reads=[Bbmod], writes=[Bbmod])
                        i4 = topi[:].rearrange("p (h c) a -> p h c a", c=2)
                        io16 = iota[:, 0:16].unsqueeze(1).unsqueeze(1).to_broadcast([128, 8, 16, 16])
                        io16s = iota16s[:].unsqueeze(1).unsqueeze(1).to_broadcast([128, 8, 16, 16])
                        k.op("dve", lambda e: e.tensor_tensor(
                            out=oh, in0=io16, in1=a16[:].unsqueeze(3).to_broadcast([128, 8, 16, 16]),
                            op=ALU.is_equal), reads=[Bio, Ba16], writes=[Boh])
                        k.op("dve", lambda e: e.tensor_tensor(
                            out=oh, in0=oh, in1=i4[:, :, 0, :].unsqueeze(2).to_broadcast([128, 8, 16, 16]),
                            op=ALU.mult), reads=[Boh, Btopi], writes=[Boh])
                        k.op("dve", lambda e: e.tensor_reduce(out=Iv[:], in_=oh, axis=AX.X, op=ALU.add),
                             reads=[Boh], writes=[BIv])
                        k.op("dve", lambda e: e.tensor_tensor(
                            out=oh, in0=io16, in1=bmod[:].unsqueeze(3).to_broadcast([128, 8, 16, 16]),
                            op=ALU.is_equal), reads=[Bio, Bbmod, BIv], writes=[Boh])
                        k.op("dve", lambda e: e.tensor_tensor(
                            out=oh, in0=oh, in1=i4[:, :, 1, :].unsqueeze(2).to_broadcast([128, 8, 16, 16]),
                            op=ALU.mult), reads=[Boh, Btopi], writes=[Boh])
                        k.op("dve", lambda e: e.tensor_reduce(out=Jv[:], in_=oh, axis=AX.X, op=ALU.add),
                             reads=[Boh], writes=[BJv])
                        k.op("dve", lambda e: e.tensor_tensor(
                            out=gv[:], in0=bests[:], in1=bests[:, :, 0:1].to_broadcast([128, 8, 16]), op=ALU.subtract),
                             reads=[Bbests], writes=[Bgv])
                        k.op("act", lambda e: e.activation(out=gv[:], in_=gv[:], func=AF.Exp), reads=[Bgv], writes=[Bgv])
                        k.op("dve", lambda e: e.tensor_reduce(out=gsum[:], in_=gv[:], axis=AX.X, op=ALU.add),
                             reads=[Bgv], writes=[Bgsum])
                        k.op("dve", lambda e: e.reciprocal(out=gsum[:], in_=gsum[:]), reads=[Bgsum], writes=[Bgsum])
                        k.op("dve", lambda e: e.tensor_tensor(
                            out=gv[:], in0=gv[:], in1=gsum[:].unsqueeze(2).to_broadcast([128, 8, 16]), op=ALU.mult),
                             reads=[Bgv, Bgsum], writes=[Bgv])
                        for n_, (src, Bsrc) in enumerate(((Iv, BIv), (Jv, BJv), (gv, Bgv))):
                            pt, Bp = psum.get()
                            k.op("pe", lambda e: e.transpose(out=pt[:, 0:128], in_=src[:].rearrange("p h k -> p (h k)"),
                                                             identity=ident_f[:]),
                                 reads=[Bsrc, B_const], writes=[Bp])
                            k.op("act", lambda e: e.copy(out=tr[n_][:], in_=pt[:, 0:128]), reads=[Bp], writes=[Btr[n_]])
                        return tr, Btr

                    def q_back(blk, tr, Btr):
                        r0 = o0 + blk * 128
                        Gst, BGst = Gr.get()
                        IT, JT, gT = tr
                        iob = iota[:].unsqueeze(1).to_broadcast([128, 16, 128])
                        for t16 in range(0, 128, 16):
                            A, BA = Ar.get()
                            Bm, BB = Br.get()
                            k.op("dve", lambda e: e.tensor_tensor(
                                out=A[:], in0=iob, in1=IT[:, t16:t16 + 16].unsqueeze(2).to_broadcast([128, 16, 128]),
                                op=ALU.is_equal), reads=[Bio, Btr[0]], writes=[BA])
                            k.op("dve", lambda e: e.tensor_tensor(
                                out=A[:], in0=A[:], in1=gT[:, t16:t16 + 16].unsqueeze(2).to_broadcast([128, 16, 128]),
                                op=ALU.mult), reads=[BA, Btr[2]], writes=[BA])
                            k.op("dve", lambda e: e.tensor_tensor(
                                out=Bm[:], in0=iob, in1=JT[:, t16:t16 + 16].unsqueeze(2).to_broadcast([128, 16, 128]),
                                op=ALU.is_equal), reads=[Bio, Btr[1]], writes=[BB])
                            for q4 in range(4):
                                pt, Bp = psum.get()
                                for tt in range(4):
                                    k.op("pe", lambda e: e.matmul(pt[:, tt * 128:(tt + 1) * 128], lhsT=Bm[:, 4 * q4 + tt, :],
                                                                  rhs=A[:, 4 * q4 + tt, :], start=True, stop=True),
                                         reads=[BA, BB], writes=[Bp], inc=(tt == 3))
                                tb = t16 + 4 * q4
                                k.op("act", lambda e: e.copy(
                                    out=Gst[:, :, tb:tb + 4],
                                    in_=pt[:].rearrange("p (t i) -> p i t", t=4)), reads=[Bp], writes=[BGst])
                        for i8 in range(8):
                            k.dma("sp", Gv[:, 16 * i8:16 * (i8 + 1), r0:r0 + 128], Gst[:, 16 * i8:16 * (i8 + 1), :],
                                  reads=[BGst], write=B_G)
                    nxt = q_front(0)
                    for blk in range(4):
                        cur = nxt
                        if blk + 1 < 4:
                            nxt = q_front(blk + 1)
                        q_back(blk, *cur)

        if "qg" in phases:
            phase_qg()

        def phase_gemm1():
            k.barrier()
            TB = min(1024, TOWN)
            with ExitStack() as st:
                xT = sb(st, "u_xT", [128, KC, TB], BF16)
                BxT = k.buf()
                wr = Ring([(sb(st, f"u_w{i}", [128, KC, 128], BF16), k.buf()) for i in range(3)])
                hr = Ring([(sb(st, f"u_h{i}", [128, TB], F32), k.buf()) for i in range(2)])
                gr = Ring([(sb(st, f"u_g{i}", [128, TB], BF16), k.buf()) for i in range(3)])
                orr = Ring([(sb(st, f"u_o{i}", [128, TB], BF16), k.buf()) for i in range(3)])
                for o0 in range(0, TOWN, TB):
                    k.dma("sp", xT[:], xn2T_d[:, :, o0:o0 + TB], reads=[B_xn2T], write=BxT)
                    for i in range(128):
                        w, Bw = wr.get()
                        k.dma("pool", w[:].rearrange("p c n -> p (c n)"), I["u_t"]()[i], write=Bw)
                        g, Bg = gr.get()
                        k.dma("sp", g[:], G_d[i, :, o0:o0 + TB], reads=[B_G], write=Bg)
                        hsb, Bh = hr.get()
                        for n0 in range(0, TB, 512):
                            pt, Bp = psum.get()
                            for c in range(KC):
                                k.op("pe", lambda e: e.matmul(pt[:], lhsT=w[:, c, :], rhs=xT[:, c, n0:n0 + 512],
                                                              start=(c == 0), stop=(c == KC - 1)),
                                     reads=[Bw, BxT], writes=[Bp], inc=(c == KC - 1))
                            k.op("act", lambda e: e.activation(out=hsb[:, n0:n0 + 512], in_=pt[:], func=AF.Gelu),
                                 reads=[Bp], writes=[Bh])
                        o, Bo = orr.get()
                        k.op("dve", lambda e: e.tensor_tensor(out=o[:], in0=hsb[:], in1=g[:], op=ALU.mult),
                             reads=[Bh, Bg], writes=[Bo])
                        k.dma("sp", HG_d[i * 128:(i + 1) * 128, o0:o0 + TB], o[:], reads=[Bo], write=B_HG)

        if "gemm1" in phases:
            phase_gemm1()

        def phase_gemm2():
            k.barrier()
            TB = min(1024, TOWN)
            nb = TB // 128
            with ExitStack() as st:
                hgr = Ring([(sb(st, f"v_hg{i}", [128, 2, TB], BF16), k.buf()) for i in range(3)])
                vr = Ring([(sb(st, f"v_v{i}", [128, 2, 512], BF16), k.buf()) for i in range(3)])
                xr = Ring([(sb(st, f"v_x{i}", [128, 512], F32), k.buf()) for i in range(3)])
                orr = Ring([(sb(st, f"v_o{i}", [128, 512], F32), k.buf()) for i in range(3)])
                for o0 in range(0, TOWN, TB):
                    for s in range(8):
                        accs = [psum.get() for _ in range(nb)]
                        for i2 in range(64):
                            hg, Bhg = hgr.get()
                            k.dma("sp", hg[:], HG_d[i2 * 256:(i2 + 1) * 256, o0:o0 + TB].rearrange("(a p) t -> p a t", p=128),
                                  reads=[B_HG], write=Bhg)
                            v, Bv = vr.get()
                            k.dma("sp", v[:], vb_d[i2 * 256:(i2 + 1) * 256, s * 512:(s + 1) * 512].rearrange(
                                "(a p) n -> p a n", p=128), reads=[B_vb], write=Bv)
                            for a in range(2):
                                i = 2 * i2 + a
                                for blk in range(nb):
                                    pt, Bp = accs[blk]
                                    k.op("pe", lambda e: e.matmul(pt[:], lhsT=hg[:, a, blk * 128:(blk + 1) * 128],
                                                                  rhs=v[:, a, :], start=(i == 0), stop=(i == 127)),
                                         reads=[Bhg, Bv], writes=[Bp], inc=(blk == nb - 1))
                        for blk in range(nb):
                            pt, Bp = accs[blk]
                            r0 = o0 + blk * 128
                            xt, Bx = xr.get()
                            k.dma("sp", xt[:], h1_d[r0:r0 + 128, s * 512:(s + 1) * 512], reads=[B_h1], write=Bx)
                            ot, Bo = orr.get()
                            k.op("dve", lambda e: e.tensor_tensor(out=ot[:], in0=pt[:], in1=xt[:], op=ALU.add),
                                 reads=[Bp, Bx], writes=[Bo])
                            k.dma("sp", h2_d[r0:r0 + 128, s * 512:(s + 1) * 512], ot[:], reads=[Bo], write=B_h2)

        if "gemm2" in phases:
            phase_gemm2()

        def phase_final():
            k.barrier()
            with ExitStack() as st:
                gain = sb(st, "f_gain", [128, D], F32)
                Bg = k.buf()
                k.dma("sp", gain[:], bcast_row(I["g_fin"](), D), write=Bg)
                xr = Ring([(sb(st, f"f_x{i}", [128, D], F32), k.buf()) for i in range(2)])
                sq = sb(st, "f_sq", [128, D], F32)
                Bsq = k.buf()
                orr = Ring([(sb(st, f"f_o{i}", [128, D], F32), k.buf()) for i in range(2)])
                smr = Ring([(sb(st, f"f_sm{i}", [128, 2], F32), k.buf()) for i in range(2)])
                for r0 in range(0, TOWN, 128):
                    xt, Bx = xr.get()
                    k.dma("sp", xt[:], h2_d[r0:r0 + 128, :], reads=[B_h2], write=Bx)
                    sm, Bsm = smr.get()
                    k.op("act", lambda e: e.activation(out=sq[:], in_=xt[:], func=AF.Square, accum_out=sm[:, 0:1]),
                         reads=[Bx], writes=[Bsq, Bsm])
                    k.op("act", lambda e: e.activation(out=sm[:, 1:2], in_=sm[:, 0:1], func=AF.Sqrt, bias=EPS,
                                                       scale=1.0 / D), reads=[Bsm], writes=[Bsm])
                    k.op("dve", lambda e: e.reciprocal(out=sm[:, 1:2], in_=sm[:, 1:2]), reads=[Bsm], writes=[Bsm])
                    ot, Bo = orr.get()
                    k.op("dve", lambda e: e.scalar_tensor_tensor(out=ot[:], in0=xt[:], scalar=sm[:, 1:2], in1=gain[:],
                                                                 op0=ALU.mult, op1=ALU.mult),
                         reads=[Bx, Bsm, Bg], writes=[Bo])
                    k.dma("sp", out[r0:r0 + 128, :], ot[:], reads=[Bo], write=B_out, is_output=True)

        if "final" in phases:
            phase_final()

        fin = list(k.out_tokens)
        for b in (B_xnT, B_xbcT, B_ysc, B_rstdsc, B_zs, B_dtr, B_out, B_yT, B_h1, B_xn2T, B_G, B_HG, B_h2, B_vb):
            fin += b.w
        k._wait("sp", fin)
    return nc


def prep_weights(inp):
    W = np.asarray(inp["w_in"])[0]
    o_z, o_x, o_B, o_C, o_dt, o_b, o_c, o_h = 0, 4096, 8192, 9216, 10240, 10304, 14400, 18496
    cols = np.concatenate([np.arange(o_x, o_x + 4096), np.arange(o_B, o_B + 1024), np.arange(o_C, o_C + 1024),
                           np.arange(o_b, o_b + 4096), np.arange(o_c, o_c + 4096), np.arange(o_h, o_h + 4096)])
    wf = W[:, cols]
    w_fm = np.ascontiguousarray(wf.reshape(KC, 128, 144, 128).transpose(2, 1, 0, 3)).reshape(144, 128, KC * 128)
    w_z = np.ascontiguousarray(W[:, 0:4096].reshape(KC, 128, 8, 512).transpose(2, 1, 0, 3)).reshape(8, 128, KC * 512)
    w_dt = np.ascontiguousarray(W[:, o_dt:o_dt + 64].reshape(KC, 128, 64).transpose(1, 0, 2)).reshape(128, KC * 64)
    cw = np.asarray(inp["ssd_conv_w"])[0]
    cw_xbc = np.ascontiguousarray(cw.reshape(4, 48, 128).transpose(2, 1, 0))
    cb_xbc = np.ascontiguousarray(np.asarray(inp["ssd_conv_b"])[0].reshape(48, 128).T)
    cs = np.asarray(inp["sc_conv_w"])[0]
    cw_sc = np.ascontiguousarray(cs.reshape(3, 32, 128).transpose(2, 1, 0))
    g_sc = np.ascontiguousarray(np.asarray(inp["sc_norm_w"])[0].reshape(32, 128).T)
    Wo = np.asarray(inp["w_out"])[0]
    w_out = np.ascontiguousarray(Wo.reshape(2, 32, 128, 8, 512).transpose(3, 0, 2, 1, 4)).reshape(8, 2, 128, 32 * 512)
    Wq = np.asarray(inp["peer_w_query"])[0]
    w_q = np.ascontiguousarray(Wq.reshape(KC, 128, 16, 128).transpose(2, 1, 0, 3)).reshape(16, 128, KC * 128)
    keys = np.asarray(inp["peer_sub_keys"])[0]
    keys_t = np.ascontiguousarray(keys.reshape(16, 128, 128).transpose(0, 2, 1))
    U = np.asarray(inp["peer_u"])[0]
    u_t = np.ascontiguousarray(U.reshape(128, 128, KC, 128).transpose(0, 3, 2, 1)).reshape(128, 128, KC * 128)
    V = np.asarray(inp["peer_v"])[0]
    u = np.arange(128)
    triu = (u[:, None] <= u[None, :]).astype(np.float32)
    negm = np.where(u[:, None] > u[None, :], np.float32(-30000.0), np.float32(0.0)).astype(np.float32)
    return dict(
        w_fm=w_fm, w_z=w_z, w_dt=w_dt, cw_xbc=cw_xbc, cb_xbc=cb_xbc, cw_sc=cw_sc, g_sc=g_sc,
        g_mix=np.asarray(inp["norm_mix_w"])[0], g_ffn=np.asarray(inp["norm_ffn_w"])[0],
        g_fin=np.asarray(inp["norm_final_w"]), g_ssd=np.asarray(inp["ssd_norm_w"])[0],
        dt_bias=np.asarray(inp["ssd_dt_bias"])[0], a_log=np.asarray(inp["ssd_a_log"])[0],
        d_skip=np.asarray(inp["ssd_d"])[0], w_out=w_out, w_q=w_q, keys_t=keys_t, u_t=u_t, v_nat=V,
        c_ident=np.eye(128, dtype=np.float32), c_triu=triu, c_negm=np.tile(negm, (1, 4)),
        c_iota=np.tile(np.arange(128, dtype=np.float32), (128, 1)),
    )


_NC_CACHE = {}


def kernel(**inputs):
    n_cores = 8
    TPRE = TOWN = 2048
    wts = prep_weights(inputs)
    x = np.asarray(inputs["x"])
    if "nc" not in _NC_CACHE:
        _NC_CACHE["nc"] = build(TPRE, TOWN)
    nc = _NC_CACHE["nc"]
    zeros = np.zeros((TPRE, D), np.float32)
    in_maps = []
    for c in range(n_cores):
        b, half = c // 2, c % 2
        own = x[b, half * TOWN:(half + 1) * TOWN]
        pre = x[b, 0:TPRE] if half == 1 else zeros
        m = dict(wts)
        m["x_all"] = np.ascontiguousarray(np.concatenate([pre, own], axis=0))
        m["flag"] = np.full((128, 1), float(half), np.float32)
        in_maps.append(m)
    res = run_bass_kernel_spmd(nc, in_maps, core_ids=list(range(n_cores)))
    out = np.empty((4, 2 * TOWN, D), np.float32)
    for c in range(n_cores):
        b, half = c // 2, c % 2
        out[b, half * TOWN:(half + 1) * TOWN] = res.results[c]["out"]
    return out
```
